# Optimizing a Trainium2 kernel written in Bass

```python
import jax, jax.numpy as jnp
from jax import lax
import numpy as np

D_MODEL = 1024
BATCH = 4
SEQ = 4096
DEPTH = 2

GRID_W = 64
CTX_LEN = 256
EPS = 1e-6

N_MIXERS = 4
D_MIX = D_MODEL
D_GROUP = D_MIX // N_MIXERS
A_HEADS = 4
A_DH = D_GROUP // A_HEADS
B_HEADS = 4
B_DH = D_GROUP // B_HEADS
WIN_H = 8
WIN_W = 16
C_HEADS = 4
C_DV = D_GROUP // C_HEADS
C_DK = C_DV // 2
C_GATE_RANK = 16
C_GATE_NORM = 16.0
D_HEADS = 4
D_NOPE = D_GROUP // D_HEADS
D_V = D_GROUP // D_HEADS
D_ROPE = D_NOPE // 2
ROPE_FREQS = D_ROPE // 4
ROPE_BASE = 10000.0
D_Q_RANK = 3 * D_GROUP // 4
D_KV_RANK = D_GROUP // 2
MLA_SCALE = (D_NOPE + D_ROPE) ** -0.5
Q_BLOCK = 128
CHUNK = 64
N_GROUPS = 4
EXPERTS_PER_GROUP = 8
N_EXPERTS = N_GROUPS * EXPERTS_PER_GROUP
TOP_K = 2
D_EXPERT = D_MODEL // 4
MOE_BLOCK = 256

IN_SIZES = (
    D_GROUP, D_GROUP, D_GROUP, D_GROUP, D_GROUP,
    D_GROUP, D_GROUP, D_GROUP,
    C_HEADS * C_DK, C_HEADS * C_DK, C_HEADS * C_DV, C_HEADS * C_DV,
    C_GATE_RANK, C_GATE_RANK,
    D_Q_RANK, D_KV_RANK, D_ROPE,
)
D_IN = sum(IN_SIZES)

kernel_name = 'hybrid_parallel_heads_dit_moe'


def rmsnorm(x, g):
    xf = x.astype(jnp.float32)
    y = xf * lax.rsqrt(jnp.mean(xf * xf, axis=-1, keepdims=True) + EPS)
    return (y * g.astype(jnp.float32)).astype(x.dtype)


def split_heads(t, n_heads):
    return t.reshape(t.shape[:-1] + (n_heads, t.shape[-1] // n_heads))


def merge_heads(t):
    return t.reshape(t.shape[:-2] + (t.shape[-2] * t.shape[-1],))


def flip(t):
    return jnp.flip(t, axis=1)


def split_projection(p):
    return jnp.split(p, np.cumsum(IN_SIZES)[:-1].tolist(), axis=-1)


def rope_2d(u, cos, sin):
    v = u.reshape(u.shape[:-1] + (2, 2, ROPE_FREQS))
    u1, u2 = v[..., 0, :], v[..., 1, :]
    out = jnp.stack([u1 * cos - u2 * sin, u1 * sin + u2 * cos], axis=-2)
    return out.reshape(u.shape).astype(u.dtype)


def chunk_gated_recurrence(q, k, v, log_a, s0):
    b, t, h, _ = q.shape
    dv = v.shape[-1]
    n = t // CHUNK

    def chunks(u):
        return jnp.moveaxis(u.astype(jnp.float32).reshape(b, n, CHUNK, h, u.shape[-1]), 1, 0)

    lower = jnp.tril(jnp.ones((CHUNK, CHUNK), dtype=bool))[None, :, :, None, None]

    def step(state, inp):
        qc, kc, vc, gc = inp
        cum = jnp.cumsum(gc, axis=1)
        rel = jnp.exp(jnp.where(lower, cum[:, :, None] - cum[:, None], -jnp.inf))
        att = jnp.einsum('bihk,bjhk,bijhk->bhij', qc, kc, rel)
        o = (jnp.einsum('bhij,bjhv->bihv', att, vc)
             + jnp.einsum('bihk,bhkv->bihv', qc * jnp.exp(cum), state))
        last = cum[:, -1]
        state = (jnp.exp(last)[..., None] * state
                 + jnp.einsum('bjhk,bjhv->bhkv', kc * jnp.exp(last[:, None] - cum), vc))
        return state, o

    state, o = lax.scan(step, s0, (chunks(q), chunks(k), chunks(v), chunks(log_a)))
    o = jnp.moveaxis(o, 0, 1).reshape(b, t, h, dv)
    return o.astype(v.dtype), state


def bidirectional_recurrence(lat, ctx):
    def both_directions(q, v, k_f, la_f, k_b, la_b, s_f, s_b):
        o_f, s_f = chunk_gated_recurrence(q, k_f, v, la_f, s_f)
        o_b, s_b = chunk_gated_recurrence(flip(q), flip(k_b), flip(v), flip(la_b), s_b)
        return o_f + flip(o_b), s_f, s_b

    qc, vc = ctx[0], ctx[1]
    s0 = jnp.zeros((qc.shape[0], qc.shape[2], qc.shape[3], vc.shape[3]), jnp.float32)
    o_ctx, s_f, s_b = both_directions(*ctx, s0, s0)
    o_lat, _, _ = both_directions(*lat, s_f, s_b)
    return o_lat, o_ctx


def hgrn_lower_bounds(logits):
    cum = jnp.cumsum(jax.nn.softmax(logits.astype(jnp.float32), axis=0), axis=0)
    return cum - cum[0]


def hgrn2_mixer(cols_lat, cols_ctx, lb, norm_g, with_ctx_out):
    def prep(q, i, f_fwd, f_bwd):
        q = split_heads(jax.nn.silu(q), A_HEADS) * (A_DH ** -0.5)
        v = split_heads(i, A_HEADS)
        dirs = []
        for f_logit, lbd in ((f_fwd, lb[0]), (f_bwd, lb[1])):
            z = f_logit.astype(jnp.float32)
            log_f = jnp.logaddexp(jnp.log(lbd), jnp.log1p(-lbd) + jax.nn.log_sigmoid(z))
            k = (1.0 - lbd) * jax.nn.sigmoid(-z)
            dirs += [split_heads(k, A_HEADS), split_heads(log_f, A_HEADS)]
        return (q, v, *dirs)

    o_lat, o_ctx = bidirectional_recurrence(prep(*cols_lat[:4]), prep(*cols_ctx[:4]))

    def readout(o, g):
        return merge_heads(rmsnorm(o, norm_g.reshape(A_HEADS, A_DH))) * jax.nn.silu(g)

    return readout(o_lat, cols_lat[4]), (readout(o_ctx, cols_ctx[4]) if with_ctx_out else None)


def gla_mixer(cols_lat, cols_ctx, wg_f, bg_f, wg_b, bg_b, norm_g, with_ctx_out):
    def prep(q, k, v, g, z_f, z_b):
        q = split_heads(q, C_HEADS) * (C_DK ** -0.5)
        k = split_heads(k, C_HEADS)
        v = split_heads(v, C_HEADS)
        la_f = split_heads(jax.nn.log_sigmoid((z_f @ wg_f + bg_f).astype(jnp.float32)) / C_GATE_NORM, C_HEADS)
        la_b = split_heads(jax.nn.log_sigmoid((z_b @ wg_b + bg_b).astype(jnp.float32)) / C_GATE_NORM, C_HEADS)
        return (q, v, k, la_f, k, la_b)

    o_lat, o_ctx = bidirectional_recurrence(prep(*cols_lat), prep(*cols_ctx))

    def readout(o, g):
        return merge_heads(rmsnorm(o, norm_g.reshape(C_HEADS, C_DV))) * jax.nn.silu(g)

    return readout(o_lat, cols_lat[3]), (readout(o_ctx, cols_ctx[3]) if with_ctx_out else None)


def neighbourhood_attention_mixer(cols_lat, cols_ctx, rpb, with_ctx_out):
    q, k, v = (split_heads(u, B_HEADS) for u in cols_lat)
    qc, kc, vc = (split_heads(u, B_HEADS) for u in cols_ctx)
    b, n = q.shape[:2]
    rows_n = n // GRID_W
    kh = min(WIN_H, rows_n)
    scale = B_DH ** -0.5
    r = jnp.arange(rows_n)
    row_idx = jnp.clip(r - kh // 2, 0, rows_n - kh)[:, None] + jnp.arange(kh)[None]
    cidx = jnp.arange(GRID_W)
    c_start = jnp.clip(cidx - WIN_W // 2, 0, GRID_W - WIN_W)
    col_in = (cidx[None] >= c_start[:, None]) & (cidx[None] < c_start[:, None] + WIN_W)
    dr = row_idx - r[:, None] + (WIN_H - 1)
    dc = jnp.clip(cidx[None] - cidx[:, None], -(WIN_W - 1), WIN_W - 1) + (WIN_W - 1)
    bias = rpb[:, dr[:, None, :, None], dc[None, :, None, :]].astype(jnp.float32)

    qg = q.reshape(b, rows_n, GRID_W, B_HEADS, B_DH) * scale
    kg = k.reshape(b, rows_n, GRID_W, B_HEADS, B_DH)[:, row_idx]
    vg = v.reshape(b, rows_n, GRID_W, B_HEADS, B_DH)[:, row_idx]
    s_win = jnp.einsum('brqhd,brjwhd->bhrqjw', qg, kg).astype(jnp.float32) + bias[None]
    s_win = jnp.where(col_in[:, None, :], s_win, -jnp.inf)
    s_ctx = jnp.einsum('brqhd,blhd->bhrql', qg, kc).astype(jnp.float32)
    n_win = kh * GRID_W
    s = jnp.concatenate([s_win.reshape(b, B_HEADS, rows_n, GRID_W, n_win), s_ctx], axis=-1)
    p = jax.nn.softmax(s, axis=-1).astype(v.dtype)
    p_win = p[..., :n_win].reshape(b, B_HEADS, rows_n, GRID_W, kh, GRID_W)
    o = (jnp.einsum('bhrqjw,brjwhd->brqhd', p_win, vg)
         + jnp.einsum('bhrql,blhd->brqhd', p[..., n_win:], vc))
    o_lat = o.reshape(b, n, D_GROUP)
    o_ctx = None
    if with_ctx_out:
        s_cc = jnp.einsum('bqhd,blhd->bhql', qc * scale, kc).astype(jnp.float32)
        o_ctx = merge_heads(jnp.einsum('bhql,blhd->bqhd', jax.nn.softmax(s_cc, axis=-1).astype(vc.dtype), vc))
    return o_lat, o_ctx


def mla_attend(qn, qr, kn, kr, v):
    s = (jnp.einsum('bqhd,bkhd->bhqk', qn, kn) + jnp.einsum('bqhr,bkr->bhqk', qr, kr)).astype(jnp.float32) * MLA_SCALE
    p = jax.nn.softmax(s, axis=-1).astype(v.dtype)
    return jnp.einsum('bhqk,bkhd->bqhd', p, v)


def mla_mixer(cols_lat, cols_ctx, q_norm_g, w_uq, kv_norm_g, w_ukv, cos, sin, with_ctx_out):
    def prep(cq, ckv, k_rope):
        q = split_heads(rmsnorm(cq, q_norm_g) @ w_uq, D_HEADS)
        kv = split_heads(rmsnorm(ckv, kv_norm_g) @ w_ukv, D_HEADS)
        return q[..., :D_NOPE], q[..., D_NOPE:], kv[..., :D_NOPE], k_rope, kv[..., D_NOPE:]

    qn, qr, kn, kr, v = prep(*cols_lat)
    qr = rope_2d(qr, cos[:, None], sin[:, None])
    kr = rope_2d(kr, cos, sin)
    qnc, qrc, knc, krc, vc = prep(*cols_ctx)
    kn_all = jnp.concatenate([knc, kn], axis=1)
    kr_all = jnp.concatenate([krc, kr], axis=1)
    v_all = jnp.concatenate([vc, v], axis=1)
    b, n = qn.shape[:2]
    nb = n // Q_BLOCK

    def blocks(u):
        return jnp.moveaxis(u.reshape((b, nb, Q_BLOCK) + u.shape[2:]), 1, 0)

    o = lax.map(lambda qs: mla_attend(qs[0], qs[1], kn_all, kr_all, v_all), (blocks(qn), blocks(qr)))
    o_lat = jnp.moveaxis(o, 0, 1).reshape(b, n, D_GROUP)
    o_ctx = merge_heads(mla_attend(qnc, qrc, knc, krc, vc)) if with_ctx_out else None
    return o_lat, o_ctx


def hierarchical_moe(h, w_rg, b_rg, w_re, b_re, w_gu, w_dn):
    t_tok, d = h.shape
    hf = h.astype(jnp.float32)
    g_logits = hf @ w_rg.astype(jnp.float32) + b_rg.astype(jnp.float32)
    _, g_sel = lax.top_k(g_logits, 1)
    p_group = jnp.take_along_axis(jax.nn.softmax(g_logits, axis=-1), g_sel, axis=-1)
    e_logits = (hf @ w_re.astype(jnp.float32) + b_re.astype(jnp.float32)).reshape(t_tok, N_GROUPS, EXPERTS_PER_GROUP)
    e_in_group = jnp.take_along_axis(e_logits, g_sel[:, :, None], axis=1)[:, 0]
    w_top, i_top = lax.top_k(jax.nn.softmax(e_in_group, axis=-1), TOP_K)
    w_top = p_group * w_top / jnp.sum(w_top, axis=-1, keepdims=True)
    expert_id = (g_sel * EXPERTS_PER_GROUP + i_top).reshape(-1)
    n_assign = t_tok * TOP_K
    token_id = jnp.repeat(jnp.arange(t_tok), TOP_K)
    order = jnp.argsort(expert_id)
    e_sorted, tok_sorted, w_sorted = expert_id[order], token_id[order], w_top.reshape(-1)[order]
    counts = jnp.zeros((N_EXPERTS,), jnp.int32).at[expert_id].add(1)
    starts = jnp.cumsum(counts) - counts
    padded = (counts + MOE_BLOCK - 1) // MOE_BLOCK * MOE_BLOCK
    padded_end = jnp.cumsum(padded)
    dest = (padded_end - padded)[e_sorted] + jnp.arange(n_assign) - starts[e_sorted]
    n_blocks = -(-n_assign // MOE_BLOCK) + N_EXPERTS
    xs = jnp.zeros((n_blocks * MOE_BLOCK, d), h.dtype).at[dest].set(h[tok_sorted])
    block_expert = jnp.minimum(jnp.searchsorted(padded_end, jnp.arange(n_blocks) * MOE_BLOCK, side='right'), N_EXPERTS - 1)

    def expert_block(args):
        xb, e = args
        gu = xb @ w_gu[e]
        return (jax.nn.silu(gu[:, :D_EXPERT]) * gu[:, D_EXPERT:]) @ w_dn[e]

    ys = lax.map(expert_block, (xs.reshape(n_blocks, MOE_BLOCK, d), block_expert)).reshape(-1, d)
    y = jnp.zeros((t_tok, d), jnp.float32).at[tok_sorted].add(ys[dest].astype(jnp.float32) * w_sorted[:, None])
    return y.astype(h.dtype)


def setup_inputs(seed: int = 0) -> dict:
    key = jax.random.key(seed)
    ks = iter(jax.random.split(key, 32))

    def normal(shape, scale):
        return scale * jax.random.normal(next(ks), shape, jnp.float32)

    def gain(shape):
        return 1.0 + normal(shape, 0.05)

    D = D_MODEL
    return {
        'x': normal((BATCH, SEQ, D), 1.0),
        'c': normal((BATCH, D), 1.0),
        'ctx': normal((BATCH, CTX_LEN, D), 1.0),
        'c_ctx': normal((D,), 1.0),
        'w_mod': normal((DEPTH, D, 6 * D), 0.5 * D ** -0.5),
        'b_mod': normal((DEPTH, 6 * D), 0.02),
        'norm1_g': gain((DEPTH, D)),
        'norm2_g': gain((DEPTH, D)),
        'w_in': normal((DEPTH, D, D_IN), D ** -0.5),
        'w_out': normal((DEPTH, D_MIX, D), D_MIX ** -0.5),
        'hgrn_lb_logits': normal((DEPTH, 2, D_GROUP), 0.5),
        'hgrn_norm_g': gain((DEPTH, D_GROUP)),
        'na_rpb': normal((DEPTH, B_HEADS, 2 * WIN_H - 1, 2 * WIN_W - 1), 0.5),
        'gla_wg_f': normal((DEPTH, C_GATE_RANK, C_HEADS * C_DK), C_GATE_RANK ** -0.5),
        'gla_bg_f': normal((DEPTH, C_HEADS * C_DK), 0.1),
        'gla_wg_b': normal((DEPTH, C_GATE_RANK, C_HEADS * C_DK), C_GATE_RANK ** -0.5),
        'gla_bg_b': normal((DEPTH, C_HEADS * C_DK), 0.1),
        'gla_norm_g': gain((DEPTH, D_GROUP)),
        'mla_q_norm_g': gain((DEPTH, D_Q_RANK)),
        'mla_w_uq': normal((DEPTH, D_Q_RANK, D_HEADS * (D_NOPE + D_ROPE)), D_Q_RANK ** -0.5),
        'mla_kv_norm_g': gain((DEPTH, D_KV_RANK)),
        'mla_w_ukv': normal((DEPTH, D_KV_RANK, D_HEADS * (D_NOPE + D_V)), D_KV_RANK ** -0.5),
        'moe_w_rg': normal((DEPTH, D, N_GROUPS), D ** -0.5),
        'moe_b_rg': normal((DEPTH, N_GROUPS), 0.01),
        'moe_w_re': normal((DEPTH, D, N_EXPERTS), D ** -0.5),
        'moe_b_re': normal((DEPTH, N_EXPERTS), 0.01),
        'moe_w_gu': normal((DEPTH, N_EXPERTS, D, 2 * D_EXPERT), D ** -0.5),
        'moe_w_dn': normal((DEPTH, N_EXPERTS, D_EXPERT, D), D_EXPERT ** -0.5),
        'final_norm_g': gain((D,)),
    }


def reference(x, c, ctx, c_ctx, w_mod, b_mod, norm1_g, norm2_g, w_in, w_out, hgrn_lb_logits, hgrn_norm_g,
              na_rpb, gla_wg_f, gla_bg_f, gla_wg_b, gla_bg_b, gla_norm_g, mla_q_norm_g, mla_w_uq,
              mla_kv_norm_g, mla_w_ukv, moe_w_rg, moe_b_rg, moe_w_re, moe_b_re, moe_w_gu, moe_w_dn, final_norm_g):
    b, n, _ = x.shape
    l_ctx = ctx.shape[1]
    t = jnp.arange(n)
    inv_freq = ROPE_BASE ** (-jnp.arange(ROPE_FREQS, dtype=jnp.float32) / ROPE_FREQS)
    ang = jnp.stack([(t // GRID_W).astype(jnp.float32)[:, None] * inv_freq,
                     (t % GRID_W).astype(jnp.float32)[:, None] * inv_freq], axis=1)
    cos, sin = jnp.cos(ang), jnp.sin(ang)
    lower_bounds = hgrn_lower_bounds(hgrn_lb_logits)
    c_act = jax.nn.silu(c)
    c_ctx_act = jax.nn.silu(c_ctx)
    xc = ctx
    for layer in range(DEPTH):
        keep_ctx = layer < DEPTH - 1
        m_lat = jnp.split((c_act @ w_mod[layer] + b_mod[layer])[:, None, :], 6, axis=-1)
        m_ctx = jnp.split(c_ctx_act @ w_mod[layer] + b_mod[layer], 6, axis=-1)
        h = rmsnorm(x, norm1_g[layer]) * (1.0 + m_lat[1]) + m_lat[0]
        hc = rmsnorm(xc, norm1_g[layer]) * (1.0 + m_ctx[1]) + m_ctx[0]
        p = split_projection(h @ w_in[layer])
        pc = split_projection(hc @ w_in[layer])
        oa, oac = hgrn2_mixer(p[0:5], pc[0:5], lower_bounds[layer], hgrn_norm_g[layer], keep_ctx)
        ob, obc = neighbourhood_attention_mixer(p[5:8], pc[5:8], na_rpb[layer], keep_ctx)
        og, ogc = gla_mixer(p[8:14], pc[8:14], gla_wg_f[layer], gla_bg_f[layer], gla_wg_b[layer], gla_bg_b[layer],
                            gla_norm_g[layer], keep_ctx)
        od, odc = mla_mixer(p[14:17], pc[14:17], mla_q_norm_g[layer], mla_w_uq[layer], mla_kv_norm_g[layer],
                            mla_w_ukv[layer], cos, sin, keep_ctx)
        x = x + m_lat[2] * (jnp.concatenate([oa, ob, og, od], axis=-1) @ w_out[layer])
        h2 = rmsnorm(x, norm2_g[layer]) * (1.0 + m_lat[4]) + m_lat[3]
        moe_args = (moe_w_rg[layer], moe_b_rg[layer], moe_w_re[layer], moe_b_re[layer], moe_w_gu[layer], moe_w_dn[layer])
        if keep_ctx:
            xc = xc + m_ctx[2] * (jnp.concatenate([oac, obc, ogc, odc], axis=-1) @ w_out[layer])
            h2c = rmsnorm(xc, norm2_g[layer]) * (1.0 + m_ctx[4]) + m_ctx[3]
            y = hierarchical_moe(jnp.concatenate([h2c.reshape(-1, D_MODEL), h2.reshape(-1, D_MODEL)], axis=0), *moe_args)
            xc = xc + m_ctx[5] * y[: b * l_ctx].reshape(b, l_ctx, D_MODEL)
            x = x + m_lat[5] * y[b * l_ctx:].reshape(b, n, D_MODEL)
        else:
            y = hierarchical_moe(h2.reshape(-1, D_MODEL), *moe_args)
            x = x + m_lat[5] * y.reshape(b, n, D_MODEL)
    return rmsnorm(x, final_norm_g)
```

```python
import numpy as np
from contextlib import ExitStack
import ml_dtypes
import concourse.bass as bass
import concourse.mybir as mybir
from concourse.bass_utils import run_bass_kernel_spmd

F32 = mybir.dt.float32
BF16 = mybir.dt.bfloat16
I32 = mybir.dt.int32
AF = mybir.ActivationFunctionType
ALU = mybir.AluOpType
AX = mybir.AxisListType

D = 1024
DIN = 3200
EPS = 1e-6
NEG = -30000.0
MOEB = 256
NEXP = 32
RSTAGE = 99
SCL = float(np.exp(-30.0))


class Prog:
    def __init__(self, nc, es, n_dma_sems=14):
        self.nc = nc
        self.es = es
        self.engs = {'pe': nc.tensor, 'act': nc.scalar, 'dve': nc.vector, 'pool': nc.gpsimd, 'sp': nc.sync}
        self.sem = {}
        self.cnt = {}
        for e in ('pe', 'act', 'dve', 'pool'):
            self.sem[e] = es.enter_context(nc.semaphore('s_' + e))
            self.cnt[e] = 0
        self.dsems = {}
        self.dcnt = {}
        self.dnext = {}
        for q in ('sp', 'pool'):
            self.dsems[q] = [es.enter_context(nc.semaphore('d_%s%d' % (q, i))) for i in range(n_dma_sems)]
            self.dcnt[q] = [0] * n_dma_sems
            self.dnext[q] = 0
        self.waited = {}
        self.lastw = {}
        self.readers = {}
        self.nbuf = 0

    def sb(self, st, shape, dt, name=None):
        self.nbuf += 1
        return st.enter_context(self.nc.sbuf_tensor(name or ('sb%d' % self.nbuf), list(shape), dt))

    def ps(self, st, shape, dt=F32, name=None):
        self.nbuf += 1
        return st.enter_context(self.nc.psum_tensor(name or ('ps%d' % self.nbuf), list(shape), dt))

    def _key(self, k):
        if isinstance(k, (str, tuple)):
            return k
        t = getattr(k, 'tensor', k)
        return getattr(t, 'name', None) or id(t)

    def _wait(self, ename, ev):
        sem, val = ev
        k = (ename, sem.num)
        if self.waited.get(k, 0) >= val:
            return
        self.waited[k] = val
        self.engs[ename].wait_ge(sem, val)

    def _deps(self, ename, reads, writes):
        deps = []
        for k in reads:
            k = self._key(k)
            if k in self.lastw:
                deps.append(self.lastw[k])
        for k in writes:
            k = self._key(k)
            if k in self.lastw:
                deps.append(self.lastw[k])
            deps.extend(self.readers.get(k, []))
        for ev in deps:
            self._wait(ename, ev)

    def _record(self, ev, reads, writes):
        for k in reads:
            k = self._key(k)
            self.readers.setdefault(k, []).append(ev)
        for k in writes:
            k = self._key(k)
            self.lastw[k] = ev
            self.readers[k] = []

    def op(self, ename, fn, reads=(), writes=()):
        self._deps(ename, reads, writes)
        ins = fn(self.engs[ename])
        self.cnt[ename] += 1
        ins.then_inc(self.sem[ename], 1)
        ev = (self.sem[ename], self.cnt[ename])
        self._record(ev, reads, writes)
        if getattr(self, '_yield', None):
            self._yield()
        return ev

    def _dma_common(self, q, emit, reads, writes):
        i = self.dnext[q]
        self.dnext[q] = (i + 1) % len(self.dsems[q])
        sem = self.dsems[q][i]
        if self.dcnt[q][i] > 0:
            self._wait(q, (sem, self.dcnt[q][i]))
        self._deps(q, reads, writes)
        ins = emit(self.engs[q])
        self.dcnt[q][i] += 16
        ins.then_inc(sem, 16)
        ev = (sem, self.dcnt[q][i])
        self._record(ev, reads, writes)
        if getattr(self, '_yield', None):
            self._yield()
        return ev

    def dma(self, q, out, in_, reads=(), writes=(), **kw):
        return self._dma_common(q, lambda e: e.dma_start(out=out, in_=in_, **kw), reads, writes)

    def idma(self, out, out_off, in_, in_off, reads=(), writes=(), **kw):
        return self._dma_common('pool', lambda e: e.indirect_dma_start(out=out, out_offset=out_off, in_=in_,
                                                                      in_offset=in_off, **kw), reads, writes)

    def interleave(self, fns):
        import threading
        n = len(fns)
        il = {'turn': 0, 'alive': [True] * n, 'cond': threading.Condition(), 'exc': None}
        tl = threading.local()

        def advance(i):
            for k in range(1, n + 1):
                j = (i + k) % n
                if il['alive'][j]:
                    il['turn'] = j
                    break
            il['cond'].notify_all()

        def wait_turn(i):
            while il['turn'] != i and il['exc'] is None:
                il['cond'].wait()

        def yield_():
            i = getattr(tl, 'idx', None)
            if i is None:
                return
            advance(i)
            wait_turn(i)
            if il['exc'] is not None:
                raise RuntimeError('interleave peer failed')

        def runner(i):
            with il['cond']:
                wait_turn(i)
                tl.idx = i
                try:
                    if il['exc'] is None:
                        fns[i]()
                except BaseException as e:
                    if il['exc'] is None:
                        il['exc'] = e
                il['alive'][i] = False
                if any(il['alive']):
                    advance(i)
                il['cond'].notify_all()

        prev = getattr(self, '_yield', None)
        self._yield = yield_
        ths = [threading.Thread(target=runner, args=(i,)) for i in range(n)]
        for t in ths:
            t.start()
        for t in ths:
            t.join()
        self._yield = prev
        if il['exc'] is not None:
            raise il['exc']

    def uk(self):
        self._ukn = getattr(self, '_ukn', 0) + 1
        return ('uk', self._ukn)

    def barrier(self):
        evs = [(self.sem[e], self.cnt[e]) for e in self.sem if self.cnt[e] > 0]
        for q in self.dsems:
            for i, s in enumerate(self.dsems[q]):
                if self.dcnt[q][i] > 0:
                    evs.append((s, self.dcnt[q][i]))
        for e in self.engs:
            for ev in evs:
                self._wait(e, ev)
        self.lastw = {}
        self.readers = {}


def host_consts(L, N):
    T = L + N
    c = {}
    c['ident_f'] = np.eye(128, dtype=np.float32)
    j = np.arange(128)[:, None]
    i = np.arange(128)[None, :]
    same = (j // 64) == (i // 64)
    U2f = (same & (j <= i)).astype(np.float32)
    Umf = (same & (j % 64 <= 31)).astype(np.float32)
    U2b = (same & (j >= i)).astype(np.float32)
    Umb = (same & (j % 64 >= 32)).astype(np.float32)
    c['uw_f'] = np.concatenate([U2f, U2f - Umf], axis=1)
    c['uw_b'] = np.concatenate([U2b, U2b - Umb], axis=1)
    c['v2_f'] = (same & (j > i)).astype(np.float32)
    c['v2_b'] = (same & (j < i)).astype(np.float32)
    jj = np.arange(64)[:, None]
    ii = np.arange(64)[None, :]
    c['mask_f'] = ((jj <= ii).astype(np.float64) * np.exp(60.0)).astype(np.float32)
    c['mask_b'] = ((jj >= ii).astype(np.float64) * np.exp(60.0)).astype(np.float32)
    c['lstrict'] = (j < i).astype(np.float32)
    t = np.arange(N)
    inv = (10000.0 ** (-np.arange(8, dtype=np.float32) / 8)).astype(np.float32)
    ang = np.stack([(t // 64).astype(np.float32)[:, None] * inv, (t % 64).astype(np.float32)[:, None] * inv], axis=1)
    rope = np.zeros((T, 2, 2, 8), np.float32)
    rope[:L, 0] = 1.0
    rope[L:, 0] = np.cos(ang)
    rope[L:, 1] = np.sin(ang)
    c['rope'] = rope
    c['pcol'] = np.arange(128, dtype=np.float32)[:, None].copy()
    return c


def na_bias_table(rpb):
    cidx = np.arange(64)
    c_start = np.clip(cidx - 8, 0, 48)
    col_in = (cidx[None] >= c_start[:, None]) & (cidx[None] < c_start[:, None] + 16)
    dc = np.clip(cidx[None] - cidx[:, None], -15, 15) + 15
    out = np.full((2, 4, 8, 64, 8, 64), NEG, np.float32)
    for cls in range(8):
        for jx in range(8):
            dr = jx + 7 - cls
            g = rpb[:, :, dr, :][:, :, dc]
            g = np.where(col_in[None, None], g, NEG)
            out[:, :, cls, :, jx, :] = np.transpose(g, (0, 1, 3, 2))
    return out


def build(L, N, dbg=False):
    T = L + N
    NT = T // 128
    NCH = T // 64
    LT = L // 128
    ROWS = N // 64
    NBLK = (2 * T + MOEB - 1) // MOEB + NEXP
    NSLOT = NBLK * MOEB
    nc = bass.Bass("TRN2", target_bir_lowering=False)

    def din(name, shape, dt=F32):
        return nc.dram_tensor(name, list(shape), dt, kind="ExternalInput").ap()

    def dscr(name, shape, dt=F32):
        kind = "ExternalOutput" if dbg else "Internal"
        return nc.dram_tensor(name, list(shape), dt, kind=kind).ap()

    xin = din('xin', [T, D])
    c2T = din('c2T', [128, 8, 2])
    w_mod = din('w_mod', [2, D, 6 * D])
    b_mod = din('b_mod', [2, 6 * D])
    norm1_g = din('norm1_g', [2, D])
    norm2_g = din('norm2_g', [2, D])
    final_g = din('final_norm_g', [1, D])
    w_in = din('w_in', [2, D, DIN])
    w_out = din('w_out', [2, D, D])
    lb_logits = din('hgrn_lb_logits', [2, 512])
    hgrn_ng = din('hgrn_norm_g', [2, 256])
    na_tab = din('na_tab', [2, 4, 8, 64, 512])
    wg_bd = din('gla_wg_bd', [2, 32, 256])
    bg_cat = din('gla_bg_cat', [2, 256])
    gla_ng = din('gla_norm_g', [2, 256])
    mla_qg = din('mla_q_norm_g', [2, 192, 1])
    mla_wuq = din('mla_w_uq', [2, 192, 384])
    mla_kvg = din('mla_kv_norm_g', [2, 128, 1])
    mla_wukv = din('mla_w_ukv', [2, 128, 512])
    w_r = din('moe_w_r', [2, D, 36])
    b_r = din('moe_b_r', [2, 36])
    w_gu = din('moe_w_gu', [2 * NEXP * D, 512])
    w_dn = din('moe_w_dn', [2 * NEXP * 256, D])
    ident_f_d = din('ident_f', [128, 128])
    uw_d = {'f': din('uw_f', [128, 256]), 'b': din('uw_b', [128, 256])}
    v2_d = {'f': din('v2_f', [128, 128]), 'b': din('v2_b', [128, 128])}
    mask_d = {'f': din('mask_f', [64, 64]), 'b': din('mask_b', [64, 64])}
    lstrict_d = din('lstrict', [128, 128])
    rope_d = din('rope', [T, 32])
    pcol_d = din('pcol', [128, 1])
    out = nc.dram_tensor('out', [N, D], F32, kind="ExternalOutput").ap()

    X = dscr('X', [T, D])
    MOD = dscr('MOD', [2, 2, 6 * D])
    mix = {}
    for m in 'AC':
        for d in 'fb':
            mix[m + d + 'QT'] = dscr('s_%s%s_QT' % (m, d), [256, T], BF16)
            mix[m + d + 'KT'] = dscr('s_%s%s_KT' % (m, d), [256, T], BF16)
            mix[m + d + 'QH'] = dscr('s_%s%s_QH' % (m, d), [256, T], BF16)
            mix[m + d + 'KH'] = dscr('s_%s%s_KH' % (m, d), [T, 256], BF16)
            mix[m + d + 'DEC'] = dscr('s_%s%s_DEC' % (m, d), [256, NCH])
            mix[m + d + 'O'] = dscr('s_%s%s_O' % (m, d), [T, 256])
        mix[m + 'V'] = dscr('s_%s_V' % m, [T, 256], BF16)
        mix[m + 'G'] = dscr('s_%s_G' % m, [T, 256])
    BQT = dscr('s_B_QT', [256, T], BF16)
    BKT = dscr('s_B_KT', [256, T], BF16)
    BV = dscr('s_B_V', [T, 256], BF16)
    DQT = dscr('s_D_QT', [4, 96, T], BF16)
    DKT = dscr('s_D_KT', [4, 96, T], BF16)
    DV = dscr('s_D_V', [T, 256], BF16)
    CATT = dscr('s_CATT', [D, T], BF16)
    H2 = dscr('s_H2', [T, D], BF16)
    XS = dscr('s_XS', [NSLOT, D], BF16)
    YS = dscr('s_YS', [NSLOT, D])

    with ExitStack() as es:
        p = Prog(nc, es)
        ident_f = p.sb(es, [128, 128], F32, 'ident_f_sb')
        ident_b = p.sb(es, [128, 128], BF16, 'ident_b_sb')
        ones_f = p.sb(es, [128, 128], F32, 'ones_f')
        ones_b = p.sb(es, [128, 128], BF16, 'ones_b')
        p.dma('sp', ident_f[:], ident_f_d, writes=[ident_f])
        p.op('dve', lambda e: e.tensor_copy(ident_b[:], ident_f[:]), reads=[ident_f], writes=[ident_b])
        p.op('dve', lambda e: e.memset(ones_f[:], 1.0), writes=[ones_f])
        p.op('dve', lambda e: e.memset(ones_b[:], 1.0), writes=[ones_b])

        def transpose_f(ps_ap, in_ap, reads, writes):
            n = in_ap.shape[0]
            p.op('pe', lambda e: e.transpose(ps_ap, in_ap, ident_f[0:n, 0:n]), reads=list(reads) + [ident_f], writes=writes)

        def transpose_b(ps_ap, in_ap, reads, writes):
            n = in_ap.shape[0]
            p.op('pe', lambda e: e.transpose(ps_ap, in_ap, ident_b[0:n, 0:n]), reads=list(reads) + [ident_b], writes=writes)

        def rsqrt(ap, keys):
            p.op('act', lambda e: e.activation(ap, ap, AF.Ln), reads=keys, writes=keys)
            p.op('act', lambda e: e.activation(ap, ap, AF.Exp, scale=-0.5), reads=keys, writes=keys)

        with ExitStack() as ph:
            xt = [p.sb(ph, [128, D], F32) for _ in range(2)]
            for tt in range(NT):
                b = xt[tt % 2]
                p.dma('sp', b[:], xin[tt * 128:(tt + 1) * 128, :], writes=[b])
                p.dma('sp', X[tt * 128:(tt + 1) * 128, :], b[:], reads=[b], writes=[('X', tt)])
            cT = p.sb(ph, [128, 8, 2], F32)
            sg = p.sb(ph, [128, 8, 2], F32)
            p.dma('sp', cT[:], c2T, writes=[cT])
            p.op('act', lambda e: e.activation(sg[:], cT[:], AF.Sigmoid), reads=[cT], writes=[sg])
            p.op('dve', lambda e: e.tensor_mul(cT[:], cT[:], sg[:]), reads=[cT, sg], writes=[cT])
            wm = [p.sb(ph, [128, 8, 512], F32) for _ in range(2)]
            bm = [p.sb(ph, [2, 512], F32) for _ in range(2)]
            mo = [p.sb(ph, [2, 512], F32) for _ in range(2)]
            pm = [p.ps(ph, [2, 512]) for _ in range(2)]
            it = 0
            for l in range(2):
                for nb in range(12):
                    w = wm[it % 2]; bb = bm[it % 2]; o = mo[it % 2]; ps = pm[it % 2]
                    p.dma('sp', w[:], w_mod[l, :, nb * 512:(nb + 1) * 512].rearrange("(k p) n -> p k n", p=128), writes=[w])
                    p.dma('sp', bb[:], b_mod[l:l + 1, nb * 512:(nb + 1) * 512].to_broadcast([2, 512]), writes=[bb])

                    def mm(e, w=w, ps=ps):
                        for kc in range(8):
                            r = e.matmul(ps[:], cT[:, kc, :], w[:, kc, :], start=(kc == 0), stop=(kc == 7))
                        return r
                    p.op('pe', mm, reads=[cT, w], writes=[ps])
                    p.op('dve', lambda e, o=o, ps=ps, bb=bb: e.tensor_add(o[:], ps[:], bb[:]), reads=[ps, bb], writes=[o])
                    p.dma('sp', MOD[l, :, nb * 512:(nb + 1) * 512], o[:], reads=[o], writes=[p.uk()])
                    it += 1
            p.barrier()

        for layer in range(2):
          with ExitStack() as lay:
            keep_ctx = layer == 0
            T0 = 0 if keep_ctx else LT
            with ExitStack() as ph:
                winb = p.sb(ph, [128, 8, DIN], BF16)
                for kc in range(8):
                    p.dma('pool', winb[:, kc, :], w_in[layer, kc * 128:(kc + 1) * 128, :], writes=[(winb.name, kc)])
                G1 = [p.sb(ph, [128, D], F32) for _ in range(2)]
                B1 = [p.sb(ph, [128, D], F32) for _ in range(2)]
                g1 = p.sb(ph, [128, D], F32)
                p.dma('sp', g1[:], norm1_g[layer:layer + 1, :].to_broadcast([128, D]), writes=[g1])
                for s in range(2):
                    p.dma('sp', B1[s][:], MOD[layer, s:s + 1, 0:D].to_broadcast([128, D]), reads=['MOD'], writes=[B1[s]])
                    p.dma('sp', G1[s][:], MOD[layer, s:s + 1, D:2 * D].to_broadcast([128, D]), reads=['MOD'], writes=[G1[s]])
                    p.op('dve', lambda e, s=s: e.scalar_tensor_tensor(G1[s][:], G1[s][:], 1.0, g1[:], ALU.add, ALU.mult),
                         reads=[G1[s], g1], writes=[G1[s]])
                LB = p.sb(ph, [128, 512], F32)
                OMLB = p.sb(ph, [128, 512], F32)
                if layer == 0:
                    p.op('dve', lambda e: e.memset(LB[:], 0.0), writes=[LB])
                    p.op('dve', lambda e: e.memset(OMLB[:], 1.0), writes=[OMLB])
                else:
                    l0 = p.sb(ph, [128, 512], F32)
                    p.dma('sp', l0[:], lb_logits[0:1, :].to_broadcast([128, 512]), writes=[l0])
                    p.dma('sp', LB[:], lb_logits[1:2, :].to_broadcast([128, 512]), writes=[LB])
                    p.op('dve', lambda e: e.tensor_sub(LB[:], LB[:], l0[:]), reads=[LB, l0], writes=[LB])
                    p.op('act', lambda e: e.activation(LB[:], LB[:], AF.Sigmoid), reads=[LB], writes=[LB])
                    p.op('dve', lambda e: e.tensor_scalar(OMLB[:], LB[:], -1.0, 1.0, ALU.mult, ALU.add), reads=[LB], writes=[OMLB])
                wgbd = p.sb(ph, [32, 256], F32)
                bgc = p.sb(ph, [128, 256], F32)
                p.dma('sp', wgbd[:], wg_bd[layer], writes=[wgbd])
                p.dma('sp', bgc[:], bg_cat[layer:layer + 1, :].to_broadcast([128, 256]), writes=[bgc])
                wuq = p.sb(ph, [128, 2, 384], F32)
                wuqb = p.sb(ph, [128, 2, 384], BF16)
                qg = p.sb(ph, [128, 2], F32)
                wukv = p.sb(ph, [128, 512], F32)
                wukvb = p.sb(ph, [128, 512], BF16)
                kvg = p.sb(ph, [128, 1], F32)
                p.dma('sp', wuq[:, 0, :], mla_wuq[layer, 0:128, :], writes=[wuq])
                p.dma('sp', wuq[0:64, 1, :], mla_wuq[layer, 128:192, :], writes=[wuq])
                p.dma('sp', qg[:, 0:1], mla_qg[layer, 0:128, :], writes=[qg])
                p.dma('sp', qg[0:64, 1:2], mla_qg[layer, 128:192, :], writes=[qg])
                p.dma('sp', wukv[:], mla_wukv[layer], writes=[wukv])
                p.dma('sp', kvg[:], mla_kvg[layer], writes=[kvg])
                p.op('dve', lambda e: e.tensor_scalar(wuqb[:, 0, :], wuq[:, 0, :], qg[:, 0:1], None, ALU.mult), reads=[wuq, qg], writes=[wuqb])
                p.op('dve', lambda e: e.tensor_scalar(wuqb[0:64, 1, :], wuq[0:64, 1, :], qg[0:64, 1:2], None, ALU.mult), reads=[wuq, qg], writes=[wuqb])
                p.op('dve', lambda e: e.tensor_scalar(wukvb[:], wukv[:], kvg[:, 0:1], None, ALU.mult), reads=[wukv, kvg], writes=[wukvb])
                uw = {}; v2 = {}
                for d in 'fb':
                    uw[d] = p.sb(ph, [128, 256], F32)
                    v2[d] = p.sb(ph, [128, 128], F32)
                    p.dma('sp', uw[d][:], uw_d[d], writes=[uw[d]])
                    p.dma('sp', v2[d][:], v2_d[d], writes=[v2[d]])
                xt = [p.sb(ph, [128, D], F32) for _ in range(2)]
                junk = p.sb(ph, [128, D], F32)
                st = [p.sb(ph, [128, 8], F32) for _ in range(2)]
                hb = p.sb(ph, [128, D], BF16)
                hT = p.sb(ph, [128, 8, 128], BF16)
                pTs = [p.sb(ph, [128, DIN], F32) for _ in range(2)]
                junk2 = p.sb(ph, [128, 192], F32)
                ps_t = p.ps(ph, [128, 8, 128], BF16)
                ps_t2 = p.ps(ph, [128, 8, 128], BF16)
                ps_p = [p.ps(ph, [128, 512]) for _ in range(2)]
                ps_a = p.ps(ph, [128, 2, 256])
                ps_b = p.ps(ph, [128, 2, 256])
                ps_c = p.ps(ph, [128, 512])
                ps_d = p.ps(ph, [128, 512])
                qA = p.sb(ph, [128, 256], F32)
                kk = p.sb(ph, [128, 256], F32)
                la = p.sb(ph, [128, 256], F32)
                qTs = p.sb(ph, [128, 2, 128], F32)
                kTs = p.sb(ph, [128, 2, 128], F32)
                x2 = p.sb(ph, [128, 2, 128], F32)
                x1 = p.sb(ph, [128, 2, 128], F32)
                x1n = p.sb(ph, [128, 2, 128], F32)
                x3 = p.sb(ph, [128, 256], F32)
                o_qh = p.sb(ph, [128, 2, 128], BF16)
                o_qt = p.sb(ph, [128, 2, 128], BF16)
                o_kt = p.sb(ph, [128, 2, 128], BF16)
                o_kh = p.sb(ph, [128, 256], BF16)
                gs = p.sb(ph, [128, 256], F32)
                qpad = p.sb(ph, [128, 4, 64], F32)
                kpad = p.sb(ph, [128, 4, 64], F32)
                lapad = p.sb(ph, [128, 4, 64], F32)
                zT = p.sb(ph, [32, 128], F32)
                lac = p.sb(ph, [128, 256], F32)
                rin = p.sb(ph, [128, 5, 32], F32)
                rout = p.sb(ph, [128, 5, 32], F32)
                rtmp5 = p.sb(ph, [128, 5, 32], F32)
                rtmp = p.sb(ph, [128, 4, 32], F32)
                bq = p.sb(ph, [128, 256], BF16)
                bqT = p.sb(ph, [128, 2, 128], BF16)
                bkT = p.sb(ph, [128, 2, 128], BF16)
                bv = p.sb(ph, [128, 256], BF16)
                ropets = [p.sb(ph, [128, 32], F32) for _ in range(2)]
                cqn = p.sb(ph, [128, 192], BF16)
                ckvn = p.sb(ph, [128, 128], BF16)
                cqT = p.sb(ph, [128, 2, 128], BF16)
                ckvT = p.sb(ph, [128, 128], BF16)
                qd = p.sb(ph, [128, 4, 96], F32)
                qd2 = p.sb(ph, [128, 4, 96], F32)
                qdb = p.sb(ph, [128, 4, 96], BF16)
                kvd = p.sb(ph, [128, 4, 128], F32)
                kdb = p.sb(ph, [128, 4, 96], BF16)
                kr = p.sb(ph, [128, 32], F32)
                kr2 = p.sb(ph, [128, 32], F32)
                vdb = p.sb(ph, [128, 256], BF16)
                dT = p.sb(ph, [96, 4, 128], BF16)
                p.op('dve', lambda e: e.memset(qpad[:], 0.0), writes=[qpad])
                p.op('dve', lambda e: e.memset(kpad[:], 0.0), writes=[kpad])
                p.op('dve', lambda e: e.memset(lapad[:], 0.0), writes=[lapad])

                def gla_prep(m, tt, d, q_ap, k_ap, la_ap, rq, rk, rl, first):
                    tok = slice(tt * 128, (tt + 1) * 128)
                    if first:
                        for ct in range(2):
                            transpose_f(ps_a[:, ct, 0:128], q_ap[:, ct * 128:(ct + 1) * 128], rq, [ps_a])
                        p.op('act', lambda e: e.copy(qTs[:], ps_a[:, :, 0:128]), reads=[ps_a], writes=[qTs])
                    for ct in range(2):
                        transpose_f(ps_a[:, ct, 128:256], k_ap[:, ct * 128:(ct + 1) * 128], rk, [ps_a])
                    p.op('act', lambda e: e.copy(kTs[:], ps_a[:, :, 128:256]), reads=[ps_a], writes=[kTs])
                    for ct in range(2):
                        p.op('pe', lambda e, ct=ct: e.matmul(ps_b[:, ct, :], la_ap[:, ct * 128:(ct + 1) * 128], uw[d][:], start=True, stop=True),
                             reads=list(rl) + [uw[d]], writes=[ps_b])
                    p.op('pe', lambda e: e.matmul(ps_c[:, 0:256], v2[d][:], la_ap, start=True, stop=True), reads=list(rl) + [v2[d]], writes=[ps_c])
                    p.op('act', lambda e: e.activation(x2[:], ps_b[:, :, 0:128], AF.Exp), reads=[ps_b], writes=[x2])
                    p.op('act', lambda e: e.activation(x1[:], ps_b[:, :, 128:256], AF.Exp), reads=[ps_b], writes=[x1])
                    p.op('act', lambda e: e.activation(x1n[:], ps_b[:, :, 128:256], AF.Exp, scale=-1.0), reads=[ps_b], writes=[x1n])
                    p.op('act', lambda e: e.activation(x3[:], ps_c[:, 0:256], AF.Exp), reads=[ps_c], writes=[x3])
                    p.op('dve', lambda e: e.tensor_mul(o_qh[:], qTs[:], x2[:]), reads=[qTs, x2], writes=[o_qh])
                    p.op('dve', lambda e: e.scalar_tensor_tensor(o_qt[:], qTs[:], SCL, x1[:], ALU.mult, ALU.mult), reads=[qTs, x1], writes=[o_qt])
                    p.op('dve', lambda e: e.scalar_tensor_tensor(o_kt[:], kTs[:], SCL, x1n[:], ALU.mult, ALU.mult), reads=[kTs, x1n], writes=[o_kt])
                    p.op('dve', lambda e: e.tensor_mul(o_kh[:], k_ap, x3[:]), reads=list(rk) + [x3], writes=[o_kh])
                    md = m + d
                    p.dma('sp', mix[md + 'QH'][:, tok].rearrange("(c p) t -> p c t", p=128), o_qh[:], reads=[o_qh], writes=[p.uk()])
                    p.dma('sp', mix[md + 'QT'][:, tok].rearrange("(c p) t -> p c t", p=128), o_qt[:], reads=[o_qt], writes=[p.uk()])
                    p.dma('sp', mix[md + 'KT'][:, tok].rearrange("(c p) t -> p c t", p=128), o_kt[:], reads=[o_kt], writes=[p.uk()])
                    p.dma('sp', mix[md + 'KH'][tok, :], o_kh[:], reads=[o_kh], writes=[p.uk()])
                    cols = (63, 127) if d == 'f' else (0, 64)
                    for cc in range(2):
                        p.dma('sp', mix[md + 'DEC'][:, 2 * tt + cc:2 * tt + cc + 1].rearrange("(c p) t -> p c t", p=128),
                              x2[:, :, cols[cc]:cols[cc] + 1], reads=[x2], writes=[p.uk()], allow_slow_non_contiguous=True)

                ncb = (DIN + 511) // 512

                def front(tt):
                    s = 1 if tt < LT else 0
                    tok = slice(tt * 128, (tt + 1) * 128)
                    xb = xt[tt % 2]; sx = st[tt % 2]; pT = pTs[tt % 2]; ropet = ropets[tt % 2]
                    p.dma('sp', xb[:], X[tok, :], reads=[('X', tt)], writes=[xb])
                    p.dma('sp', ropet[:], rope_d[tok, :], writes=[ropet])
                    p.op('act', lambda e: e.activation(junk[:], xb[:], AF.Square, accum_out=sx[:, 0:1]), reads=[xb], writes=[junk, sx])
                    p.op('dve', lambda e: e.tensor_scalar(sx[:, 1:2], sx[:, 0:1], 1.0 / D, EPS, ALU.mult, ALU.add), reads=[sx], writes=[sx])
                    rsqrt(sx[:, 1:2], [sx])
                    p.op('dve', lambda e: e.scalar_tensor_tensor(junk[:], xb[:], sx[:, 1:2], G1[s][:], ALU.mult, ALU.mult), reads=[xb, sx, G1[s]], writes=[junk])
                    p.op('dve', lambda e: e.tensor_add(hb[:], junk[:], B1[s][:]), reads=[junk, B1[s]], writes=[hb])
                    for kc in range(8):
                        transpose_b(ps_t2[:, kc, :], hb[:, kc * 128:(kc + 1) * 128], [hb], [ps_t2])
                    p.op('act', lambda e: e.copy(hT[:], ps_t2[:]), reads=[ps_t2], writes=[hT])
                    for cb in range(ncb):
                        c0 = cb * 512; c1 = min(DIN, c0 + 512)
                        pp = ps_p[cb % 2]

                        def mm(e, pp=pp, c0=c0, c1=c1):
                            for kc in range(8):
                                r = e.matmul(pp[:, 0:c1 - c0], hT[:, kc, :], winb[:, kc, c0:c1], start=(kc == 0), stop=(kc == 7))
                            return r
                        p.op('pe', mm, reads=[hT] + [(winb.name, kc) for kc in range(8)], writes=[pp])
                        eng = 'act' if cb % 2 == 0 else 'dve'
                        if eng == 'act':
                            p.op('act', lambda e, pp=pp, c0=c0, c1=c1: e.copy(pT[:, c0:c1], pp[:, 0:c1 - c0]), reads=[pp], writes=[(pT.name, cb)])
                        else:
                            p.op('dve', lambda e, pp=pp, c0=c0, c1=c1: e.tensor_copy(pT[:, c0:c1], pp[:, 0:c1 - c0]), reads=[pp], writes=[(pT.name, cb)])

                def back(tt):
                    s = 1 if tt < LT else 0
                    tok = slice(tt * 128, (tt + 1) * 128)
                    sx = st[tt % 2]; pT = pTs[tt % 2]; ropet = ropets[tt % 2]
                    RP = [(pT.name, cb) for cb in range(ncb)]
                    def chain_x():
                        p.op('act', lambda e: e.activation(qA[:], pT[:, 0:256], AF.Silu), reads=RP, writes=[qA])
                        p.op('dve', lambda e: e.tensor_scalar(qA[:], qA[:], 0.125, None, ALU.mult), reads=[qA], writes=[qA])
                        p.dma('pool', mix['AV'][tok, :], pT[:, 256:512], reads=RP, writes=[p.uk()])
                        p.op('act', lambda e: e.activation(gs[:], pT[:, 1024:1280], AF.Silu), reads=RP, writes=[gs])
                        p.dma('sp', mix['AG'][tok, :], gs[:], reads=[gs], writes=[p.uk()])
                        for di, d in enumerate('fb'):
                            zc = slice(512 + 256 * di, 768 + 256 * di)
                            lc = slice(256 * di, 256 * di + 256)
                            p.op('act', lambda e: e.activation(la[:], pT[:, zc], AF.Sigmoid), reads=RP, writes=[la])
                            p.op('dve', lambda e: e.tensor_mul(la[:], la[:], OMLB[:, lc]), reads=[la, OMLB], writes=[la])
                            p.op('dve', lambda e: e.tensor_add(la[:], la[:], LB[:, lc]), reads=[la, LB], writes=[la])
                            p.op('dve', lambda e: e.tensor_scalar(kk[:], la[:], -1.0, 1.0, ALU.mult, ALU.add), reads=[la], writes=[kk])
                            p.op('act', lambda e: e.activation(la[:], la[:], AF.Ln), reads=[la], writes=[la])
                            gla_prep('A', tt, d, qA[:], kk[:], la[:], [qA], [kk], [la], di == 0)
                        p.op('dve', lambda e: e.tensor_scalar(qpad[:, :, 0:32], pT[:, 2048:2176].rearrange("p (h k) -> p h k", h=4), 32.0 ** -0.5, None, ALU.mult),
                             reads=RP, writes=[qpad])
                        p.op('dve', lambda e: e.tensor_copy(kpad[:, :, 0:32], pT[:, 2176:2304].rearrange("p (h k) -> p h k", h=4)), reads=RP, writes=[kpad])
                        p.dma('pool', mix['CV'][tok, :], pT[:, 2304:2560], reads=RP, writes=[p.uk()])
                        p.op('act', lambda e: e.activation(gs[:], pT[:, 2560:2816], AF.Silu), reads=RP, writes=[gs])
                        p.dma('sp', mix['CG'][tok, :], gs[:], reads=[gs], writes=[p.uk()])
                        transpose_f(ps_c[0:32, 256:384], pT[:, 2816:2848], RP, [ps_c])
                        p.op('act', lambda e: e.copy(zT[:], ps_c[0:32, 256:384]), reads=[ps_c], writes=[zT])
                        p.op('pe', lambda e: e.matmul(ps_c[:, 0:256], zT[:], wgbd[:], start=True, stop=True), reads=[zT, wgbd], writes=[ps_c])
                        p.op('dve', lambda e: e.tensor_add(lac[:], ps_c[:, 0:256], bgc[:]), reads=[ps_c, bgc], writes=[lac])
                        p.op('act', lambda e: e.activation(lac[:], lac[:], AF.Sigmoid), reads=[lac], writes=[lac])
                        p.op('act', lambda e: e.activation(lac[:], lac[:], AF.Ln), reads=[lac], writes=[lac])
                        for di, d in enumerate('fb'):
                            p.op('dve', lambda e: e.tensor_scalar(lapad[:, :, 0:32], lac[:, 128 * di:128 * di + 128].rearrange("p (h k) -> p h k", h=4), 1.0 / 16.0, None, ALU.mult),
                                 reads=[lac], writes=[lapad])
                            gla_prep('C', tt, d, qpad[:].rearrange("p h k -> p (h k)"), kpad[:].rearrange("p h k -> p (h k)"),
                                     lapad[:].rearrange("p h k -> p (h k)"), [qpad], [kpad], [lapad], di == 0)

                    def chain_y():
                        p.op('dve', lambda e: e.tensor_scalar(bq[:], pT[:, 1280:1536], 0.125, None, ALU.mult), reads=RP, writes=[bq])
                        for ct in range(2):
                            transpose_b(ps_t[:, ct, :], bq[:, ct * 128:(ct + 1) * 128], [bq], [ps_t])
                        p.op('act', lambda e: e.copy(bqT[:], ps_t[:, 0:2, :]), reads=[ps_t], writes=[bqT])
                        p.dma('sp', BQT[:, tok].rearrange("(c p) t -> p c t", p=128), bqT[:], reads=[bqT], writes=[p.uk()])
                        p.op('dve', lambda e: e.tensor_copy(bq[:], pT[:, 1536:1792]), reads=RP + [bqT], writes=[bq])
                        for ct in range(2):
                            transpose_b(ps_t[:, 2 + ct, :], bq[:, ct * 128:(ct + 1) * 128], [bq], [ps_t])
                        p.op('act', lambda e: e.copy(bkT[:], ps_t[:, 2:4, :]), reads=[ps_t], writes=[bkT])
                        p.dma('sp', BKT[:, tok].rearrange("(c p) t -> p c t", p=128), bkT[:], reads=[bkT], writes=[p.uk()])
                        p.op('dve', lambda e: e.tensor_copy(bv[:], pT[:, 1792:2048]), reads=RP, writes=[bv])
                        p.dma('sp', BV[tok, :], bv[:], reads=[bv], writes=[p.uk()])
                        p.op('act', lambda e: e.activation(junk2[:, 0:192], pT[:, 2848:3040], AF.Square, accum_out=sx[:, 2:3]), reads=RP, writes=[junk2, sx])
                        p.op('act', lambda e: e.activation(junk2[:, 0:128], pT[:, 3040:3168], AF.Square, accum_out=sx[:, 3:4]), reads=RP, writes=[junk2, sx])
                        p.op('dve', lambda e: e.tensor_scalar(sx[:, 4:5], sx[:, 2:3], 1.0 / 192, EPS, ALU.mult, ALU.add), reads=[sx], writes=[sx])
                        rsqrt(sx[:, 4:5], [sx])
                        p.op('dve', lambda e: e.tensor_scalar(sx[:, 5:6], sx[:, 3:4], 1.0 / 128, EPS, ALU.mult, ALU.add), reads=[sx], writes=[sx])
                        rsqrt(sx[:, 5:6], [sx])
                        p.op('dve', lambda e: e.tensor_scalar(cqn[:], pT[:, 2848:3040], sx[:, 4:5], None, ALU.mult), reads=RP + [sx], writes=[cqn])
                        p.op('dve', lambda e: e.tensor_scalar(ckvn[:], pT[:, 3040:3168], sx[:, 5:6], None, ALU.mult), reads=RP + [sx], writes=[ckvn])
                        transpose_b(ps_t[:, 4, :], cqn[:, 0:128], [cqn], [ps_t])
                        transpose_b(ps_t[0:64, 5, :], cqn[:, 128:192], [cqn], [ps_t])
                        transpose_b(ps_t[:, 6, :], ckvn[:], [ckvn], [ps_t])
                        p.op('act', lambda e: e.copy(cqT[:, 0, :], ps_t[:, 4, :]), reads=[ps_t], writes=[cqT])
                        p.op('act', lambda e: e.copy(cqT[0:64, 1, :], ps_t[0:64, 5, :]), reads=[ps_t], writes=[cqT])
                        p.op('act', lambda e: e.copy(ckvT[:], ps_t[:, 6, :]), reads=[ps_t], writes=[ckvT])

                        def mmq(e):
                            e.matmul(ps_d[:, 0:384], cqT[:, 0, :], wuqb[:, 0, :], start=True, stop=False)
                            return e.matmul(ps_d[:, 0:384], cqT[0:64, 1, :], wuqb[0:64, 1, :], start=False, stop=True)
                        p.op('pe', mmq, reads=[cqT, wuqb], writes=[ps_d])
                        p.op('act', lambda e: e.activation(qd[:].rearrange("p h k -> p (h k)"), ps_d[:, 0:384], AF.Copy, scale=96.0 ** -0.5), reads=[ps_d], writes=[qd])
                        p.op('pe', lambda e: e.matmul(ps_d[:, 0:512], ckvT[:], wukvb[:], start=True, stop=True), reads=[ckvT, wukvb], writes=[ps_d])
                        p.op('act', lambda e: e.copy(kvd[:].rearrange("p h k -> p (h k)"), ps_d[:, 0:512]), reads=[ps_d], writes=[kvd])
                        cosb = ropet[:, 0:16].rearrange("p (a f) -> p a f", a=2)
                        sinb = ropet[:, 16:32].rearrange("p (a f) -> p a f", a=2)

                        p.op('dve', lambda e: e.tensor_copy(rin[:, 0:4, :], qd[:, :, 64:96]), reads=[qd], writes=[rin])
                        p.op('dve', lambda e: e.tensor_copy(rin[:, 4, :], pT[:, 3168:3200]), reads=RP + [rin], writes=[rin])
                        s5 = rin[:].rearrange("p h (a x f) -> p h a x f", a=2, x=2)
                        d5 = rout[:].rearrange("p h (a x f) -> p h a x f", a=2, x=2)
                        t5 = rtmp5[:].rearrange("p h (a x f) -> p h a x f", a=2, x=2)
                        u1 = s5[:, :, :, 0, :]; u2 = s5[:, :, :, 1, :]
                        cos5 = cosb.unsqueeze(1).to_broadcast([128, 5, 2, 8])
                        sin5 = sinb.unsqueeze(1).to_broadcast([128, 5, 2, 8])
                        p.op('dve', lambda e: e.tensor_mul(d5[:, :, :, 0, :], u1, cos5), reads=[rin, ropet], writes=[rout])
                        p.op('dve', lambda e: e.tensor_mul(t5[:, :, :, 0, :], u2, sin5), reads=[rin, ropet], writes=[rtmp5])
                        p.op('dve', lambda e: e.tensor_mul(d5[:, :, :, 1, :], u1, sin5), reads=[rin, ropet, rout], writes=[rout])
                        p.op('dve', lambda e: e.tensor_mul(t5[:, :, :, 1, :], u2, cos5), reads=[rin, ropet, rtmp5], writes=[rtmp5])
                        p.op('dve', lambda e: e.tensor_sub(d5[:, :, :, 0, :], d5[:, :, :, 0, :], t5[:, :, :, 0, :]), reads=[rout, rtmp5], writes=[rout])
                        p.op('dve', lambda e: e.tensor_add(d5[:, :, :, 1, :], d5[:, :, :, 1, :], t5[:, :, :, 1, :]), reads=[rout, rtmp5], writes=[rout])
                        p.op('dve', lambda e: e.tensor_copy(qdb[:, :, 0:64], qd[:, :, 0:64]), reads=[qd], writes=[qdb])
                        p.op('dve', lambda e: e.tensor_copy(qdb[:, :, 64:96], rout[:, 0:4, :]), reads=[rout, qdb], writes=[qdb])
                        p.op('dve', lambda e: e.tensor_copy(kdb[:, :, 0:64], kvd[:, :, 0:64]), reads=[kvd], writes=[kdb])
                        p.op('dve', lambda e: e.tensor_copy(kdb[:, :, 64:96], rout[:, 4:5, :].to_broadcast([128, 4, 32])), reads=[rout, kdb], writes=[kdb])
                        p.op('dve', lambda e: e.tensor_copy(vdb[:].rearrange("p (h k) -> p h k", h=4), kvd[:, :, 64:128]), reads=[kvd], writes=[vdb])
                        p.dma('sp', DV[tok, :], vdb[:], reads=[vdb], writes=[p.uk()])
                        for h in range(4):
                            transpose_b(ps_t[0:96, h, :], qdb[:, h, :], [qdb], [ps_t])
                        p.op('act', lambda e: e.copy(dT[:], ps_t[0:96, 0:4, :]), reads=[ps_t], writes=[dT])
                        p.dma('sp', DQT[:, :, tok].rearrange("h k t -> k h t"), dT[:], reads=[dT], writes=[p.uk()])
                        for h in range(4):
                            transpose_b(ps_t[0:96, 4 + h, :], kdb[:, h, :], [kdb], [ps_t])
                        p.op('act', lambda e: e.copy(dT[:], ps_t[0:96, 4:8, :]), reads=[ps_t], writes=[dT])
                        p.dma('sp', DKT[:, :, tok].rearrange("h k t -> k h t"), dT[:], reads=[dT], writes=[p.uk()])

                    if tt + 1 < NT:
                        p.interleave([lambda: front(tt + 1), chain_x, chain_y])
                    else:
                        p.interleave([chain_x, chain_y])

                front(0)
                for tt in range(NT):
                    back(tt)
                p.barrier()
            if dbg == 'P':
                break

            def normalize(ph_bufs, OT, LTp, n, dst_ap, dst_key):
                rl, on = ph_bufs
                p.op('dve', lambda e: e.reciprocal(rl[:, 0:n], LTp[:, 0:n]), reads=[LTp], writes=[rl])
                p.op('dve', lambda e: e.tensor_mul(on[:, 0:n], OT[:, 0:n], rl[:, 0:n]), reads=[OT, rl], writes=[on])
                p.dma('sp', dst_ap, on[:, 0:n], reads=[on], writes=[p.uk()])

            with ExitStack() as ph:
                GC = 4
                NG = NCH // GC
                LG = (L // 64) // GC
                masks = {}
                for d in 'fb':
                    masks[d] = p.sb(ph, [64, 64], F32)
                    p.dma('sp', masks[d][:], mask_d[d], writes=[masks[d]])
                T2 = [p.ps(ph, [64, 512]) for _ in range(2)]
                streams = []
                for si, (m, d) in enumerate((('A', 'f'), ('A', 'b'), ('C', 'f'), ('C', 'b'))):
                    st_ = dict(m=m, d=d, md=m + d)
                    for nm in ('qt', 'kt', 'qh', 'kh', 'v'):
                        st_[nm] = [p.sb(ph, [64, 4, 256], BF16) for _ in range(2)]
                    st_['dec'] = [p.sb(ph, [64, 4, 4], F32) for _ in range(2)]
                    st_['ob'] = [p.sb(ph, [64, 4, 256], F32) for _ in range(2)]
                    st_['at'] = p.sb(ph, [64, 256], BF16)
                    st_['S'] = p.sb(ph, [64, 4, 64], F32)
                    st_['Sb'] = p.sb(ph, [64, 4, 64], BF16)
                    st_['T1'] = p.ps(ph, [64, 512])
                    st_['pS'] = T2[si // 2][:, (si % 2) * 256:(si % 2) * 256 + 256]
                    st_['pSk'] = T2[si // 2]
                    if d == 'f':
                        st_['gorder'] = list(range(NG))
                    else:
                        st_['gorder'] = list(range(LG - 1, -1, -1)) + list(range(NG - 1, LG - 1, -1))
                    p.op('dve', lambda e: e.memset(st_['S'][:], 0.0), writes=[st_['S']])
                    p.op('dve', lambda e: e.memset(st_['Sb'][:], 0.0), writes=[st_['Sb']])
                    streams.append(st_)
                for gi in range(NG):
                    b2 = gi % 2
                    for st_ in streams:
                        m, d, md = st_['m'], st_['d'], st_['md']
                        g = st_['gorder'][gi]
                        tk = slice(g * 256, (g + 1) * 256)
                        p.dma('sp', st_['qt'][b2][:], mix[md + 'QT'][:, tk].rearrange("(h k) t -> k h t", k=64), reads=[md + 'QT'], writes=[st_['qt'][b2]])
                        p.dma('sp', st_['kt'][b2][:], mix[md + 'KT'][:, tk].rearrange("(h k) t -> k h t", k=64), reads=[md + 'KT'], writes=[st_['kt'][b2]])
                        p.dma('sp', st_['qh'][b2][:], mix[md + 'QH'][:, tk].rearrange("(h k) t -> k h t", k=64), reads=[md + 'QH'], writes=[st_['qh'][b2]])
                        p.dma('sp', st_['kh'][b2][:], mix[md + 'KH'][tk, :].rearrange("(c p) n -> p c n", p=64), reads=[md + 'KH'], writes=[st_['kh'][b2]])
                        p.dma('sp', st_['v'][b2][:], mix[m + 'V'][tk, :].rearrange("(c p) n -> p c n", p=64), reads=[m + 'V'], writes=[st_['v'][b2]])
                        p.dma('sp', st_['dec'][b2][:], mix[md + 'DEC'][:, g * 4:(g + 1) * 4].rearrange("(h k) t -> k h t", k=64), reads=[md + 'DEC'], writes=[st_['dec'][b2]])
                    for ci in range(GC):
                        for st_ in streams:
                            d = st_['d']
                            c = ci if d == 'f' else GC - 1 - ci
                            cs = slice(c * 64, (c + 1) * 64)
                            qt, kt, qh, kh, v, dec, ob = (st_[n][b2] for n in ('qt', 'kt', 'qh', 'kh', 'v', 'dec', 'ob'))
                            at, S, Sb, T1, pS, pSk = st_['at'], st_['S'], st_['Sb'], st_['T1'], st_['pS'], st_['pSk']
                            pa = T1[:, 0:256]; po = T1[:, 256:512]
                            ka = T1; ko = T1

                            def mmA(e):
                                for h in range(4):
                                    r = e.matmul(pa[:, h * 64:(h + 1) * 64], kt[:, h, cs], qt[:, h, cs], start=True, stop=True)
                                return r
                            p.op('pe', mmA, reads=[kt, qt], writes=[ka])
                            p.op('dve', lambda e: e.tensor_scalar(at[:], pa, 1e30, -1e30, ALU.min, ALU.max), reads=[ka], writes=[at])
                            p.op('dve', lambda e: e.tensor_mul(at[:].rearrange("p (h i) -> p h i", h=4), at[:].rearrange("p (h i) -> p h i", h=4),
                                                              masks[d][:].unsqueeze(1).to_broadcast([64, 4, 64])), reads=[at, masks[d]], writes=[at])

                            def mmO(e):
                                for h in range(4):
                                    e.matmul(po[:, h * 64:(h + 1) * 64], at[:, h * 64:(h + 1) * 64], v[:, c, h * 64:(h + 1) * 64], start=True, stop=False)
                                    r = e.matmul(po[:, h * 64:(h + 1) * 64], qh[:, h, cs], Sb[:, h, :], start=False, stop=True)
                                return r
                            p.op('pe', mmO, reads=[at, v, qh, Sb], writes=[ko])
                            p.op('act', lambda e: e.copy(ob[:, c, :], po), reads=[ko], writes=[ob])

                            def mmS(e):
                                for h in range(4):
                                    r = e.matmul(pS[:, h * 64:(h + 1) * 64], kh[:, c, h * 64:(h + 1) * 64], v[:, c, h * 64:(h + 1) * 64], start=True, stop=True)
                                return r
                            p.op('pe', mmS, reads=[kh, v], writes=[pSk])
                            p.op('dve', lambda e: e.tensor_mul(S[:], S[:], dec[:, :, c:c + 1].to_broadcast([64, 4, 64])), reads=[S, dec], writes=[S])
                            p.op('dve', lambda e: e.tensor_add(S[:], S[:], pS.rearrange("p (h v) -> p h v", h=4)), reads=[S, pSk], writes=[S])
                            p.op('act', lambda e: e.copy(Sb[:], S[:]), reads=[S], writes=[Sb])
                    for st_ in streams:
                        g = st_['gorder'][gi]
                        tk = slice(g * 256, (g + 1) * 256)
                        p.dma('sp', mix[st_['md'] + 'O'][tk, :].rearrange("(c p) n -> p c n", p=64), st_['ob'][b2][:], reads=[st_['ob'][b2]], writes=[p.uk()])
                p.barrier()
            if dbg == 'R':
                break
            with ExitStack() as ph:
                ngb = {}
                for m, src in (('A', hgrn_ng), ('C', gla_ng)):
                    ngb[m] = p.sb(ph, [128, 256], F32)
                    p.dma('sp', ngb[m][:], src[layer:layer + 1, :].to_broadcast([128, 256]), writes=[ngb[m]])
                def ro_stream(m, roff):
                    of = [p.sb(ph, [128, 256], F32) for _ in range(2)]
                    ob_ = [p.sb(ph, [128, 256], F32) for _ in range(2)]
                    gg = [p.sb(ph, [128, 256], F32) for _ in range(2)]
                    sq = p.sb(ph, [128, 256], F32)
                    ss = p.sb(ph, [128, 4], F32)
                    yb = p.sb(ph, [128, 256], BF16)
                    yT = p.sb(ph, [128, 2, 128], BF16)
                    pst = p.ps(ph, [128, 2, 128], BF16)

                    def run():
                        it = 0
                        for tt in range(T0, NT):
                            tok = slice(tt * 128, (tt + 1) * 128)
                            a = of[it % 2]; b2 = ob_[it % 2]; g = gg[it % 2]
                            it += 1
                            p.dma('sp', a[:], mix[m + 'fO'][tok, :], reads=[m + 'fO'], writes=[a])
                            p.dma('sp', b2[:], mix[m + 'bO'][tok, :], reads=[m + 'bO'], writes=[b2])
                            p.dma('sp', g[:], mix[m + 'G'][tok, :], reads=[m + 'G'], writes=[g])
                            p.op('dve', lambda e: e.tensor_add(a[:], a[:], b2[:]), reads=[a, b2], writes=[a])
                            p.op('dve', lambda e: e.tensor_mul(sq[:], a[:], a[:]), reads=[a], writes=[sq])
                            p.op('dve', lambda e: e.tensor_reduce(ss[:], sq[:].rearrange("p (h v) -> p h v", h=4), AX.X, ALU.add), reads=[sq], writes=[ss])
                            p.op('dve', lambda e: e.tensor_scalar(ss[:], ss[:], 1.0 / 64, EPS, ALU.mult, ALU.add), reads=[ss], writes=[ss])
                            rsqrt(ss[:], [ss])
                            p.op('dve', lambda e: e.tensor_mul(a[:].rearrange("p (h v) -> p h v", h=4), a[:].rearrange("p (h v) -> p h v", h=4),
                                                              ss[:].unsqueeze(2).to_broadcast([128, 4, 64])), reads=[a, ss], writes=[a])
                            p.op('dve', lambda e: e.tensor_mul(a[:], a[:], ngb[m][:]), reads=[a, ngb[m]], writes=[a])
                            p.op('dve', lambda e: e.tensor_mul(yb[:], a[:], g[:]), reads=[a, g], writes=[yb])

                            def tr(e):
                                for ct in range(2):
                                    r = e.transpose(pst[:, ct, :], yb[:, ct * 128:(ct + 1) * 128], ident_b[:])
                                return r
                            p.op('pe', tr, reads=[yb, ident_b], writes=[pst])
                            p.op('act', lambda e: e.copy(yT[:], pst[:]), reads=[pst], writes=[yT])
                            p.dma('sp', CATT[roff:roff + 256, tok].rearrange("(c p) t -> p c t", p=128), yT[:], reads=[yT], writes=[p.uk()])
                    return run
                p.interleave([ro_stream('A', 0), ro_stream('C', 512)])
                p.barrier()

            with ExitStack() as ph:
                KTn = p.sb(ph, [64, 4, T], BF16)
                QTn = p.sb(ph, [64, 4, T], BF16)
                p.dma('sp', KTn[:], BKT.rearrange("(h d) t -> d h t", d=64), reads=['BKT'], writes=[KTn])
                p.dma('sp', QTn[:], BQT.rearrange("(h d) t -> d h t", d=64), reads=['BQT'], writes=[QTn])
                V1l = p.sb(ph, [64, ROWS, 256], BF16)
                V1c = p.sb(ph, [128, LT, 256], BF16)
                for r8 in range(0, ROWS, 8):
                    p.dma('sp', V1l[:, r8:r8 + 8, :], BV[L + r8 * 64:L + (r8 + 8) * 64, :].rearrange("(r w) c -> w r c", w=64), reads=['BV'], writes=[V1l])
                p.dma('sp', V1c[:], BV[0:L, :].rearrange("(n p) c -> p n c", p=128), reads=['BV'], writes=[V1c])
                EBs = [p.sb(ph, [64, 8, 512], F32) for _ in range(2)]
                psS = [p.ps(ph, [64, 512]) for _ in range(2)]
                psC = [p.ps(ph, [128, 512]) for _ in range(2)]
                OTp = p.ps(ph, [64, 512])
                LTp = p.ps(ph, [64, 512])
                nb_ = (p.sb(ph, [64, 512], F32), p.sb(ph, [64, 512], BF16))
                pe_ = [p.sb(ph, [64, 512], F32) for _ in range(2)]
                pb16 = [p.sb(ph, [64, 512], BF16) for _ in range(2)]
                pc16 = [p.sb(ph, [128, 512], BF16) for _ in range(2)]
                rows_ = [(h, r) for h in range(4) for r in range(ROWS)]

                def na_front(i):
                    h, r = rows_[i]
                    EB = EBs[h % 2]
                    if r == 0:
                        p.dma('sp', EB[:], na_tab[layer, h].rearrange("c w x -> w c x"), writes=[EB])
                        p.op('act', lambda e: e.activation(EB[:], EB[:], AF.Exp), reads=[EB], writes=[EB])
                    rs = min(max(r - 4, 0), ROWS - 8)
                    cls = r - rs
                    qc = slice(L + r * 64, L + (r + 1) * 64)
                    ps = psS[i % 2]; pc = psC[i % 2]; pe = pe_[i % 2]; pbb = pb16[i % 2]; pcs = pc16[i % 2]

                    def mmS(e):
                        for j in range(8):
                            kc = slice(L + (rs + j) * 64, L + (rs + j + 1) * 64)
                            r_ = e.matmul(ps[:, j * 64:(j + 1) * 64], KTn[:, h, kc], QTn[:, h, qc], start=True, stop=True)
                        for kt in range(LT):
                            r_ = e.matmul(pc[:, kt * 64:(kt + 1) * 64], KTn[:, h, kt * 128:(kt + 1) * 128], QTn[:, h, qc], start=True, stop=True)
                        return r_
                    p.op('pe', mmS, reads=[KTn, QTn], writes=[ps, pc])
                    p.op('act', lambda e: e.activation(pe[:], ps[:], AF.Exp), reads=[ps], writes=[pe])
                    p.op('act', lambda e: e.activation(pcs[:, 0:LT * 64], pc[:, 0:LT * 64], AF.Exp), reads=[pc], writes=[pcs])
                    p.op('dve', lambda e: e.tensor_mul(pbb[:], pe[:], EB[:, cls, :]), reads=[pe, EB], writes=[pbb])

                def na_back(i):
                    h, r = rows_[i]
                    hv = slice(h * 64, (h + 1) * 64)
                    rs = min(max(r - 4, 0), ROWS - 8)
                    ri = r % 8
                    r0 = r - ri
                    pbb = pb16[i % 2]; pcs = pc16[i % 2]

                    def mmV(e):
                        for j in range(8):
                            e.matmul(OTp[:, ri * 64:(ri + 1) * 64], V1l[:, rs + j, hv], pbb[:, j * 64:(j + 1) * 64], start=(j == 0), stop=False)
                        for kt in range(LT):
                            e.matmul(OTp[:, ri * 64:(ri + 1) * 64], V1c[:, kt, hv], pcs[:, kt * 64:(kt + 1) * 64], start=False, stop=(kt == LT - 1))
                        for j in range(8):
                            e.matmul(LTp[:, ri * 64:(ri + 1) * 64], ones_b[0:64, 0:64], pbb[:, j * 64:(j + 1) * 64], start=(j == 0), stop=False)
                        for kt in range(LT):
                            r_ = e.matmul(LTp[:, ri * 64:(ri + 1) * 64], ones_b[:, 0:64], pcs[:, kt * 64:(kt + 1) * 64], start=False, stop=(kt == LT - 1))
                        return r_
                    p.op('pe', mmV, reads=[V1l, V1c, pbb, pcs, ones_b], writes=[OTp, LTp])
                    if ri == 7:
                        normalize(nb_, OTp, LTp, 512, CATT[256 + h * 64:256 + (h + 1) * 64, L + r0 * 64:L + r0 * 64 + 512], 'CATT')


                def na_ctx(h):
                    hv = slice(h * 64, (h + 1) * 64)
                    for kt in range(LT):
                        pc = psC[kt % 2]; pcs = pc16[kt % 2]
                        p.op('pe', lambda e: e.matmul(pc[:, 0:L], KTn[:, h, kt * 128:(kt + 1) * 128], QTn[:, h, 0:L], start=True, stop=True),
                             reads=[KTn, QTn], writes=[pc])
                        p.op('act', lambda e: e.activation(pcs[:, 0:L], pc[:, 0:L], AF.Exp), reads=[pc], writes=[pcs])
                        p.op('pe', lambda e: e.matmul(OTp[:, 0:L], V1c[:, kt, hv], pcs[:, 0:L], start=(kt == 0), stop=(kt == LT - 1)),
                             reads=[V1c, pcs], writes=[OTp])
                        p.op('pe', lambda e: e.matmul(LTp[:, 0:L], ones_b[:, 0:64], pcs[:, 0:L], start=(kt == 0), stop=(kt == LT - 1)),
                             reads=[ones_b, pcs], writes=[LTp])
                    normalize(nb_, OTp, LTp, L, CATT[256 + h * 64:256 + (h + 1) * 64, 0:L], 'CATT')

                if keep_ctx:
                    for h in range(4):
                        na_ctx(h)
                na_front(0)
                for i in range(len(rows_)):
                    if i + 1 < len(rows_):
                        na_front(i + 1)
                    na_back(i)
                p.barrier()

            with ExitStack() as ph:
                KTd = p.sb(ph, [96, 4, T], BF16)
                p.dma('sp', KTd[:], DKT.rearrange("h k t -> k h t"), reads=['DKT'], writes=[KTd])
                V1 = p.sb(ph, [128, NT, 256], BF16)
                for n8 in range(0, NT, 8):
                    n9 = min(NT, n8 + 8)
                    p.dma('sp', V1[:, n8:n9, :], DV[n8 * 128:n9 * 128, :].rearrange("(n p) c -> p n c", p=128), reads=['DV'], writes=[V1])
                QTt = [p.sb(ph, [96, 512], BF16) for _ in range(2)]
                psS = [p.ps(ph, [128, 2, 512]) for _ in range(3)]
                OTp = p.ps(ph, [64, 512])
                LTp = p.ps(ph, [64, 512])
                nb_ = (p.sb(ph, [64, 512], F32), p.sb(ph, [64, 512], BF16))
                pe16 = [p.sb(ph, [128, 2, 512], BF16) for _ in range(3)]
                steps = []
                qn = 0
                for h in range(4):
                    qtiles = []
                    if keep_ctx:
                        qtiles.append((0, L, list(range(LT))))
                    for q0 in range(L, T, 512):
                        qtiles.append((q0, min(512, T - q0), list(range(NT))))
                    for (q0, nq, kts) in qtiles:
                        pairs = [kts[i:i + 2] for i in range(0, len(kts), 2)]
                        for ki, kp in enumerate(pairs):
                            steps.append((h, q0, nq, kp, ki == 0, ki == len(pairs) - 1, qn))
                        qn += 1

                def mla_front(i):
                    h, q0, nq, kp, first, last, qi = steps[i]
                    qb = QTt[qi % 2]
                    if first:
                        p.dma('sp', qb[:, 0:nq], DQT[h, :, q0:q0 + nq], reads=['DQT'], writes=[qb])
                    ps = psS[i % 3]; pe = pe16[i % 3]
                    nk = len(kp)

                    def mms(e):
                        for j, kt in enumerate(kp):
                            r = e.matmul(ps[:, j, 0:nq], KTd[:, h, kt * 128:(kt + 1) * 128], qb[:, 0:nq], start=True, stop=True)
                        return r
                    p.op('pe', mms, reads=[KTd, qb], writes=[ps])
                    p.op('act', lambda e: e.activation(pe[:, 0:nk, 0:nq], ps[:, 0:nk, 0:nq], AF.Exp), reads=[ps], writes=[pe])

                def mla_back(i):
                    h, q0, nq, kp, first, last, qi = steps[i]
                    pe = pe16[i % 3]
                    nk = len(kp)

                    def mmv(e):
                        for j, kt in enumerate(kp):
                            e.matmul(OTp[:, 0:nq], V1[:, kt, h * 64:(h + 1) * 64], pe[:, j, 0:nq], start=(first and j == 0), stop=(last and j == nk - 1))
                        for j, kt in enumerate(kp):
                            r = e.matmul(LTp[:, 0:nq], ones_b[:, 0:64], pe[:, j, 0:nq], start=(first and j == 0), stop=(last and j == nk - 1))
                        return r
                    p.op('pe', mmv, reads=[V1, pe, ones_b], writes=[OTp, LTp])
                    if last:
                        normalize(nb_, OTp, LTp, nq, CATT[768 + h * 64:768 + (h + 1) * 64, q0:q0 + nq], 'CATT')

                LOOK = 2
                for i in range(min(LOOK, len(steps))):
                    mla_front(i)
                for i in range(len(steps)):
                    if i + LOOK < len(steps):
                        mla_front(i + LOOK)
                    mla_back(i)
                p.barrier()
            if dbg == 'M':
                break

            W01 = p.sb(lay, [128, NT, 2], F32)
            DESTi = p.sb(lay, [128, NT, 2], I32)
            IDXGU = p.sb(lay, [128, NBLK, 8], I32)
            IDXDN = p.sb(lay, [128, NBLK, 2], I32)
            with ExitStack() as ph:
                woutb = p.sb(ph, [128, 8, D], BF16)
                for kc in range(8):
                    p.dma('pool', woutb[:, kc, :], w_out[layer, kc * 128:(kc + 1) * 128, :], writes=[(woutb.name, kc)])
                RW = [(woutb.name, kc) for kc in range(8)]
                wr = p.sb(ph, [128, 8, 36], F32)
                p.dma('sp', wr[:], w_r[layer].rearrange("(k p) n -> p k n", p=128), writes=[wr])
                brb = p.sb(ph, [128, 36], F32)
                p.dma('sp', brb[:], b_r[layer:layer + 1, :].to_broadcast([128, 36]), writes=[brb])
                g2 = p.sb(ph, [128, D], F32)
                p.dma('sp', g2[:], norm2_g[layer:layer + 1, :].to_broadcast([128, D]), writes=[g2])
                M2 = [p.sb(ph, [128, D], F32) for _ in range(2)]
                G2 = [p.sb(ph, [128, D], F32) for _ in range(2)]
                B2 = [p.sb(ph, [128, D], F32) for _ in range(2)]
                for s in range(2):
                    p.dma('sp', M2[s][:], MOD[layer, s:s + 1, 2 * D:3 * D].to_broadcast([128, D]), writes=[M2[s]])
                    p.dma('sp', B2[s][:], MOD[layer, s:s + 1, 3 * D:4 * D].to_broadcast([128, D]), writes=[B2[s]])
                    p.dma('sp', G2[s][:], MOD[layer, s:s + 1, 4 * D:5 * D].to_broadcast([128, D]), writes=[G2[s]])
                    p.op('dve', lambda e: e.scalar_tensor_tensor(G2[s][:], G2[s][:], 1.0, g2[:], ALU.add, ALU.mult), reads=[G2[s], g2], writes=[G2[s]])
                M0 = p.sb(ph, [128, NT, 32], F32)
                M1 = p.sb(ph, [128, NT, 32], F32)
                Mh = p.sb(ph, [128, NT, 32], BF16)
                p.op('dve', lambda e: e.memset(M0[:], 0.0), writes=[M0])
                p.op('dve', lambda e: e.memset(M1[:], 0.0), writes=[M1])
                p.op('dve', lambda e: e.memset(Mh[:], 0.0), writes=[Mh])
                p.op('dve', lambda e: e.memset(W01[:], 0.0), writes=[W01])
                Bs = []
                for si in range(2):
                    Bs.append(dict(cb=p.sb(ph, [128, 8, 128], BF16), xb=p.sb(ph, [128, D], F32), tmp=p.sb(ph, [128, D], F32), xn=p.sb(ph, [128, D], F32),
                                   h2=p.sb(ph, [128, D], F32), h2b=p.sb(ph, [128, D], BF16), h2T=p.sb(ph, [128, 8, 128], F32), sx=p.sb(ph, [128, 16], F32),
                                   lg=p.sb(ph, [128, 36], F32), r8=p.sb(ph, [128, 4, 8], F32), sel=p.sb(ph, [128, 8], F32), sel2=p.sb(ph, [128, 8], F32),
                                   oh=p.sb(ph, [128, 3, 8], F32), po=p.ps(ph, [128, 512]), pt=p.ps(ph, [128, 4, 128]), pl=p.ps(ph, [128, 64])))
                pl = Bs[0]['pl']

                def o_stream(si):
                    def run():
                        for tt in range(T0 + si, NT, 2):
                            s = 1 if tt < LT else 0
                            tok = slice(tt * 128, (tt + 1) * 128)
                            cb, xb, tmp, xn, h2, h2b, h2T, sx, lg, r8, sel, sel2, oh, po, pt, pl = (Bs[si][k] for k in ('cb', 'xb', 'tmp', 'xn', 'h2', 'h2b', 'h2T', 'sx', 'lg', 'r8', 'sel', 'sel2', 'oh', 'po', 'pt', 'pl'))
                            p.dma('sp', cb[:], CATT[:, tok].rearrange("(c p) t -> p c t", p=128), reads=['CATT'], writes=[cb])
                            p.dma('sp', xb[:], X[tok, :], reads=[('X', tt)], writes=[xb])

                            for nb in range(2):
                                def mm(e):
                                    for c in range(8):
                                        r = e.matmul(po[:], cb[:, c, :], woutb[:, c, nb * 512:(nb + 1) * 512], start=(c == 0), stop=(c == 7))
                                    return r
                                p.op('pe', mm, reads=[cb] + RW, writes=[po])
                                p.op('dve', lambda e: e.tensor_mul(tmp[:, nb * 512:(nb + 1) * 512], po[:], M2[s][:, nb * 512:(nb + 1) * 512]), reads=[po, M2[s]], writes=[tmp])
                            p.op('dve', lambda e: e.tensor_add(xn[:], xb[:], tmp[:]), reads=[xb, tmp], writes=[xn])
                            p.dma('sp', X[tok, :], xn[:], reads=[xn], writes=[('X', tt)])
                            p.op('act', lambda e: e.activation(tmp[:], xn[:], AF.Square, accum_out=sx[:, 0:1]), reads=[xn], writes=[tmp, sx])
                            p.op('dve', lambda e: e.tensor_scalar(sx[:, 1:2], sx[:, 0:1], 1.0 / D, EPS, ALU.mult, ALU.add), reads=[sx], writes=[sx])
                            rsqrt(sx[:, 1:2], [sx])
                            p.op('dve', lambda e: e.scalar_tensor_tensor(tmp[:], xn[:], sx[:, 1:2], G2[s][:], ALU.mult, ALU.mult), reads=[xn, sx, G2[s]], writes=[tmp])
                            p.op('dve', lambda e: e.tensor_add(h2[:], tmp[:], B2[s][:]), reads=[tmp, B2[s]], writes=[h2])
                            p.op('act', lambda e: e.copy(h2b[:], h2[:]), reads=[h2], writes=[h2b])
                            p.dma('sp', H2[tok, :], h2b[:], reads=[h2b], writes=['H2'])
                            for rnd in range(2):
                                def tr(e):
                                    for k4 in range(4):
                                        kc = rnd * 4 + k4
                                        r = e.transpose(pt[:, k4, :], h2[:, kc * 128:(kc + 1) * 128], ident_f[:])
                                    return r
                                p.op('pe', tr, reads=[h2, ident_f], writes=[pt])
                                p.op('act', lambda e: e.copy(h2T[:, rnd * 4:(rnd + 1) * 4, :], pt[:]), reads=[pt], writes=[h2T])

                            def mmr(e):
                                for kc in range(8):
                                    r = e.matmul(pl[:, 0:36], h2T[:, kc, :], wr[:, kc, :], start=(kc == 0), stop=(kc == 7))
                                return r
                            p.op('pe', mmr, reads=[h2T, wr], writes=[pl])
                            p.op('dve', lambda e: e.tensor_add(lg[:], pl[:, 0:36], brb[:]), reads=[pl, brb], writes=[lg])
                            K_ = [sx, lg, r8, sel, sel2, oh]
                            p.op('dve', lambda e: e.tensor_reduce(sx[:, 2:3], lg[:, 0:4], AX.X, ALU.max), reads=K_, writes=[sx])
                            p.op('dve', lambda e: e.tensor_scalar(oh[:, 0, 0:4], lg[:, 0:4], sx[:, 2:3], None, ALU.is_equal), reads=K_, writes=[oh])
                            p.op('dve', lambda e: e.tensor_scalar(sx[:, 3:4], sx[:, 2:3], -1.0, None, ALU.mult), reads=K_, writes=[sx])
                            p.op('act', lambda e: e.activation(sel2[:, 0:4], lg[:, 0:4], AF.Exp, bias=sx[:, 3:4], accum_out=sx[:, 4:5]), reads=K_, writes=[sel2, sx])
                            p.op('dve', lambda e: e.reciprocal(sx[:, 5:6], sx[:, 4:5]), reads=K_, writes=[sx])
                            p.op('dve', lambda e: e.tensor_mul(r8[:], lg[:, 4:36].rearrange("p (g j) -> p g j", g=4), oh[:, 0, 0:4].unsqueeze(2).to_broadcast([128, 4, 8])), reads=K_, writes=[r8])
                            p.op('dve', lambda e: e.tensor_reduce(sel[:], r8[:].rearrange("p g j -> p j g"), AX.X, ALU.add), reads=K_, writes=[sel])
                            p.op('dve', lambda e: e.tensor_reduce(sx[:, 6:7], sel[:], AX.X, ALU.max), reads=K_, writes=[sx])
                            p.op('dve', lambda e: e.tensor_scalar(oh[:, 1, :], sel[:], sx[:, 6:7], None, ALU.is_equal), reads=K_, writes=[oh])
                            p.op('dve', lambda e: e.scalar_tensor_tensor(sel2[:], oh[:, 1, :], -1e30, sel[:], ALU.mult, ALU.add), reads=K_, writes=[sel2])
                            p.op('dve', lambda e: e.tensor_reduce(sx[:, 7:8], sel2[:], AX.X, ALU.max), reads=K_, writes=[sx])
                            p.op('dve', lambda e: e.tensor_scalar(oh[:, 2, :], sel2[:], sx[:, 7:8], None, ALU.is_equal), reads=K_, writes=[oh])
                            p.op('dve', lambda e: e.tensor_sub(sx[:, 8:9], sx[:, 6:7], sx[:, 7:8]), reads=K_, writes=[sx])
                            p.op('act', lambda e: e.activation(sx[:, 9:10], sx[:, 8:9], AF.Sigmoid), reads=K_, writes=[sx])
                            p.op('dve', lambda e: e.tensor_mul(W01[:, tt, 0:1], sx[:, 9:10], sx[:, 5:6]), reads=K_, writes=[W01])
                            p.op('dve', lambda e: e.tensor_sub(W01[:, tt, 1:2], sx[:, 5:6], W01[:, tt, 0:1]), reads=K_ + [W01], writes=[W01])
                            for k_, Mk in ((1, M0), (2, M1)):
                                p.op('dve', lambda e: e.tensor_mul(Mk[:, tt, :].rearrange("p (g j) -> p g j", g=4), oh[:, 0, 0:4].unsqueeze(2).to_broadcast([128, 4, 8]),
                                                                  oh[:, k_, :].unsqueeze(1).to_broadcast([128, 4, 8])), reads=K_, writes=[Mk])
                            p.op('dve', lambda e: e.tensor_add(Mh[:, tt, :], M0[:, tt, :], M1[:, tt, :]), reads=[M0, M1], writes=[Mh])
                    return run
                p.interleave([o_stream(0), o_stream(1)])
                lsb = p.sb(ph, [128, 128], BF16)
                lsf = p.sb(ph, [128, 128], F32)
                p.dma('sp', lsf[:], lstrict_d, writes=[lsf])
                p.op('dve', lambda e: e.tensor_copy(lsb[:], lsf[:]), reads=[lsf], writes=[lsb])
                carry = p.sb(ph, [128, 32], F32)
                RANK = p.sb(ph, [128, NT, 32], F32)
                p.op('dve', lambda e: e.memset(carry[:], 0.0), writes=[carry])
                p.op('dve', lambda e: e.memset(RANK[:], 0.0), writes=[RANK])
                for tt in range(T0, NT):
                    def mmk(e):
                        e.matmul(pl[:, 0:32], lsb[:], Mh[:, tt, :], start=True, stop=True)
                        return e.matmul(pl[:, 32:64], ones_b[:], Mh[:, tt, :], start=True, stop=True)
                    p.op('pe', mmk, reads=[lsb, ones_b, Mh], writes=[pl])
                    p.op('dve', lambda e: e.tensor_add(RANK[:, tt, :], pl[:, 0:32], carry[:]), reads=[pl, carry], writes=[RANK])
                    p.op('dve', lambda e: e.tensor_add(carry[:], carry[:], pl[:, 32:64]), reads=[pl, carry], writes=[carry])
                NK = (2 * T) // MOEB + 2
                thr = p.sb(ph, [128, NK], F32)
                p.op('pool', lambda e: e.iota(thr[:], [[MOEB, NK]], base=0, channel_multiplier=0, allow_small_or_imprecise_dtypes=True), writes=[thr])
                cmp_ = p.sb(ph, [128, 32, NK], F32)
                padded = p.sb(ph, [128, 32], F32)
                pend = [p.sb(ph, [128, 32], F32) for _ in range(2)]
                p.op('dve', lambda e: e.tensor_tensor(cmp_[:], carry[:].unsqueeze(2).to_broadcast([128, 32, NK]), thr[:].unsqueeze(1).to_broadcast([128, 32, NK]), ALU.is_gt),
                     reads=[carry, thr], writes=[cmp_])
                p.op('dve', lambda e: e.tensor_reduce(padded[:], cmp_[:], AX.X, ALU.add), reads=[cmp_], writes=[padded])
                p.op('dve', lambda e: e.tensor_scalar(padded[:], padded[:], float(MOEB), None, ALU.mult), reads=[padded], writes=[padded])
                p.op('dve', lambda e: e.tensor_copy(pend[0][:], padded[:]), reads=[padded], writes=[pend[0]])
                cur = 0
                for sft in (1, 2, 4, 8, 16):
                    a = pend[cur]; b2 = pend[1 - cur]
                    p.op('dve', lambda e: e.tensor_copy(b2[:, 0:sft], a[:, 0:sft]), reads=[a], writes=[b2])
                    p.op('dve', lambda e: e.tensor_add(b2[:, sft:32], a[:, sft:32], a[:, 0:32 - sft]), reads=[a, b2], writes=[b2])
                    cur = 1 - cur
                pe_ = pend[cur]
                pstart = pend[1 - cur]
                p.op('dve', lambda e: e.tensor_sub(pstart[:], pe_[:], padded[:]), reads=[pe_, padded], writes=[pstart])
                destf = p.sb(ph, [128, NT, 2], F32)
                big = p.sb(ph, [128, NT, 32], F32)
                p.op('dve', lambda e: e.tensor_add(RANK[:], RANK[:], pstart[:].unsqueeze(1).to_broadcast([128, NT, 32])), reads=[RANK, pstart], writes=[RANK])
                for k_, Mk in ((0, M0), (1, M1)):
                    p.op('dve', lambda e: e.tensor_mul(big[:], RANK[:], Mk[:]), reads=[RANK, Mk], writes=[big])
                    p.op('dve', lambda e: e.tensor_reduce(destf[:, :, k_], big[:], AX.X, ALU.add), reads=[big], writes=[destf])
                p.op('dve', lambda e: e.tensor_copy(DESTi[:], destf[:]), reads=[destf], writes=[DESTi])
                bvals = p.sb(ph, [128, NBLK], F32)
                p.op('pool', lambda e: e.iota(bvals[:], [[MOEB, NBLK]], base=0, channel_multiplier=0, allow_small_or_imprecise_dtypes=True), writes=[bvals])
                cmpb = p.sb(ph, [128, NBLK, 32], F32)
                bex = p.sb(ph, [128, NBLK], F32)
                p.op('dve', lambda e: e.tensor_tensor(cmpb[:], pe_[:].unsqueeze(1).to_broadcast([128, NBLK, 32]), bvals[:].unsqueeze(2).to_broadcast([128, NBLK, 32]), ALU.is_le),
                     reads=[pe_, bvals], writes=[cmpb])
                p.op('dve', lambda e: e.tensor_reduce(bex[:], cmpb[:], AX.X, ALU.add), reads=[cmpb], writes=[bex])
                p.op('dve', lambda e: e.tensor_scalar(bex[:], bex[:], float(NEXP - 1), None, ALU.min), reads=[bex], writes=[bex])
                pcol = p.sb(ph, [128, 1], F32)
                p.dma('sp', pcol[:], pcol_d, writes=[pcol])
                idxf = p.sb(ph, [128, NBLK, 8], F32)
                base = p.sb(ph, [128, NBLK], F32)
                p.op('dve', lambda e: e.tensor_scalar(base[:], bex[:], float(D), float(layer * NEXP * D), ALU.mult, ALU.add), reads=[bex], writes=[base])
                for kc in range(8):
                    p.op('dve', lambda e: e.tensor_scalar(idxf[:, :, kc], base[:], pcol[:, 0:1], float(kc * 128), ALU.add, ALU.add), reads=[base, pcol], writes=[idxf])
                p.op('dve', lambda e: e.tensor_copy(IDXGU[:], idxf[:]), reads=[idxf], writes=[IDXGU])
                p.op('dve', lambda e: e.tensor_scalar(base[:], bex[:], 256.0, float(layer * NEXP * 256), ALU.mult, ALU.add), reads=[bex], writes=[base])
                for fc in range(2):
                    p.op('dve', lambda e: e.tensor_scalar(idxf[:, :, fc], base[:], pcol[:, 0:1], float(fc * 128), ALU.add, ALU.add), reads=[base, pcol, IDXGU], writes=[idxf])
                p.op('dve', lambda e: e.tensor_copy(IDXDN[:], idxf[:, :, 0:2]), reads=[idxf], writes=[IDXDN])
                zt = p.sb(ph, [128, 8, D], BF16)
                p.op('dve', lambda e: e.memset(zt[:], 0.0), writes=[zt])
                for s0 in range(0, NSLOT, 1024):
                    n_ = min(1024, NSLOT - s0) // 128
                    p.dma('sp', XS[s0:s0 + n_ * 128, :].rearrange("(n p) d -> p n d", p=128), zt[:, 0:n_, :], reads=[zt], writes=['XS'])
                hd = [p.sb(ph, [128, D], BF16) for _ in range(2)]
                for tt in range(T0, NT):
                    hb_ = hd[tt % 2]
                    p.dma('sp', hb_[:], H2[tt * 128:(tt + 1) * 128, :], reads=['H2'], writes=[hb_])
                    for k_ in range(2):
                        p.idma(XS[:, :], bass.IndirectOffsetOnAxis(ap=DESTi[:, tt, k_:k_ + 1], axis=0), hb_[:], None,
                               reads=[hb_, DESTi, 'XS'], writes=[p.uk()])
                p.barrier()
            if dbg == 'O':
                break

            with ExitStack() as ph:
                sets = []
                for si in range(2):
                    B_ = dict(
                        wf=p.sb(ph, [128, 8, 512], F32), df=p.sb(ph, [128, 2, D], F32),
                        wgub=p.sb(ph, [128, 8, 512], BF16), wdnb=p.sb(ph, [128, 2, D], BF16),
                        xb=p.sb(ph, [128, 2, D], BF16), xsT=p.sb(ph, [128, 8, 256], BF16),
                        sg=p.sb(ph, [128, 2, 256], F32), hT=p.sb(ph, [128, 2, 256], BF16),
                        ysb=[p.sb(ph, [128, D], F32) for _ in range(2)],
                        pg=p.ps(ph, [128, 2, 256]), py=p.ps(ph, [128, D]), pst=p.ps(ph, [128, 8, 128], BF16))
                    sets.append(B_)

                def blk(b, B_):
                    wf, df, wgub, wdnb, xb, xsT, sg, hT, ysb, pg, py, pst = (B_[k] for k in ('wf', 'df', 'wgub', 'wdnb', 'xb', 'xsT', 'sg', 'hT', 'ysb', 'pg', 'py', 'pst'))
                    for kc in range(8):
                        p.idma(wf[:, kc, :], None, w_gu[:, :], bass.IndirectOffsetOnAxis(ap=IDXGU[:, b, kc:kc + 1], axis=0), reads=[IDXGU], writes=[(wf.name, kc)])
                    for fc in range(2):
                        p.idma(df[:, fc, :], None, w_dn[:, :], bass.IndirectOffsetOnAxis(ap=IDXDN[:, b, fc:fc + 1], axis=0), reads=[IDXDN], writes=[(df.name, fc)])
                    s0 = b * MOEB
                    p.dma('sp', xb[:], XS[s0:s0 + 256, :].rearrange("(s p) d -> p s d", p=128), reads=['XS'], writes=[xb])
                    p.op('act', lambda e: e.copy(wgub[:, 0:4, :], wf[:, 0:4, :]), reads=[(wf.name, kc) for kc in range(4)], writes=[(wgub.name, 0)])
                    p.op('dve', lambda e: e.tensor_copy(wgub[:, 4:8, :], wf[:, 4:8, :]), reads=[(wf.name, kc) for kc in range(4, 8)], writes=[(wgub.name, 1)])
                    p.op('act', lambda e: e.copy(wdnb[:, 0, :], df[:, 0, :]), reads=[(df.name, 0)], writes=[(wdnb.name, 0)])
                    p.op('dve', lambda e: e.tensor_copy(wdnb[:, 1, :], df[:, 1, :]), reads=[(df.name, 1)], writes=[(wdnb.name, 1)])
                    for s in range(2):
                        def tr(e):
                            for kc in range(8):
                                r = e.transpose(pst[:, kc, :], xb[:, s, kc * 128:(kc + 1) * 128], ident_b[:])
                            return r
                        p.op('pe', tr, reads=[xb, ident_b], writes=[pst])
                        p.op('act', lambda e: e.copy(xsT[:, :, s * 128:(s + 1) * 128], pst[:]), reads=[pst], writes=[xsT])
                    for half in range(2):
                        def mmg(e):
                            for n in range(2):
                                for kc in range(8):
                                    c0 = (half * 2 + n) * 128
                                    r = e.matmul(pg[:, n, :], wgub[:, kc, c0:c0 + 128], xsT[:, kc, :], start=(kc == 0), stop=(kc == 7))
                            return r
                        p.op('pe', mmg, reads=[(wgub.name, 0), (wgub.name, 1), xsT], writes=[pg])
                        if half == 0:
                            p.op('act', lambda e: e.activation(sg[:], pg[:], AF.Silu), reads=[pg], writes=[sg])
                        else:
                            p.op('dve', lambda e: e.tensor_mul(hT[:], sg[:], pg[:]), reads=[sg, pg], writes=[hT])
                    for s in range(2):
                        yb_ = ysb[s]

                        def mmy(e):
                            for nb in range(2):
                                for fc in range(2):
                                    r = e.matmul(py[:, nb * 512:(nb + 1) * 512], hT[:, fc, s * 128:(s + 1) * 128], wdnb[:, fc, nb * 512:(nb + 1) * 512], start=(fc == 0), stop=(fc == 1))
                            return r
                        p.op('pe', mmy, reads=[hT, (wdnb.name, 0), (wdnb.name, 1)], writes=[py])
                        if s == 0:
                            p.op('act', lambda e: e.copy(yb_[:], py[:]), reads=[py], writes=[yb_])
                        else:
                            p.op('dve', lambda e: e.tensor_copy(yb_[:], py[:]), reads=[py], writes=[yb_])
                        p.dma('sp', YS[s0 + s * 128:s0 + (s + 1) * 128, :], yb_[:], reads=[yb_], writes=[p.uk()])

                def run_set(si):
                    for b in range(si, NBLK, 2):
                        blk(b, sets[si])
                p.interleave([lambda: run_set(0), lambda: run_set(1)])
                p.barrier()

            with ExitStack() as ph:
                M5 = [p.sb(ph, [128, D], F32) for _ in range(2)]
                for s in range(2):
                    p.dma('sp', M5[s][:], MOD[layer, s:s + 1, 5 * D:6 * D].to_broadcast([128, D]), writes=[M5[s]])
                fg = p.sb(ph, [128, D], F32)
                p.dma('sp', fg[:], final_g[0:1, :].to_broadcast([128, D]), writes=[fg])
                g0 = [p.sb(ph, [128, D], F32) for _ in range(2)]
                g1_ = [p.sb(ph, [128, D], F32) for _ in range(2)]
                xt = [p.sb(ph, [128, D], F32) for _ in range(2)]
                ys_ = [p.sb(ph, [128, D], F32) for _ in range(2)]
                sxs = [p.sb(ph, [128, 4], F32) for _ in range(2)]

                def cb_stream(si):
                    def run():
                        for tt in range(T0 + si, NT, 2):
                            s = 1 if tt < LT else 0
                            tok = slice(tt * 128, (tt + 1) * 128)
                            a = g0[si]; b2 = g1_[si]; xb = xt[si]; y = ys_[si]; sx = sxs[si]
                            p.idma(a[:], None, YS[:, :], bass.IndirectOffsetOnAxis(ap=DESTi[:, tt, 0:1], axis=0), reads=['YS', DESTi], writes=[a])
                            p.idma(b2[:], None, YS[:, :], bass.IndirectOffsetOnAxis(ap=DESTi[:, tt, 1:2], axis=0), reads=['YS', DESTi], writes=[b2])
                            p.dma('sp', xb[:], X[tok, :], reads=[('X', tt)], writes=[xb])
                            p.op('dve', lambda e: e.tensor_scalar(y[:], a[:], W01[:, tt, 0:1], None, ALU.mult), reads=[a, W01], writes=[y])
                            p.op('dve', lambda e: e.scalar_tensor_tensor(y[:], b2[:], W01[:, tt, 1:2], y[:], ALU.mult, ALU.add), reads=[b2, W01, y], writes=[y])
                            p.op('dve', lambda e: e.tensor_mul(y[:], y[:], M5[s][:]), reads=[y, M5[s]], writes=[y])
                            p.op('dve', lambda e: e.tensor_add(xb[:], xb[:], y[:]), reads=[xb, y], writes=[xb])
                            if layer == 0:
                                p.dma('sp', X[tok, :], xb[:], reads=[xb], writes=[('X', tt)])
                            else:
                                p.op('act', lambda e: e.activation(y[:], xb[:], AF.Square, accum_out=sx[:, 0:1]), reads=[xb], writes=[y, sx])
                                p.op('dve', lambda e: e.tensor_scalar(sx[:, 1:2], sx[:, 0:1], 1.0 / D, EPS, ALU.mult, ALU.add), reads=[sx], writes=[sx])
                                rsqrt(sx[:, 1:2], [sx])
                                p.op('dve', lambda e: e.scalar_tensor_tensor(y[:], xb[:], sx[:, 1:2], fg[:], ALU.mult, ALU.mult), reads=[xb, sx, fg], writes=[y])
                                p.dma('sp', out[(tt - LT) * 128:(tt - LT + 1) * 128, :], y[:], reads=[y], writes=[p.uk()])
                    return run
                p.interleave([cb_stream(0), cb_stream(1)])
                p.barrier()
    return nc


_L, _N = 256, 4096
_NC_CACHE = {}


def _core_inputs(inp, b):
    L, N = _L, _N
    T = L + N
    f = lambda a: np.ascontiguousarray(np.asarray(a, dtype=np.float32))
    m = {}
    m['xin'] = f(np.concatenate([inp['ctx'][b], inp['x'][b]], axis=0))
    c2 = np.stack([np.asarray(inp['c'][b]), np.asarray(inp['c_ctx'])], axis=0)
    m['c2T'] = f(c2.reshape(2, 8, 128).transpose(2, 1, 0))
    return m


def _shared_inputs(inp):
    L, N = _L, _N
    T = L + N
    f = lambda a: np.ascontiguousarray(np.asarray(a, dtype=np.float32))
    m = {}
    for k in ['w_mod', 'b_mod', 'norm1_g', 'norm2_g', 'w_in', 'w_out', 'hgrn_norm_g', 'gla_norm_g', 'mla_w_uq', 'mla_w_ukv']:
        m[k] = f(inp[k])
    m['final_norm_g'] = f(np.asarray(inp['final_norm_g']).reshape(1, -1))
    m['hgrn_lb_logits'] = f(np.asarray(inp['hgrn_lb_logits']).reshape(2, 512))
    m['na_tab'] = f(na_bias_table(np.asarray(inp['na_rpb'], dtype=np.float32)).reshape(2, 4, 8, 64, 512))
    wg = np.zeros((2, 32, 256), np.float32)
    wg[:, 0:16, 0:128] = np.asarray(inp['gla_wg_f'])
    wg[:, 16:32, 128:256] = np.asarray(inp['gla_wg_b'])
    m['gla_wg_bd'] = wg
    m['gla_bg_cat'] = f(np.concatenate([np.asarray(inp['gla_bg_f']), np.asarray(inp['gla_bg_b'])], axis=1))
    m['mla_q_norm_g'] = f(np.asarray(inp['mla_q_norm_g']).reshape(2, 192, 1))
    m['mla_kv_norm_g'] = f(np.asarray(inp['mla_kv_norm_g']).reshape(2, 128, 1))
    m['moe_w_r'] = f(np.concatenate([np.asarray(inp['moe_w_rg']), np.asarray(inp['moe_w_re'])], axis=2))
    m['moe_b_r'] = f(np.concatenate([np.asarray(inp['moe_b_rg']), np.asarray(inp['moe_b_re'])], axis=1))
    m['moe_w_gu'] = f(np.asarray(inp['moe_w_gu']).reshape(2 * 32 * 1024, 512))
    m['moe_w_dn'] = f(np.asarray(inp['moe_w_dn']).reshape(2 * 32 * 256, 1024))
    hc = host_consts(L, N)
    hc['rope'] = hc['rope'].reshape(T, 32)
    m.update(hc)
    return m


def kernel(**inputs):
    inp = {k: np.asarray(v) for k, v in inputs.items()}
    B = inp['x'].shape[0]
    if 'nc' not in _NC_CACHE:
        _NC_CACHE['nc'] = build(_L, _N)
    nc = _NC_CACHE['nc']
    shared = _shared_inputs(inp)
    in_maps = []
    for core in range(8):
        m = dict(shared)
        m.update(_core_inputs(inp, core % B))
        in_maps.append(m)
    res = run_bass_kernel_spmd(nc, in_maps, core_ids=list(range(8)))
    outs = [np.asarray(res.results[b]['out'], dtype=np.float32) for b in range(B)]
    return np.stack(outs, axis=0)
```

```python
import numpy as np
from contextlib import ExitStack
import ml_dtypes
import concourse.bass as bass
import concourse.mybir as mybir
from concourse.bass_utils import run_bass_kernel_spmd

F32 = mybir.dt.float32
BF16 = mybir.dt.bfloat16
I32 = mybir.dt.int32
AF = mybir.ActivationFunctionType
ALU = mybir.AluOpType
AX = mybir.AxisListType

D = 1024
DIN = 3200
EPS = 1e-6
NEG = -30000.0
MOEB = 256
NEXP = 32
RSTAGE = 99
SCL = float(np.exp(-30.0))


class Prog:
    def __init__(self, nc, es, n_dma_sems=14):
        self.nc = nc
        self.es = es
        self.engs = {'pe': nc.tensor, 'act': nc.scalar, 'dve': nc.vector, 'pool': nc.gpsimd, 'sp': nc.sync}
        self.sem = {}
        self.cnt = {}
        for e in ('pe', 'act', 'dve', 'pool'):
            self.sem[e] = es.enter_context(nc.semaphore('s_' + e))
            self.cnt[e] = 0
        self.dsems = {}
        self.dcnt = {}
        self.dnext = {}
        for q in ('sp', 'pool'):
            self.dsems[q] = [es.enter_context(nc.semaphore('d_%s%d' % (q, i))) for i in range(n_dma_sems)]
            self.dcnt[q] = [0] * n_dma_sems
            self.dnext[q] = 0
        self.waited = {}
        self.lastw = {}
        self.readers = {}
        self.nbuf = 0

    def sb(self, st, shape, dt, name=None):
        self.nbuf += 1
        return st.enter_context(self.nc.sbuf_tensor(name or ('sb%d' % self.nbuf), list(shape), dt))

    def ps(self, st, shape, dt=F32, name=None):
        self.nbuf += 1
        return st.enter_context(self.nc.psum_tensor(name or ('ps%d' % self.nbuf), list(shape), dt))

    def _key(self, k):
        if isinstance(k, (str, tuple)):
            return k
        t = getattr(k, 'tensor', k)
        return getattr(t, 'name', None) or id(t)

    def _wait(self, ename, ev):
        sem, val = ev
        k = (ename, sem.num)
        if self.waited.get(k, 0) >= val:
            return
        self.waited[k] = val
        self.engs[ename].wait_ge(sem, val)

    def _deps(self, ename, reads, writes):
        deps = []
        for k in reads:
            k = self._key(k)
            if k in self.lastw:
                deps.append(self.lastw[k])
        for k in writes:
            k = self._key(k)
            if k in self.lastw:
                deps.append(self.lastw[k])
            deps.extend(self.readers.get(k, []))
        for ev in deps:
            self._wait(ename, ev)

    def _record(self, ev, reads, writes):
        for k in reads:
            k = self._key(k)
            self.readers.setdefault(k, []).append(ev)
        for k in writes:
            k = self._key(k)
            self.lastw[k] = ev
            self.readers[k] = []

    def op(self, ename, fn, reads=(), writes=()):
        self._deps(ename, reads, writes)
        ins = fn(self.engs[ename])
        self.cnt[ename] += 1
        ins.then_inc(self.sem[ename], 1)
        ev = (self.sem[ename], self.cnt[ename])
        self._record(ev, reads, writes)
        if getattr(self, '_yield', None):
            self._yield()
        return ev

    def _dma_common(self, q, emit, reads, writes):
        i = self.dnext[q]
        self.dnext[q] = (i + 1) % len(self.dsems[q])
        sem = self.dsems[q][i]
        if self.dcnt[q][i] > 0:
            self._wait(q, (sem, self.dcnt[q][i]))
        self._deps(q, reads, writes)
        ins = emit(self.engs[q])
        self.dcnt[q][i] += 16
        ins.then_inc(sem, 16)
        ev = (sem, self.dcnt[q][i])
        self._record(ev, reads, writes)
        if getattr(self, '_yield', None):
            self._yield()
        return ev

    def dma(self, q, out, in_, reads=(), writes=(), **kw):
        return self._dma_common(q, lambda e: e.dma_start(out=out, in_=in_, **kw), reads, writes)

    def idma(self, out, out_off, in_, in_off, reads=(), writes=(), **kw):
        return self._dma_common('pool', lambda e: e.indirect_dma_start(out=out, out_offset=out_off, in_=in_,
                                                                      in_offset=in_off, **kw), reads, writes)

    def interleave(self, fns):
        import threading
        n = len(fns)
        il = {'turn': 0, 'alive': [True] * n, 'cond': threading.Condition(), 'exc': None}
        tl = threading.local()

        def advance(i):
            for k in range(1, n + 1):
                j = (i + k) % n
                if il['alive'][j]:
                    il['turn'] = j
                    break
            il['cond'].notify_all()

        def wait_turn(i):
            while il['turn'] != i and il['exc'] is None:
                il['cond'].wait()

        def yield_():
            i = getattr(tl, 'idx', None)
            if i is None:
                return
            advance(i)
            wait_turn(i)
            if il['exc'] is not None:
                raise RuntimeError('interleave peer failed')

        def runner(i):
            with il['cond']:
                wait_turn(i)
                tl.idx = i
                try:
                    if il['exc'] is None:
                        fns[i]()
                except BaseException as e:
                    if il['exc'] is None:
                        il['exc'] = e
                il['alive'][i] = False
                if any(il['alive']):
                    advance(i)
                il['cond'].notify_all()

        prev = getattr(self, '_yield', None)
        self._yield = yield_
        ths = [threading.Thread(target=runner, args=(i,)) for i in range(n)]
        for t in ths:
            t.start()
        for t in ths:
            t.join()
        self._yield = prev
        if il['exc'] is not None:
            raise il['exc']

    def uk(self):
        self._ukn = getattr(self, '_ukn', 0) + 1
        return ('uk', self._ukn)

    def barrier(self):
        evs = [(self.sem[e], self.cnt[e]) for e in self.sem if self.cnt[e] > 0]
        for q in self.dsems:
            for i, s in enumerate(self.dsems[q]):
                if self.dcnt[q][i] > 0:
                    evs.append((s, self.dcnt[q][i]))
        for e in self.engs:
            for ev in evs:
                self._wait(e, ev)
        self.lastw = {}
        self.readers = {}


def host_consts(L, N):
    T = L + N
    c = {}
    c['ident_f'] = np.eye(128, dtype=np.float32)
    j = np.arange(128)[:, None]
    i = np.arange(128)[None, :]
    same = (j // 64) == (i // 64)
    U2f = (same & (j <= i)).astype(np.float32)
    Umf = (same & (j % 64 <= 31)).astype(np.float32)
    U2b = (same & (j >= i)).astype(np.float32)
    Umb = (same & (j % 64 >= 32)).astype(np.float32)
    c['uw_f'] = np.concatenate([U2f, U2f - Umf], axis=1)
    c['uw_b'] = np.concatenate([U2b, U2b - Umb], axis=1)
    c['v2_f'] = (same & (j > i)).astype(np.float32)
    c['v2_b'] = (same & (j < i)).astype(np.float32)
    jj = np.arange(64)[:, None]
    ii = np.arange(64)[None, :]
    c['mask_f'] = ((jj <= ii).astype(np.float64) * np.exp(60.0)).astype(np.float32)
    c['mask_b'] = ((jj >= ii).astype(np.float64) * np.exp(60.0)).astype(np.float32)
    c['lstrict'] = (j < i).astype(np.float32)
    t = np.arange(N)
    inv = (10000.0 ** (-np.arange(8, dtype=np.float32) / 8)).astype(np.float32)
    ang = np.stack([(t // 64).astype(np.float32)[:, None] * inv, (t % 64).astype(np.float32)[:, None] * inv], axis=1)
    rope = np.zeros((T, 2, 2, 8), np.float32)
    rope[:L, 0] = 1.0
    rope[L:, 0] = np.cos(ang)
    rope[L:, 1] = np.sin(ang)
    c['rope'] = rope
    c['pcol'] = np.arange(128, dtype=np.float32)[:, None].copy()
    return c


def na_bias_table(rpb):
    cidx = np.arange(64)
    c_start = np.clip(cidx - 8, 0, 48)
    col_in = (cidx[None] >= c_start[:, None]) & (cidx[None] < c_start[:, None] + 16)
    dc = np.clip(cidx[None] - cidx[:, None], -15, 15) + 15
    out = np.full((2, 4, 8, 64, 8, 64), NEG, np.float32)
    for cls in range(8):
        for jx in range(8):
            dr = jx + 7 - cls
            g = rpb[:, :, dr, :][:, :, dc]
            g = np.where(col_in[None, None], g, NEG)
            out[:, :, cls, :, jx, :] = np.transpose(g, (0, 1, 3, 2))
    return out


def build(L, N, dbg=False):
    T = L + N
    NT = T // 128
    NCH = T // 64
    LT = L // 128
    ROWS = N // 64
    NBLK = (2 * T + MOEB - 1) // MOEB + NEXP
    NSLOT = NBLK * MOEB
    nc = bass.Bass("TRN2", target_bir_lowering=False)

    def din(name, shape, dt=F32):
        return nc.dram_tensor(name, list(shape), dt, kind="ExternalInput").ap()

    def dscr(name, shape, dt=F32):
        kind = "ExternalOutput" if dbg else "Internal"
        return nc.dram_tensor(name, list(shape), dt, kind=kind).ap()

    xin = din('xin', [T, D])
    c2T = din('c2T', [128, 8, 2])
    w_mod = din('w_mod', [2, D, 6 * D])
    b_mod = din('b_mod', [2, 6 * D])
    norm1_g = din('norm1_g', [2, D])
    norm2_g = din('norm2_g', [2, D])
    final_g = din('final_norm_g', [1, D])
    w_in = din('w_in', [2, D, DIN])
    w_out = din('w_out', [2, D, D])
    lb_logits = din('hgrn_lb_logits', [2, 512])
    hgrn_ng = din('hgrn_norm_g', [2, 256])
    na_tab = din('na_tab', [2, 4, 8, 64, 512])
    wg_bd = din('gla_wg_bd', [2, 32, 256])
    bg_cat = din('gla_bg_cat', [2, 256])
    gla_ng = din('gla_norm_g', [2, 256])
    mla_qg = din('mla_q_norm_g', [2, 192, 1])
    mla_wuq = din('mla_w_uq', [2, 192, 384])
    mla_kvg = din('mla_kv_norm_g', [2, 128, 1])
    mla_wukv = din('mla_w_ukv', [2, 128, 512])
    w_r = din('moe_w_r', [2, D, 36])
    b_r = din('moe_b_r', [2, 36])
    w_gu = din('moe_w_gu', [2 * NEXP * D, 512])
    w_dn = din('moe_w_dn', [2 * NEXP * 256, D])
    ident_f_d = din('ident_f', [128, 128])
    uw_d = {'f': din('uw_f', [128, 256]), 'b': din('uw_b', [128, 256])}
    v2_d = {'f': din('v2_f', [128, 128]), 'b': din('v2_b', [128, 128])}
    mask_d = {'f': din('mask_f', [64, 64]), 'b': din('mask_b', [64, 64])}
    lstrict_d = din('lstrict', [128, 128])
    rope_d = din('rope', [T, 32])
    pcol_d = din('pcol', [128, 1])
    out = nc.dram_tensor('out', [N, D], F32, kind="ExternalOutput").ap()

    X = dscr('X', [T, D])
    MOD = dscr('MOD', [2, 2, 6 * D])
    mix = {}
    for m in 'AC':
        for d in 'fb':
            mix[m + d + 'QT'] = dscr('s_%s%s_QT' % (m, d), [256, T], BF16)
            mix[m + d + 'KT'] = dscr('s_%s%s_KT' % (m, d), [256, T], BF16)
            mix[m + d + 'QH'] = dscr('s_%s%s_QH' % (m, d), [256, T], BF16)
            mix[m + d + 'KH'] = dscr('s_%s%s_KH' % (m, d), [T, 256], BF16)
            mix[m + d + 'DEC'] = dscr('s_%s%s_DEC' % (m, d), [256, NCH])
            mix[m + d + 'O'] = dscr('s_%s%s_O' % (m, d), [T, 256])
        mix[m + 'V'] = dscr('s_%s_V' % m, [T, 256], BF16)
        mix[m + 'G'] = dscr('s_%s_G' % m, [T, 256])
    BQT = dscr('s_B_QT', [256, T], BF16)
    BKT = dscr('s_B_KT', [256, T], BF16)
    BV = dscr('s_B_V', [T, 256], BF16)
    DQT = dscr('s_D_QT', [4, 96, T], BF16)
    DKT = dscr('s_D_KT', [4, 96, T], BF16)
    DV = dscr('s_D_V', [T, 256], BF16)
    CATT = dscr('s_CATT', [D, T], BF16)
    H2 = dscr('s_H2', [T, D], BF16)
    XS = dscr('s_XS', [NSLOT, D], BF16)
    YS = dscr('s_YS', [NSLOT, D])

    with ExitStack() as es:
        p = Prog(nc, es)
        ident_f = p.sb(es, [128, 128], F32, 'ident_f_sb')
        ident_b = p.sb(es, [128, 128], BF16, 'ident_b_sb')
        ones_f = p.sb(es, [128, 128], F32, 'ones_f')
        ones_b = p.sb(es, [128, 128], BF16, 'ones_b')
        p.dma('sp', ident_f[:], ident_f_d, writes=[ident_f])
        p.op('dve', lambda e: e.tensor_copy(ident_b[:], ident_f[:]), reads=[ident_f], writes=[ident_b])
        p.op('dve', lambda e: e.memset(ones_f[:], 1.0), writes=[ones_f])
        p.op('dve', lambda e: e.memset(ones_b[:], 1.0), writes=[ones_b])

        def transpose_f(ps_ap, in_ap, reads, writes):
            n = in_ap.shape[0]
            p.op('pe', lambda e: e.transpose(ps_ap, in_ap, ident_f[0:n, 0:n]), reads=list(reads) + [ident_f], writes=writes)

        def transpose_b(ps_ap, in_ap, reads, writes):
            n = in_ap.shape[0]
            p.op('pe', lambda e: e.transpose(ps_ap, in_ap, ident_b[0:n, 0:n]), reads=list(reads) + [ident_b], writes=writes)

        def rsqrt(ap, keys):
            p.op('act', lambda e: e.activation(ap, ap, AF.Ln), reads=keys, writes=keys)
            p.op('act', lambda e: e.activation(ap, ap, AF.Exp, scale=-0.5), reads=keys, writes=keys)

        with ExitStack() as ph:
            xt = [p.sb(ph, [128, D], F32) for _ in range(2)]
            for tt in range(NT):
                b = xt[tt % 2]
                p.dma('sp', b[:], xin[tt * 128:(tt + 1) * 128, :], writes=[b])
                p.dma('sp', X[tt * 128:(tt + 1) * 128, :], b[:], reads=[b], writes=[('X', tt)])
            cT = p.sb(ph, [128, 8, 2], F32)
            sg = p.sb(ph, [128, 8, 2], F32)
            p.dma('sp', cT[:], c2T, writes=[cT])
            p.op('act', lambda e: e.activation(sg[:], cT[:], AF.Sigmoid), reads=[cT], writes=[sg])
            p.op('dve', lambda e: e.tensor_mul(cT[:], cT[:], sg[:]), reads=[cT, sg], writes=[cT])
            wm = [p.sb(ph, [128, 8, 512], F32) for _ in range(2)]
            bm = [p.sb(ph, [2, 512], F32) for _ in range(2)]
            mo = [p.sb(ph, [2, 512], F32) for _ in range(2)]
            pm = [p.ps(ph, [2, 512]) for _ in range(2)]
            it = 0
            for l in range(2):
                for nb in range(12):
                    w = wm[it % 2]; bb = bm[it % 2]; o = mo[it % 2]; ps = pm[it % 2]
                    p.dma('sp', w[:], w_mod[l, :, nb * 512:(nb + 1) * 512].rearrange("(k p) n -> p k n", p=128), writes=[w])
                    p.dma('sp', bb[:], b_mod[l:l + 1, nb * 512:(nb + 1) * 512].to_broadcast([2, 512]), writes=[bb])

                    def mm(e, w=w, ps=ps):
                        for kc in range(8):
                            r = e.matmul(ps[:], cT[:, kc, :], w[:, kc, :], start=(kc == 0), stop=(kc == 7))
                        return r
                    p.op('pe', mm, reads=[cT, w], writes=[ps])
                    p.op('dve', lambda e, o=o, ps=ps, bb=bb: e.tensor_add(o[:], ps[:], bb[:]), reads=[ps, bb], writes=[o])
                    p.dma('sp', MOD[l, :, nb * 512:(nb + 1) * 512], o[:], reads=[o], writes=[p.uk()])
                    it += 1
            p.barrier()

        for layer in range(2):
          with ExitStack() as lay:
            keep_ctx = layer == 0
            T0 = 0 if keep_ctx else LT
            with ExitStack() as ph:
                winb = p.sb(ph, [128, 8, DIN], BF16)
                for kc in range(8):
                    p.dma('pool', winb[:, kc, :], w_in[layer, kc * 128:(kc + 1) * 128, :], writes=[(winb.name, kc)])
                G1 = [p.sb(ph, [128, D], F32) for _ in range(2)]
                B1 = [p.sb(ph, [128, D], F32) for _ in range(2)]
                g1 = p.sb(ph, [128, D], F32)
                p.dma('sp', g1[:], norm1_g[layer:layer + 1, :].to_broadcast([128, D]), writes=[g1])
                for s in range(2):
                    p.dma('sp', B1[s][:], MOD[layer, s:s + 1, 0:D].to_broadcast([128, D]), reads=['MOD'], writes=[B1[s]])
                    p.dma('sp', G1[s][:], MOD[layer, s:s + 1, D:2 * D].to_broadcast([128, D]), reads=['MOD'], writes=[G1[s]])
                    p.op('dve', lambda e, s=s: e.scalar_tensor_tensor(G1[s][:], G1[s][:], 1.0, g1[:], ALU.add, ALU.mult),
                         reads=[G1[s], g1], writes=[G1[s]])
                LB = p.sb(ph, [128, 512], F32)
                OMLB = p.sb(ph, [128, 512], F32)
                if layer == 0:
                    p.op('dve', lambda e: e.memset(LB[:], 0.0), writes=[LB])
                    p.op('dve', lambda e: e.memset(OMLB[:], 1.0), writes=[OMLB])
                else:
                    l0 = p.sb(ph, [128, 512], F32)
                    p.dma('sp', l0[:], lb_logits[0:1, :].to_broadcast([128, 512]), writes=[l0])
                    p.dma('sp', LB[:], lb_logits[1:2, :].to_broadcast([128, 512]), writes=[LB])
                    p.op('dve', lambda e: e.tensor_sub(LB[:], LB[:], l0[:]), reads=[LB, l0], writes=[LB])
                    p.op('act', lambda e: e.activation(LB[:], LB[:], AF.Sigmoid), reads=[LB], writes=[LB])
                    p.op('dve', lambda e: e.tensor_scalar(OMLB[:], LB[:], -1.0, 1.0, ALU.mult, ALU.add), reads=[LB], writes=[OMLB])
                wgbd = p.sb(ph, [32, 256], F32)
                bgc = p.sb(ph, [128, 256], F32)
                p.dma('sp', wgbd[:], wg_bd[layer], writes=[wgbd])
                p.dma('sp', bgc[:], bg_cat[layer:layer + 1, :].to_broadcast([128, 256]), writes=[bgc])
                wuq = p.sb(ph, [128, 2, 384], F32)
                wuqb = p.sb(ph, [128, 2, 384], BF16)
                qg = p.sb(ph, [128, 2], F32)
                wukv = p.sb(ph, [128, 512], F32)
                wukvb = p.sb(ph, [128, 512], BF16)
                kvg = p.sb(ph, [128, 1], F32)
                p.dma('sp', wuq[:, 0, :], mla_wuq[layer, 0:128, :], writes=[wuq])
                p.dma('sp', wuq[0:64, 1, :], mla_wuq[layer, 128:192, :], writes=[wuq])
                p.dma('sp', qg[:, 0:1], mla_qg[layer, 0:128, :], writes=[qg])
                p.dma('sp', qg[0:64, 1:2], mla_qg[layer, 128:192, :], writes=[qg])
                p.dma('sp', wukv[:], mla_wukv[layer], writes=[wukv])
                p.dma('sp', kvg[:], mla_kvg[layer], writes=[kvg])
                p.op('dve', lambda e: e.tensor_scalar(wuqb[:, 0, :], wuq[:, 0, :], qg[:, 0:1], None, ALU.mult), reads=[wuq, qg], writes=[wuqb])
                p.op('dve', lambda e: e.tensor_scalar(wuqb[0:64, 1, :], wuq[0:64, 1, :], qg[0:64, 1:2], None, ALU.mult), reads=[wuq, qg], writes=[wuqb])
                p.op('dve', lambda e: e.tensor_scalar(wukvb[:], wukv[:], kvg[:, 0:1], None, ALU.mult), reads=[wukv, kvg], writes=[wukvb])
                uw = {}; v2 = {}
                for d in 'fb':
                    uw[d] = p.sb(ph, [128, 256], F32)
                    v2[d] = p.sb(ph, [128, 128], F32)
                    p.dma('sp', uw[d][:], uw_d[d], writes=[uw[d]])
                    p.dma('sp', v2[d][:], v2_d[d], writes=[v2[d]])
                xt = [p.sb(ph, [128, D], F32) for _ in range(2)]
                junk = p.sb(ph, [128, D], F32)
                st = [p.sb(ph, [128, 8], F32) for _ in range(2)]
                hb = p.sb(ph, [128, D], BF16)
                hT = p.sb(ph, [128, 8, 128], BF16)
                pTs = [p.sb(ph, [128, DIN], F32) for _ in range(2)]
                junk2 = p.sb(ph, [128, 192], F32)
                ps_t = p.ps(ph, [128, 8, 128], BF16)
                ps_t2 = p.ps(ph, [128, 8, 128], BF16)
                ps_p = [p.ps(ph, [128, 512]) for _ in range(2)]
                ps_a = p.ps(ph, [128, 2, 256])
                ps_b = p.ps(ph, [128, 2, 256])
                ps_c = p.ps(ph, [128, 512])
                ps_d = p.ps(ph, [128, 512])
                qA = p.sb(ph, [128, 256], F32)
                kk = p.sb(ph, [128, 256], F32)
                la = p.sb(ph, [128, 256], F32)
                qTs = p.sb(ph, [128, 2, 128], F32)
                kTs = p.sb(ph, [128, 2, 128], F32)
                x2 = p.sb(ph, [128, 2, 128], F32)
                x1 = p.sb(ph, [128, 2, 128], F32)
                x1n = p.sb(ph, [128, 2, 128], F32)
                x3 = p.sb(ph, [128, 256], F32)
                o_qh = p.sb(ph, [128, 2, 128], BF16)
                o_qt = p.sb(ph, [128, 2, 128], BF16)
                o_kt = p.sb(ph, [128, 2, 128], BF16)
                o_kh = p.sb(ph, [128, 256], BF16)
                gs = p.sb(ph, [128, 256], F32)
                qpad = p.sb(ph, [128, 4, 64], F32)
                kpad = p.sb(ph, [128, 4, 64], F32)
                lapad = p.sb(ph, [128, 4, 64], F32)
                zT = p.sb(ph, [32, 128], F32)
                lac = p.sb(ph, [128, 256], F32)
                rin = p.sb(ph, [128, 5, 32], F32)
                rout = p.sb(ph, [128, 5, 32], F32)
                rtmp5 = p.sb(ph, [128, 5, 32], F32)
                rtmp = p.sb(ph, [128, 4, 32], F32)
                bq = p.sb(ph, [128, 256], BF16)
                bqT = p.sb(ph, [128, 2, 128], BF16)
                bkT = p.sb(ph, [128, 2, 128], BF16)
                bv = p.sb(ph, [128, 256], BF16)
                ropets = [p.sb(ph, [128, 32], F32) for _ in range(2)]
                cqn = p.sb(ph, [128, 192], BF16)
                ckvn = p.sb(ph, [128, 128], BF16)
                cqT = p.sb(ph, [128, 2, 128], BF16)
                ckvT = p.sb(ph, [128, 128], BF16)
                qd = p.sb(ph, [128, 4, 96], F32)
                qd2 = p.sb(ph, [128, 4, 96], F32)
                qdb = p.sb(ph, [128, 4, 96], BF16)
                kvd = p.sb(ph, [128, 4, 128], F32)
                kdb = p.sb(ph, [128, 4, 96], BF16)
                kr = p.sb(ph, [128, 32], F32)
                kr2 = p.sb(ph, [128, 32], F32)
                vdb = p.sb(ph, [128, 256], BF16)
                dT = p.sb(ph, [96, 4, 128], BF16)
                p.op('dve', lambda e: e.memset(qpad[:], 0.0), writes=[qpad])
                p.op('dve', lambda e: e.memset(kpad[:], 0.0), writes=[kpad])
                p.op('dve', lambda e: e.memset(lapad[:], 0.0), writes=[lapad])

                def gla_prep(m, tt, d, q_ap, k_ap, la_ap, rq, rk, rl, first):
                    tok = slice(tt * 128, (tt + 1) * 128)
                    if first:
                        for ct in range(2):
                            transpose_f(ps_a[:, ct, 0:128], q_ap[:, ct * 128:(ct + 1) * 128], rq, [ps_a])
                        p.op('act', lambda e: e.copy(qTs[:], ps_a[:, :, 0:128]), reads=[ps_a], writes=[qTs])
                    for ct in range(2):
                        transpose_f(ps_a[:, ct, 128:256], k_ap[:, ct * 128:(ct + 1) * 128], rk, [ps_a])
                    p.op('act', lambda e: e.copy(kTs[:], ps_a[:, :, 128:256]), reads=[ps_a], writes=[kTs])
                    for ct in range(2):
                        p.op('pe', lambda e, ct=ct: e.matmul(ps_b[:, ct, :], la_ap[:, ct * 128:(ct + 1) * 128], uw[d][:], start=True, stop=True),
                             reads=list(rl) + [uw[d]], writes=[ps_b])
                    p.op('pe', lambda e: e.matmul(ps_c[:, 0:256], v2[d][:], la_ap, start=True, stop=True), reads=list(rl) + [v2[d]], writes=[ps_c])
                    p.op('act', lambda e: e.activation(x2[:], ps_b[:, :, 0:128], AF.Exp), reads=[ps_b], writes=[x2])
                    p.op('act', lambda e: e.activation(x1[:], ps_b[:, :, 128:256], AF.Exp), reads=[ps_b], writes=[x1])
                    p.op('act', lambda e: e.activation(x1n[:], ps_b[:, :, 128:256], AF.Exp, scale=-1.0), reads=[ps_b], writes=[x1n])
                    p.op('act', lambda e: e.activation(x3[:], ps_c[:, 0:256], AF.Exp), reads=[ps_c], writes=[x3])
                    p.op('dve', lambda e: e.tensor_mul(o_qh[:], qTs[:], x2[:]), reads=[qTs, x2], writes=[o_qh])
                    p.op('dve', lambda e: e.scalar_tensor_tensor(o_qt[:], qTs[:], SCL, x1[:], ALU.mult, ALU.mult), reads=[qTs, x1], writes=[o_qt])
                    p.op('dve', lambda e: e.scalar_tensor_tensor(o_kt[:], kTs[:], SCL, x1n[:], ALU.mult, ALU.mult), reads=[kTs, x1n], writes=[o_kt])
                    p.op('dve', lambda e: e.tensor_mul(o_kh[:], k_ap, x3[:]), reads=list(rk) + [x3], writes=[o_kh])
                    md = m + d
                    p.dma('pool', mix[md + 'QH'][:, tok].rearrange("(c p) t -> p c t", p=128), o_qh[:], reads=[o_qh], writes=[p.uk()])
                    p.dma('pool', mix[md + 'QT'][:, tok].rearrange("(c p) t -> p c t", p=128), o_qt[:], reads=[o_qt], writes=[p.uk()])
                    p.dma('pool', mix[md + 'KT'][:, tok].rearrange("(c p) t -> p c t", p=128), o_kt[:], reads=[o_kt], writes=[p.uk()])
                    p.dma('pool', mix[md + 'KH'][tok, :], o_kh[:], reads=[o_kh], writes=[p.uk()])
                    cols = (63, 127) if d == 'f' else (0, 64)
                    for cc in range(2):
                        p.dma('pool', mix[md + 'DEC'][:, 2 * tt + cc:2 * tt + cc + 1].rearrange("(c p) t -> p c t", p=128),
                              x2[:, :, cols[cc]:cols[cc] + 1], reads=[x2], writes=[p.uk()], allow_slow_non_contiguous=True)

                ncb = (DIN + 511) // 512

                def front(tt):
                    s = 1 if tt < LT else 0
                    tok = slice(tt * 128, (tt + 1) * 128)
                    xb = xt[tt % 2]; sx = st[tt % 2]; pT = pTs[tt % 2]; ropet = ropets[tt % 2]
                    p.dma('sp', xb[:], X[tok, :], reads=[('X', tt)], writes=[xb])
                    p.dma('sp', ropet[:], rope_d[tok, :], writes=[ropet])
                    p.op('act', lambda e: e.activation(junk[:], xb[:], AF.Square, accum_out=sx[:, 0:1]), reads=[xb], writes=[junk, sx])
                    p.op('dve', lambda e: e.tensor_scalar(sx[:, 1:2], sx[:, 0:1], 1.0 / D, EPS, ALU.mult, ALU.add), reads=[sx], writes=[sx])
                    rsqrt(sx[:, 1:2], [sx])
                    p.op('dve', lambda e: e.scalar_tensor_tensor(junk[:], xb[:], sx[:, 1:2], G1[s][:], ALU.mult, ALU.mult), reads=[xb, sx, G1[s]], writes=[junk])
                    p.op('dve', lambda e: e.tensor_add(hb[:], junk[:], B1[s][:]), reads=[junk, B1[s]], writes=[hb])
                    for kc in range(8):
                        transpose_b(ps_t2[:, kc, :], hb[:, kc * 128:(kc + 1) * 128], [hb], [ps_t2])
                    p.op('act', lambda e: e.copy(hT[:], ps_t2[:]), reads=[ps_t2], writes=[hT])
                    for cb in range(ncb):
                        c0 = cb * 512; c1 = min(DIN, c0 + 512)
                        pp = ps_p[cb % 2]

                        def mm(e, pp=pp, c0=c0, c1=c1):
                            for kc in range(8):
                                r = e.matmul(pp[:, 0:c1 - c0], hT[:, kc, :], winb[:, kc, c0:c1], start=(kc == 0), stop=(kc == 7))
                            return r
                        p.op('pe', mm, reads=[hT] + [(winb.name, kc) for kc in range(8)], writes=[pp])
                        eng = 'act' if cb % 2 == 0 else 'dve'
                        if eng == 'act':
                            p.op('act', lambda e, pp=pp, c0=c0, c1=c1: e.copy(pT[:, c0:c1], pp[:, 0:c1 - c0]), reads=[pp], writes=[(pT.name, cb)])
                        else:
                            p.op('dve', lambda e, pp=pp, c0=c0, c1=c1: e.tensor_copy(pT[:, c0:c1], pp[:, 0:c1 - c0]), reads=[pp], writes=[(pT.name, cb)])

                def back(tt):
                    s = 1 if tt < LT else 0
                    tok = slice(tt * 128, (tt + 1) * 128)
                    sx = st[tt % 2]; pT = pTs[tt % 2]; ropet = ropets[tt % 2]
                    RP = [(pT.name, cb) for cb in range(ncb)]
                    def chain_x():
                        p.op('act', lambda e: e.activation(qA[:], pT[:, 0:256], AF.Silu), reads=RP, writes=[qA])
                        p.op('dve', lambda e: e.tensor_scalar(qA[:], qA[:], 0.125, None, ALU.mult), reads=[qA], writes=[qA])
                        p.dma('pool', mix['AV'][tok, :], pT[:, 256:512], reads=RP, writes=[p.uk()])
                        p.op('act', lambda e: e.activation(gs[:], pT[:, 1024:1280], AF.Silu), reads=RP, writes=[gs])
                        p.dma('pool', mix['AG'][tok, :], gs[:], reads=[gs], writes=[p.uk()])
                        for di, d in enumerate('fb'):
                            zc = slice(512 + 256 * di, 768 + 256 * di)
                            lc = slice(256 * di, 256 * di + 256)
                            p.op('act', lambda e: e.activation(la[:], pT[:, zc], AF.Sigmoid), reads=RP, writes=[la])
                            p.op('dve', lambda e: e.tensor_mul(la[:], la[:], OMLB[:, lc]), reads=[la, OMLB], writes=[la])
                            p.op('dve', lambda e: e.tensor_add(la[:], la[:], LB[:, lc]), reads=[la, LB], writes=[la])
                            p.op('dve', lambda e: e.tensor_scalar(kk[:], la[:], -1.0, 1.0, ALU.mult, ALU.add), reads=[la], writes=[kk])
                            p.op('act', lambda e: e.activation(la[:], la[:], AF.Ln), reads=[la], writes=[la])
                            gla_prep('A', tt, d, qA[:], kk[:], la[:], [qA], [kk], [la], di == 0)
                        p.op('dve', lambda e: e.tensor_scalar(qpad[:, :, 0:32], pT[:, 2048:2176].rearrange("p (h k) -> p h k", h=4), 32.0 ** -0.5, None, ALU.mult),
                             reads=RP, writes=[qpad])
                        p.op('dve', lambda e: e.tensor_copy(kpad[:, :, 0:32], pT[:, 2176:2304].rearrange("p (h k) -> p h k", h=4)), reads=RP, writes=[kpad])
                        p.dma('pool', mix['CV'][tok, :], pT[:, 2304:2560], reads=RP, writes=[p.uk()])
                        p.op('act', lambda e: e.activation(gs[:], pT[:, 2560:2816], AF.Silu), reads=RP, writes=[gs])
                        p.dma('pool', mix['CG'][tok, :], gs[:], reads=[gs], writes=[p.uk()])
                        transpose_f(ps_c[0:32, 256:384], pT[:, 2816:2848], RP, [ps_c])
                        p.op('act', lambda e: e.copy(zT[:], ps_c[0:32, 256:384]), reads=[ps_c], writes=[zT])
                        p.op('pe', lambda e: e.matmul(ps_c[:, 0:256], zT[:], wgbd[:], start=True, stop=True), reads=[zT, wgbd], writes=[ps_c])
                        p.op('dve', lambda e: e.tensor_add(lac[:], ps_c[:, 0:256], bgc[:]), reads=[ps_c, bgc], writes=[lac])
                        p.op('act', lambda e: e.activation(lac[:], lac[:], AF.Sigmoid), reads=[lac], writes=[lac])
                        p.op('act', lambda e: e.activation(lac[:], lac[:], AF.Ln), reads=[lac], writes=[lac])
                        for di, d in enumerate('fb'):
                            p.op('dve', lambda e: e.tensor_scalar(lapad[:, :, 0:32], lac[:, 128 * di:128 * di + 128].rearrange("p (h k) -> p h k", h=4), 1.0 / 16.0, None, ALU.mult),
                                 reads=[lac], writes=[lapad])
                            gla_prep('C', tt, d, qpad[:].rearrange("p h k -> p (h k)"), kpad[:].rearrange("p h k -> p (h k)"),
                                     lapad[:].rearrange("p h k -> p (h k)"), [qpad], [kpad], [lapad], di == 0)

                    def chain_y():
                        p.op('dve', lambda e: e.tensor_scalar(bq[:], pT[:, 1280:1536], 0.125, None, ALU.mult), reads=RP, writes=[bq])
                        for ct in range(2):
                            transpose_b(ps_t[:, ct, :], bq[:, ct * 128:(ct + 1) * 128], [bq], [ps_t])
                        p.op('act', lambda e: e.copy(bqT[:], ps_t[:, 0:2, :]), reads=[ps_t], writes=[bqT])
                        p.dma('sp', BQT[:, tok].rearrange("(c p) t -> p c t", p=128), bqT[:], reads=[bqT], writes=[p.uk()])
                        p.op('dve', lambda e: e.tensor_copy(bq[:], pT[:, 1536:1792]), reads=RP + [bqT], writes=[bq])
                        for ct in range(2):
                            transpose_b(ps_t[:, 2 + ct, :], bq[:, ct * 128:(ct + 1) * 128], [bq], [ps_t])
                        p.op('act', lambda e: e.copy(bkT[:], ps_t[:, 2:4, :]), reads=[ps_t], writes=[bkT])
                        p.dma('sp', BKT[:, tok].rearrange("(c p) t -> p c t", p=128), bkT[:], reads=[bkT], writes=[p.uk()])
                        p.op('dve', lambda e: e.tensor_copy(bv[:], pT[:, 1792:2048]), reads=RP, writes=[bv])
                        p.dma('sp', BV[tok, :], bv[:], reads=[bv], writes=[p.uk()])
                        p.op('act', lambda e: e.activation(junk2[:, 0:192], pT[:, 2848:3040], AF.Square, accum_out=sx[:, 2:3]), reads=RP, writes=[junk2, sx])
                        p.op('act', lambda e: e.activation(junk2[:, 0:128], pT[:, 3040:3168], AF.Square, accum_out=sx[:, 3:4]), reads=RP, writes=[junk2, sx])
                        p.op('dve', lambda e: e.tensor_scalar(sx[:, 4:5], sx[:, 2:3], 1.0 / 192, EPS, ALU.mult, ALU.add), reads=[sx], writes=[sx])
                        rsqrt(sx[:, 4:5], [sx])
                        p.op('dve', lambda e: e.tensor_scalar(sx[:, 5:6], sx[:, 3:4], 1.0 / 128, EPS, ALU.mult, ALU.add), reads=[sx], writes=[sx])
                        rsqrt(sx[:, 5:6], [sx])
                        p.op('dve', lambda e: e.tensor_scalar(cqn[:], pT[:, 2848:3040], sx[:, 4:5], None, ALU.mult), reads=RP + [sx], writes=[cqn])
                        p.op('dve', lambda e: e.tensor_scalar(ckvn[:], pT[:, 3040:3168], sx[:, 5:6], None, ALU.mult), reads=RP + [sx], writes=[ckvn])
                        transpose_b(ps_t[:, 4, :], cqn[:, 0:128], [cqn], [ps_t])
                        transpose_b(ps_t[0:64, 5, :], cqn[:, 128:192], [cqn], [ps_t])
                        transpose_b(ps_t[:, 6, :], ckvn[:], [ckvn], [ps_t])
                        p.op('act', lambda e: e.copy(cqT[:, 0, :], ps_t[:, 4, :]), reads=[ps_t], writes=[cqT])
                        p.op('act', lambda e: e.copy(cqT[0:64, 1, :], ps_t[0:64, 5, :]), reads=[ps_t], writes=[cqT])
                        p.op('act', lambda e: e.copy(ckvT[:], ps_t[:, 6, :]), reads=[ps_t], writes=[ckvT])

                        def mmq(e):
                            e.matmul(ps_d[:, 0:384], cqT[:, 0, :], wuqb[:, 0, :], start=True, stop=False)
                            return e.matmul(ps_d[:, 0:384], cqT[0:64, 1, :], wuqb[0:64, 1, :], start=False, stop=True)
                        p.op('pe', mmq, reads=[cqT, wuqb], writes=[ps_d])
                        p.op('act', lambda e: e.activation(qd[:].rearrange("p h k -> p (h k)"), ps_d[:, 0:384], AF.Copy, scale=96.0 ** -0.5), reads=[ps_d], writes=[qd])
                        p.op('pe', lambda e: e.matmul(ps_d[:, 0:512], ckvT[:], wukvb[:], start=True, stop=True), reads=[ckvT, wukvb], writes=[ps_d])
                        p.op('act', lambda e: e.copy(kvd[:].rearrange("p h k -> p (h k)"), ps_d[:, 0:512]), reads=[ps_d], writes=[kvd])
                        cosb = ropet[:, 0:16].rearrange("p (a f) -> p a f", a=2)
                        sinb = ropet[:, 16:32].rearrange("p (a f) -> p a f", a=2)

                        p.op('dve', lambda e: e.tensor_copy(rin[:, 0:4, :], qd[:, :, 64:96]), reads=[qd], writes=[rin])
                        p.op('dve', lambda e: e.tensor_copy(rin[:, 4, :], pT[:, 3168:3200]), reads=RP + [rin], writes=[rin])
                        s5 = rin[:].rearrange("p h (a x f) -> p h a x f", a=2, x=2)
                        d5 = rout[:].rearrange("p h (a x f) -> p h a x f", a=2, x=2)
                        t5 = rtmp5[:].rearrange("p h (a x f) -> p h a x f", a=2, x=2)
                        u1 = s5[:, :, :, 0, :]; u2 = s5[:, :, :, 1, :]
                        cos5 = cosb.unsqueeze(1).to_broadcast([128, 5, 2, 8])
                        sin5 = sinb.unsqueeze(1).to_broadcast([128, 5, 2, 8])
                        p.op('dve', lambda e: e.tensor_mul(d5[:, :, :, 0, :], u1, cos5), reads=[rin, ropet], writes=[rout])
                        p.op('dve', lambda e: e.tensor_mul(t5[:, :, :, 0, :], u2, sin5), reads=[rin, ropet], writes=[rtmp5])
                        p.op('dve', lambda e: e.tensor_mul(d5[:, :, :, 1, :], u1, sin5), reads=[rin, ropet, rout], writes=[rout])
                        p.op('dve', lambda e: e.tensor_mul(t5[:, :, :, 1, :], u2, cos5), reads=[rin, ropet, rtmp5], writes=[rtmp5])
                        p.op('dve', lambda e: e.tensor_sub(d5[:, :, :, 0, :], d5[:, :, :, 0, :], t5[:, :, :, 0, :]), reads=[rout, rtmp5], writes=[rout])
                        p.op('dve', lambda e: e.tensor_add(d5[:, :, :, 1, :], d5[:, :, :, 1, :], t5[:, :, :, 1, :]), reads=[rout, rtmp5], writes=[rout])
                        p.op('dve', lambda e: e.tensor_copy(qdb[:, :, 0:64], qd[:, :, 0:64]), reads=[qd], writes=[qdb])
                        p.op('dve', lambda e: e.tensor_copy(qdb[:, :, 64:96], rout[:, 0:4, :]), reads=[rout, qdb], writes=[qdb])
                        p.op('dve', lambda e: e.tensor_copy(kdb[:, :, 0:64], kvd[:, :, 0:64]), reads=[kvd], writes=[kdb])
                        p.op('dve', lambda e: e.tensor_copy(kdb[:, :, 64:96], rout[:, 4:5, :].to_broadcast([128, 4, 32])), reads=[rout, kdb], writes=[kdb])
                        p.op('dve', lambda e: e.tensor_copy(vdb[:].rearrange("p (h k) -> p h k", h=4), kvd[:, :, 64:128]), reads=[kvd], writes=[vdb])
                        p.dma('sp', DV[tok, :], vdb[:], reads=[vdb], writes=[p.uk()])
                        for h in range(4):
                            transpose_b(ps_t[0:96, h, :], qdb[:, h, :], [qdb], [ps_t])
                        p.op('act', lambda e: e.copy(dT[:], ps_t[0:96, 0:4, :]), reads=[ps_t], writes=[dT])
                        p.dma('sp', DQT[:, :, tok].rearrange("h k t -> k h t"), dT[:], reads=[dT], writes=[p.uk()])
                        for h in range(4):
                            transpose_b(ps_t[0:96, 4 + h, :], kdb[:, h, :], [kdb], [ps_t])
                        p.op('act', lambda e: e.copy(dT[:], ps_t[0:96, 4:8, :]), reads=[ps_t], writes=[dT])
                        p.dma('sp', DKT[:, :, tok].rearrange("h k t -> k h t"), dT[:], reads=[dT], writes=[p.uk()])

                    if tt + 1 < NT:
                        p.interleave([lambda: front(tt + 1), chain_x, chain_y])
                    else:
                        p.interleave([chain_x, chain_y])

                front(0)
                for tt in range(NT):
                    back(tt)
                p.barrier()
            if dbg == 'P':
                break

            def normalize(ph_bufs, OT, LTp, n, dst_ap, dst_key):
                rl, on = ph_bufs
                p.op('dve', lambda e: e.reciprocal(rl[:, 0:n], LTp[:, 0:n]), reads=[LTp], writes=[rl])
                p.op('dve', lambda e: e.tensor_mul(on[:, 0:n], OT[:, 0:n], rl[:, 0:n]), reads=[OT, rl], writes=[on])
                p.dma('sp', dst_ap, on[:, 0:n], reads=[on], writes=[p.uk()])

            with ExitStack() as ph:
                GC = 4
                NG = NCH // GC
                LG = (L // 64) // GC
                masks = {}
                for d in 'fb':
                    masks[d] = p.sb(ph, [64, 64], F32)
                    p.dma('sp', masks[d][:], mask_d[d], writes=[masks[d]])
                T2 = [p.ps(ph, [64, 512]) for _ in range(2)]
                streams = []
                for si, (m, d) in enumerate((('A', 'f'), ('A', 'b'), ('C', 'f'), ('C', 'b'))):
                    st_ = dict(m=m, d=d, md=m + d)
                    for nm in ('qt', 'kt', 'qh', 'kh', 'v'):
                        st_[nm] = [p.sb(ph, [64, 4, 256], BF16) for _ in range(2)]
                    st_['dec'] = [p.sb(ph, [64, 4, 4], F32) for _ in range(2)]
                    st_['ob'] = [p.sb(ph, [64, 4, 256], F32) for _ in range(2)]
                    st_['at'] = p.sb(ph, [64, 256], BF16)
                    st_['S'] = p.sb(ph, [64, 4, 64], F32)
                    st_['Sb'] = p.sb(ph, [64, 4, 64], BF16)
                    st_['T1'] = p.ps(ph, [64, 512])
                    st_['pS'] = T2[si // 2][:, (si % 2) * 256:(si % 2) * 256 + 256]
                    st_['pSk'] = T2[si // 2]
                    if d == 'f':
                        st_['gorder'] = list(range(NG))
                    else:
                        st_['gorder'] = list(range(LG - 1, -1, -1)) + list(range(NG - 1, LG - 1, -1))
                    p.op('dve', lambda e: e.memset(st_['S'][:], 0.0), writes=[st_['S']])
                    p.op('dve', lambda e: e.memset(st_['Sb'][:], 0.0), writes=[st_['Sb']])
                    streams.append(st_)
                for gi in range(NG):
                    b2 = gi % 2
                    for st_ in streams:
                        m, d, md = st_['m'], st_['d'], st_['md']
                        g = st_['gorder'][gi]
                        tk = slice(g * 256, (g + 1) * 256)
                        p.dma('sp', st_['qt'][b2][:], mix[md + 'QT'][:, tk].rearrange("(h k) t -> k h t", k=64), reads=[md + 'QT'], writes=[st_['qt'][b2]])
                        p.dma('sp', st_['kt'][b2][:], mix[md + 'KT'][:, tk].rearrange("(h k) t -> k h t", k=64), reads=[md + 'KT'], writes=[st_['kt'][b2]])
                        p.dma('sp', st_['qh'][b2][:], mix[md + 'QH'][:, tk].rearrange("(h k) t -> k h t", k=64), reads=[md + 'QH'], writes=[st_['qh'][b2]])
                        p.dma('sp', st_['kh'][b2][:], mix[md + 'KH'][tk, :].rearrange("(c p) n -> p c n", p=64), reads=[md + 'KH'], writes=[st_['kh'][b2]])
                        p.dma('sp', st_['v'][b2][:], mix[m + 'V'][tk, :].rearrange("(c p) n -> p c n", p=64), reads=[m + 'V'], writes=[st_['v'][b2]])
                        p.dma('sp', st_['dec'][b2][:], mix[md + 'DEC'][:, g * 4:(g + 1) * 4].rearrange("(h k) t -> k h t", k=64), reads=[md + 'DEC'], writes=[st_['dec'][b2]])
                    for ci in range(GC):
                        for st_ in streams:
                            d = st_['d']
                            c = ci if d == 'f' else GC - 1 - ci
                            cs = slice(c * 64, (c + 1) * 64)
                            qt, kt, qh, kh, v, dec, ob = (st_[n][b2] for n in ('qt', 'kt', 'qh', 'kh', 'v', 'dec', 'ob'))
                            at, S, Sb, T1, pS, pSk = st_['at'], st_['S'], st_['Sb'], st_['T1'], st_['pS'], st_['pSk']
                            pa = T1[:, 0:256]; po = T1[:, 256:512]
                            ka = T1; ko = T1

                            def mmA(e):
                                for h in range(4):
                                    r = e.matmul(pa[:, h * 64:(h + 1) * 64], kt[:, h, cs], qt[:, h, cs], start=True, stop=True)
                                return r
                            p.op('pe', mmA, reads=[kt, qt], writes=[ka])
                            p.op('dve', lambda e: e.tensor_scalar(at[:], pa, 1e30, -1e30, ALU.min, ALU.max), reads=[ka], writes=[at])
                            p.op('dve', lambda e: e.tensor_mul(at[:].rearrange("p (h i) -> p h i", h=4), at[:].rearrange("p (h i) -> p h i", h=4),
                                                              masks[d][:].unsqueeze(1).to_broadcast([64, 4, 64])), reads=[at, masks[d]], writes=[at])

                            def mmO(e):
                                for h in range(4):
                                    e.matmul(po[:, h * 64:(h + 1) * 64], at[:, h * 64:(h + 1) * 64], v[:, c, h * 64:(h + 1) * 64], start=True, stop=False)
                                    r = e.matmul(po[:, h * 64:(h + 1) * 64], qh[:, h, cs], Sb[:, h, :], start=False, stop=True)
                                return r
                            p.op('pe', mmO, reads=[at, v, qh, Sb], writes=[ko])
                            p.op('act', lambda e: e.copy(ob[:, c, :], po), reads=[ko], writes=[ob])

                            def mmS(e):
                                for h in range(4):
                                    r = e.matmul(pS[:, h * 64:(h + 1) * 64], kh[:, c, h * 64:(h + 1) * 64], v[:, c, h * 64:(h + 1) * 64], start=True, stop=True)
                                return r
                            p.op('pe', mmS, reads=[kh, v], writes=[pSk])
                            p.op('dve', lambda e: e.tensor_mul(S[:], S[:], dec[:, :, c:c + 1].to_broadcast([64, 4, 64])), reads=[S, dec], writes=[S])
                            p.op('dve', lambda e: e.tensor_add(S[:], S[:], pS.rearrange("p (h v) -> p h v", h=4)), reads=[S, pSk], writes=[S])
                            p.op('act', lambda e: e.copy(Sb[:], S[:]), reads=[S], writes=[Sb])
                    for st_ in streams:
                        g = st_['gorder'][gi]
                        tk = slice(g * 256, (g + 1) * 256)
                        p.dma('pool', mix[st_['md'] + 'O'][tk, :].rearrange("(c p) n -> p c n", p=64), st_['ob'][b2][:], reads=[st_['ob'][b2]], writes=[p.uk()])
                p.barrier()
            if dbg == 'R':
                break
            with ExitStack() as ph:
                ngb = {}
                for m, src in (('A', hgrn_ng), ('C', gla_ng)):
                    ngb[m] = p.sb(ph, [128, 256], F32)
                    p.dma('sp', ngb[m][:], src[layer:layer + 1, :].to_broadcast([128, 256]), writes=[ngb[m]])
                def ro_stream(m, roff):
                    of = [p.sb(ph, [128, 256], F32) for _ in range(2)]
                    ob_ = [p.sb(ph, [128, 256], F32) for _ in range(2)]
                    gg = [p.sb(ph, [128, 256], F32) for _ in range(2)]
                    sq = p.sb(ph, [128, 256], F32)
                    ss = p.sb(ph, [128, 4], F32)
                    yb = p.sb(ph, [128, 256], BF16)
                    yT = p.sb(ph, [128, 2, 128], BF16)
                    pst = p.ps(ph, [128, 2, 128], BF16)

                    def run():
                        it = 0
                        for tt in range(T0, NT):
                            tok = slice(tt * 128, (tt + 1) * 128)
                            a = of[it % 2]; b2 = ob_[it % 2]; g = gg[it % 2]
                            it += 1
                            p.dma('sp', a[:], mix[m + 'fO'][tok, :], reads=[m + 'fO'], writes=[a])
                            p.dma('sp', b2[:], mix[m + 'bO'][tok, :], reads=[m + 'bO'], writes=[b2])
                            p.dma('sp', g[:], mix[m + 'G'][tok, :], reads=[m + 'G'], writes=[g])
                            p.op('dve', lambda e: e.tensor_add(a[:], a[:], b2[:]), reads=[a, b2], writes=[a])
                            p.op('dve', lambda e: e.tensor_mul(sq[:], a[:], a[:]), reads=[a], writes=[sq])
                            p.op('dve', lambda e: e.tensor_reduce(ss[:], sq[:].rearrange("p (h v) -> p h v", h=4), AX.X, ALU.add), reads=[sq], writes=[ss])
                            p.op('dve', lambda e: e.tensor_scalar(ss[:], ss[:], 1.0 / 64, EPS, ALU.mult, ALU.add), reads=[ss], writes=[ss])
                            rsqrt(ss[:], [ss])
                            p.op('dve', lambda e: e.tensor_mul(a[:].rearrange("p (h v) -> p h v", h=4), a[:].rearrange("p (h v) -> p h v", h=4),
                                                              ss[:].unsqueeze(2).to_broadcast([128, 4, 64])), reads=[a, ss], writes=[a])
                            p.op('dve', lambda e: e.tensor_mul(a[:], a[:], ngb[m][:]), reads=[a, ngb[m]], writes=[a])
                            p.op('dve', lambda e: e.tensor_mul(yb[:], a[:], g[:]), reads=[a, g], writes=[yb])

                            def tr(e):
                                for ct in range(2):
                                    r = e.transpose(pst[:, ct, :], yb[:, ct * 128:(ct + 1) * 128], ident_b[:])
                                return r
                            p.op('pe', tr, reads=[yb, ident_b], writes=[pst])
                            p.op('act', lambda e: e.copy(yT[:], pst[:]), reads=[pst], writes=[yT])
                            p.dma('sp', CATT[roff:roff + 256, tok].rearrange("(c p) t -> p c t", p=128), yT[:], reads=[yT], writes=[p.uk()])
                    return run
                p.interleave([ro_stream('A', 0), ro_stream('C', 512)])
                p.barrier()

            with ExitStack() as ph:
                KTn = p.sb(ph, [64, 4, T], BF16)
                QTn = p.sb(ph, [64, 4, T], BF16)
                p.dma('sp', KTn[:], BKT.rearrange("(h d) t -> d h t", d=64), reads=['BKT'], writes=[KTn])
                p.dma('sp', QTn[:], BQT.rearrange("(h d) t -> d h t", d=64), reads=['BQT'], writes=[QTn])
                V1l = p.sb(ph, [64, ROWS, 256], BF16)
                V1c = p.sb(ph, [128, LT, 256], BF16)
                for r8 in range(0, ROWS, 8):
                    p.dma('sp', V1l[:, r8:r8 + 8, :], BV[L + r8 * 64:L + (r8 + 8) * 64, :].rearrange("(r w) c -> w r c", w=64), reads=['BV'], writes=[V1l])
                p.dma('sp', V1c[:], BV[0:L, :].rearrange("(n p) c -> p n c", p=128), reads=['BV'], writes=[V1c])
                EBs = [p.sb(ph, [64, 8, 512], F32) for _ in range(2)]
                psS = [p.ps(ph, [64, 512]) for _ in range(2)]
                psC = [p.ps(ph, [128, 512]) for _ in range(2)]
                OTp = p.ps(ph, [64, 512])
                LTp = p.ps(ph, [64, 512])
                nb_ = (p.sb(ph, [64, 512], F32), p.sb(ph, [64, 512], BF16))
                pe_ = [p.sb(ph, [64, 512], F32) for _ in range(2)]
                pb16 = [p.sb(ph, [64, 512], BF16) for _ in range(2)]
                pc16 = [p.sb(ph, [128, 512], BF16) for _ in range(2)]
                rows_ = [(h, r) for h in range(4) for r in range(ROWS)]

                def na_front(i):
                    h, r = rows_[i]
                    EB = EBs[h % 2]
                    if r == 0:
                        p.dma('sp', EB[:], na_tab[layer, h].rearrange("c w x -> w c x"), writes=[EB])
                        p.op('act', lambda e: e.activation(EB[:], EB[:], AF.Exp), reads=[EB], writes=[EB])
                    rs = min(max(r - 4, 0), ROWS - 8)
                    cls = r - rs
                    qc = slice(L + r * 64, L + (r + 1) * 64)
                    ps = psS[i % 2]; pc = psC[i % 2]; pe = pe_[i % 2]; pbb = pb16[i % 2]; pcs = pc16[i % 2]

                    def mmS(e):
                        for j in range(8):
                            kc = slice(L + (rs + j) * 64, L + (rs + j + 1) * 64)
                            r_ = e.matmul(ps[:, j * 64:(j + 1) * 64], KTn[:, h, kc], QTn[:, h, qc], start=True, stop=True)
                        for kt in range(LT):
                            r_ = e.matmul(pc[:, kt * 64:(kt + 1) * 64], KTn[:, h, kt * 128:(kt + 1) * 128], QTn[:, h, qc], start=True, stop=True)
                        return r_
                    p.op('pe', mmS, reads=[KTn, QTn], writes=[ps, pc])
                    p.op('act', lambda e: e.activation(pe[:], ps[:], AF.Exp), reads=[ps], writes=[pe])
                    p.op('act', lambda e: e.activation(pcs[:, 0:LT * 64], pc[:, 0:LT * 64], AF.Exp), reads=[pc], writes=[pcs])
                    p.op('dve', lambda e: e.tensor_mul(pbb[:], pe[:], EB[:, cls, :]), reads=[pe, EB], writes=[pbb])

                def na_back(i):
                    h, r = rows_[i]
                    hv = slice(h * 64, (h + 1) * 64)
                    rs = min(max(r - 4, 0), ROWS - 8)
                    ri = r % 8
                    r0 = r - ri
                    pbb = pb16[i % 2]; pcs = pc16[i % 2]

                    def mmV(e):
                        for j in range(8):
                            e.matmul(OTp[:, ri * 64:(ri + 1) * 64], V1l[:, rs + j, hv], pbb[:, j * 64:(j + 1) * 64], start=(j == 0), stop=False)
                        for kt in range(LT):
                            e.matmul(OTp[:, ri * 64:(ri + 1) * 64], V1c[:, kt, hv], pcs[:, kt * 64:(kt + 1) * 64], start=False, stop=(kt == LT - 1))
                        for j in range(8):
                            e.matmul(LTp[:, ri * 64:(ri + 1) * 64], ones_b[0:64, 0:64], pbb[:, j * 64:(j + 1) * 64], start=(j == 0), stop=False)
                        for kt in range(LT):
                            r_ = e.matmul(LTp[:, ri * 64:(ri + 1) * 64], ones_b[:, 0:64], pcs[:, kt * 64:(kt + 1) * 64], start=False, stop=(kt == LT - 1))
                        return r_
                    p.op('pe', mmV, reads=[V1l, V1c, pbb, pcs, ones_b], writes=[OTp, LTp])
                    if ri == 7:
                        normalize(nb_, OTp, LTp, 512, CATT[256 + h * 64:256 + (h + 1) * 64, L + r0 * 64:L + r0 * 64 + 512], 'CATT')


                def na_ctx(h):
                    hv = slice(h * 64, (h + 1) * 64)
                    for kt in range(LT):
                        pc = psC[kt % 2]; pcs = pc16[kt % 2]
                        p.op('pe', lambda e: e.matmul(pc[:, 0:L], KTn[:, h, kt * 128:(kt + 1) * 128], QTn[:, h, 0:L], start=True, stop=True),
                             reads=[KTn, QTn], writes=[pc])
                        p.op('act', lambda e: e.activation(pcs[:, 0:L], pc[:, 0:L], AF.Exp), reads=[pc], writes=[pcs])
                        p.op('pe', lambda e: e.matmul(OTp[:, 0:L], V1c[:, kt, hv], pcs[:, 0:L], start=(kt == 0), stop=(kt == LT - 1)),
                             reads=[V1c, pcs], writes=[OTp])
                        p.op('pe', lambda e: e.matmul(LTp[:, 0:L], ones_b[:, 0:64], pcs[:, 0:L], start=(kt == 0), stop=(kt == LT - 1)),
                             reads=[ones_b, pcs], writes=[LTp])
                    normalize(nb_, OTp, LTp, L, CATT[256 + h * 64:256 + (h + 1) * 64, 0:L], 'CATT')

                if keep_ctx:
                    for h in range(4):
                        na_ctx(h)
                na_front(0)
                for i in range(len(rows_)):
                    if i + 1 < len(rows_):
                        na_front(i + 1)
                    na_back(i)
                p.barrier()

            with ExitStack() as ph:
                KTd = p.sb(ph, [96, 4, T], BF16)
                p.dma('sp', KTd[:], DKT.rearrange("h k t -> k h t"), reads=['DKT'], writes=[KTd])
                V1 = p.sb(ph, [128, NT, 256], BF16)
                for n8 in range(0, NT, 8):
                    n9 = min(NT, n8 + 8)
                    p.dma('sp', V1[:, n8:n9, :], DV[n8 * 128:n9 * 128, :].rearrange("(n p) c -> p n c", p=128), reads=['DV'], writes=[V1])
                QTt = [p.sb(ph, [96, 512], BF16) for _ in range(2)]
                psS = [p.ps(ph, [128, 2, 512]) for _ in range(3)]
                OTp = p.ps(ph, [64, 512])
                LTp = p.ps(ph, [64, 512])
                nb_ = (p.sb(ph, [64, 512], F32), p.sb(ph, [64, 512], BF16))
                pe16 = [p.sb(ph, [128, 2, 512], BF16) for _ in range(3)]
                steps = []
                qn = 0
                for h in range(4):
                    qtiles = []
                    if keep_ctx:
                        qtiles.append((0, L, list(range(LT))))
                    for q0 in range(L, T, 512):
                        qtiles.append((q0, min(512, T - q0), list(range(NT))))
                    for (q0, nq, kts) in qtiles:
                        pairs = [kts[i:i + 2] for i in range(0, len(kts), 2)]
                        for ki, kp in enumerate(pairs):
                            steps.append((h, q0, nq, kp, ki == 0, ki == len(pairs) - 1, qn))
                        qn += 1

                def mla_front(i):
                    h, q0, nq, kp, first, last, qi = steps[i]
                    qb = QTt[qi % 2]
                    if first:
                        p.dma('sp', qb[:, 0:nq], DQT[h, :, q0:q0 + nq], reads=['DQT'], writes=[qb])
                    ps = psS[i % 3]; pe = pe16[i % 3]
                    nk = len(kp)

                    def mms(e):
                        for j, kt in enumerate(kp):
                            r = e.matmul(ps[:, j, 0:nq], KTd[:, h, kt * 128:(kt + 1) * 128], qb[:, 0:nq], start=True, stop=True)
                        return r
                    p.op('pe', mms, reads=[KTd, qb], writes=[ps])
                    p.op('act', lambda e: e.activation(pe[:, 0:nk, 0:nq], ps[:, 0:nk, 0:nq], AF.Exp), reads=[ps], writes=[pe])

                def mla_back(i):
                    h, q0, nq, kp, first, last, qi = steps[i]
                    pe = pe16[i % 3]
                    nk = len(kp)

                    def mmv(e):
                        for j, kt in enumerate(kp):
                            e.matmul(OTp[:, 0:nq], V1[:, kt, h * 64:(h + 1) * 64], pe[:, j, 0:nq], start=(first and j == 0), stop=(last and j == nk - 1))
                        for j, kt in enumerate(kp):
                            r = e.matmul(LTp[:, 0:nq], ones_b[:, 0:64], pe[:, j, 0:nq], start=(first and j == 0), stop=(last and j == nk - 1))
                        return r
                    p.op('pe', mmv, reads=[V1, pe, ones_b], writes=[OTp, LTp])
                    if last:
                        normalize(nb_, OTp, LTp, nq, CATT[768 + h * 64:768 + (h + 1) * 64, q0:q0 + nq], 'CATT')

                LOOK = 2
                for i in range(min(LOOK, len(steps))):
                    mla_front(i)
                for i in range(len(steps)):
                    if i + LOOK < len(steps):
                        mla_front(i + LOOK)
                    mla_back(i)
                p.barrier()
            if dbg == 'M':
                break

            W01 = p.sb(lay, [128, NT, 2], F32)
            DESTi = p.sb(lay, [128, NT, 2], I32)
            IDXGU = p.sb(lay, [128, NBLK, 8], I32)
            IDXDN = p.sb(lay, [128, NBLK, 2], I32)
            with ExitStack() as ph:
                woutb = p.sb(ph, [128, 8, D], BF16)
                for kc in range(8):
                    p.dma('pool', woutb[:, kc, :], w_out[layer, kc * 128:(kc + 1) * 128, :], writes=[(woutb.name, kc)])
                RW = [(woutb.name, kc) for kc in range(8)]
                wr = p.sb(ph, [128, 8, 36], F32)
                p.dma('sp', wr[:], w_r[layer].rearrange("(k p) n -> p k n", p=128), writes=[wr])
                brb = p.sb(ph, [128, 36], F32)
                p.dma('sp', brb[:], b_r[layer:layer + 1, :].to_broadcast([128, 36]), writes=[brb])
                g2 = p.sb(ph, [128, D], F32)
                p.dma('sp', g2[:], norm2_g[layer:layer + 1, :].to_broadcast([128, D]), writes=[g2])
                M2 = [p.sb(ph, [128, D], F32) for _ in range(2)]
                G2 = [p.sb(ph, [128, D], F32) for _ in range(2)]
                B2 = [p.sb(ph, [128, D], F32) for _ in range(2)]
                for s in range(2):
                    p.dma('sp', M2[s][:], MOD[layer, s:s + 1, 2 * D:3 * D].to_broadcast([128, D]), writes=[M2[s]])
                    p.dma('sp', B2[s][:], MOD[layer, s:s + 1, 3 * D:4 * D].to_broadcast([128, D]), writes=[B2[s]])
                    p.dma('sp', G2[s][:], MOD[layer, s:s + 1, 4 * D:5 * D].to_broadcast([128, D]), writes=[G2[s]])
                    p.op('dve', lambda e: e.scalar_tensor_tensor(G2[s][:], G2[s][:], 1.0, g2[:], ALU.add, ALU.mult), reads=[G2[s], g2], writes=[G2[s]])
                M0 = p.sb(ph, [128, NT, 32], F32)
                M1 = p.sb(ph, [128, NT, 32], F32)
                Mh = p.sb(ph, [128, NT, 32], BF16)
                p.op('dve', lambda e: e.memset(M0[:], 0.0), writes=[M0])
                p.op('dve', lambda e: e.memset(M1[:], 0.0), writes=[M1])
                p.op('dve', lambda e: e.memset(Mh[:], 0.0), writes=[Mh])
                p.op('dve', lambda e: e.memset(W01[:], 0.0), writes=[W01])
                Bs = []
                for si in range(2):
                    Bs.append(dict(cb=p.sb(ph, [128, 8, 128], BF16), xb=p.sb(ph, [128, D], F32), tmp=p.sb(ph, [128, D], F32), xn=p.sb(ph, [128, D], F32),
                                   h2=p.sb(ph, [128, D], F32), h2b=p.sb(ph, [128, D], BF16), h2T=p.sb(ph, [128, 8, 128], F32), sx=p.sb(ph, [128, 16], F32),
                                   lg=p.sb(ph, [128, 36], F32), r8=p.sb(ph, [128, 4, 8], F32), sel=p.sb(ph, [128, 8], F32), sel2=p.sb(ph, [128, 8], F32),
                                   oh=p.sb(ph, [128, 3, 8], F32), po=p.ps(ph, [128, 512]), pt=p.ps(ph, [128, 4, 128]), pl=p.ps(ph, [128, 64])))
                pl = Bs[0]['pl']

                def o_stream(si):
                    def run():
                        for tt in range(T0 + si, NT, 2):
                            s = 1 if tt < LT else 0
                            tok = slice(tt * 128, (tt + 1) * 128)
                            cb, xb, tmp, xn, h2, h2b, h2T, sx, lg, r8, sel, sel2, oh, po, pt, pl = (Bs[si][k] for k in ('cb', 'xb', 'tmp', 'xn', 'h2', 'h2b', 'h2T', 'sx', 'lg', 'r8', 'sel', 'sel2', 'oh', 'po', 'pt', 'pl'))
                            p.dma('sp', cb[:], CATT[:, tok].rearrange("(c p) t -> p c t", p=128), reads=['CATT'], writes=[cb])
                            p.dma('sp', xb[:], X[tok, :], reads=[('X', tt)], writes=[xb])

                            for nb in range(2):
                                def mm(e):
                                    for c in range(8):
                                        r = e.matmul(po[:], cb[:, c, :], woutb[:, c, nb * 512:(nb + 1) * 512], start=(c == 0), stop=(c == 7))
                                    return r
                                p.op('pe', mm, reads=[cb] + RW, writes=[po])
                                p.op('dve', lambda e: e.tensor_mul(tmp[:, nb * 512:(nb + 1) * 512], po[:], M2[s][:, nb * 512:(nb + 1) * 512]), reads=[po, M2[s]], writes=[tmp])
                            p.op('dve', lambda e: e.tensor_add(xn[:], xb[:], tmp[:]), reads=[xb, tmp], writes=[xn])
                            p.dma('sp', X[tok, :], xn[:], reads=[xn], writes=[('X', tt)])
                            p.op('act', lambda e: e.activation(tmp[:], xn[:], AF.Square, accum_out=sx[:, 0:1]), reads=[xn], writes=[tmp, sx])
                            p.op('dve', lambda e: e.tensor_scalar(sx[:, 1:2], sx[:, 0:1], 1.0 / D, EPS, ALU.mult, ALU.add), reads=[sx], writes=[sx])
                            rsqrt(sx[:, 1:2], [sx])
                            p.op('dve', lambda e: e.scalar_tensor_tensor(tmp[:], xn[:], sx[:, 1:2], G2[s][:], ALU.mult, ALU.mult), reads=[xn, sx, G2[s]], writes=[tmp])
                            p.op('dve', lambda e: e.tensor_add(h2[:], tmp[:], B2[s][:]), reads=[tmp, B2[s]], writes=[h2])
                            p.op('act', lambda e: e.copy(h2b[:], h2[:]), reads=[h2], writes=[h2b])
                            p.dma('sp', H2[tok, :], h2b[:], reads=[h2b], writes=['H2'])
                            for rnd in range(2):
                                def tr(e):
                                    for k4 in range(4):
                                        kc = rnd * 4 + k4
                                        r = e.transpose(pt[:, k4, :], h2[:, kc * 128:(kc + 1) * 128], ident_f[:])
                                    return r
                                p.op('pe', tr, reads=[h2, ident_f], writes=[pt])
                                p.op('act', lambda e: e.copy(h2T[:, rnd * 4:(rnd + 1) * 4, :], pt[:]), reads=[pt], writes=[h2T])

                            def mmr(e):
                                for kc in range(8):
                                    r = e.matmul(pl[:, 0:36], h2T[:, kc, :], wr[:, kc, :], start=(kc == 0), stop=(kc == 7))
                                return r
                            p.op('pe', mmr, reads=[h2T, wr], writes=[pl])
                            p.op('dve', lambda e: e.tensor_add(lg[:], pl[:, 0:36], brb[:]), reads=[pl, brb], writes=[lg])
                            K_ = [sx, lg, r8, sel, sel2, oh]
                            p.op('dve', lambda e: e.tensor_reduce(sx[:, 2:3], lg[:, 0:4], AX.X, ALU.max), reads=K_, writes=[sx])
                            p.op('dve', lambda e: e.tensor_scalar(oh[:, 0, 0:4], lg[:, 0:4], sx[:, 2:3], None, ALU.is_equal), reads=K_, writes=[oh])
                            p.op('dve', lambda e: e.tensor_scalar(sx[:, 3:4], sx[:, 2:3], -1.0, None, ALU.mult), reads=K_, writes=[sx])
                            p.op('act', lambda e: e.activation(sel2[:, 0:4], lg[:, 0:4], AF.Exp, bias=sx[:, 3:4], accum_out=sx[:, 4:5]), reads=K_, writes=[sel2, sx])
                            p.op('dve', lambda e: e.reciprocal(sx[:, 5:6], sx[:, 4:5]), reads=K_, writes=[sx])
                            p.op('dve', lambda e: e.tensor_mul(r8[:], lg[:, 4:36].rearrange("p (g j) -> p g j", g=4), oh[:, 0, 0:4].unsqueeze(2).to_broadcast([128, 4, 8])), reads=K_, writes=[r8])
                            p.op('dve', lambda e: e.tensor_reduce(sel[:], r8[:].rearrange("p g j -> p j g"), AX.X, ALU.add), reads=K_, writes=[sel])
                            p.op('dve', lambda e: e.tensor_reduce(sx[:, 6:7], sel[:], AX.X, ALU.max), reads=K_, writes=[sx])
                            p.op('dve', lambda e: e.tensor_scalar(oh[:, 1, :], sel[:], sx[:, 6:7], None, ALU.is_equal), reads=K_, writes=[oh])
                            p.op('dve', lambda e: e.scalar_tensor_tensor(sel2[:], oh[:, 1, :], -1e30, sel[:], ALU.mult, ALU.add), reads=K_, writes=[sel2])
                            p.op('dve', lambda e: e.tensor_reduce(sx[:, 7:8], sel2[:], AX.X, ALU.max), reads=K_, writes=[sx])
                            p.op('dve', lambda e: e.tensor_scalar(oh[:, 2, :], sel2[:], sx[:, 7:8], None, ALU.is_equal), reads=K_, writes=[oh])
                            p.op('dve', lambda e: e.tensor_sub(sx[:, 8:9], sx[:, 6:7], sx[:, 7:8]), reads=K_, writes=[sx])
                            p.op('act', lambda e: e.activation(sx[:, 9:10], sx[:, 8:9], AF.Sigmoid), reads=K_, writes=[sx])
                            p.op('dve', lambda e: e.tensor_mul(W01[:, tt, 0:1], sx[:, 9:10], sx[:, 5:6]), reads=K_, writes=[W01])
                            p.op('dve', lambda e: e.tensor_sub(W01[:, tt, 1:2], sx[:, 5:6], W01[:, tt, 0:1]), reads=K_ + [W01], writes=[W01])
                            for k_, Mk in ((1, M0), (2, M1)):
                                p.op('dve', lambda e: e.tensor_mul(Mk[:, tt, :].rearrange("p (g j) -> p g j", g=4), oh[:, 0, 0:4].unsqueeze(2).to_broadcast([128, 4, 8]),
                                                                  oh[:, k_, :].unsqueeze(1).to_broadcast([128, 4, 8])), reads=K_, writes=[Mk])
                            p.op('dve', lambda e: e.tensor_add(Mh[:, tt, :], M0[:, tt, :], M1[:, tt, :]), reads=[M0, M1], writes=[Mh])
                    return run
                p.interleave([o_stream(0), o_stream(1)])
                lsb = p.sb(ph, [128, 128], BF16)
                lsf = p.sb(ph, [128, 128], F32)
                p.dma('sp', lsf[:], lstrict_d, writes=[lsf])
                p.op('dve', lambda e: e.tensor_copy(lsb[:], lsf[:]), reads=[lsf], writes=[lsb])
                carry = p.sb(ph, [128, 32], F32)
                RANK = p.sb(ph, [128, NT, 32], F32)
                p.op('dve', lambda e: e.memset(carry[:], 0.0), writes=[carry])
                p.op('dve', lambda e: e.memset(RANK[:], 0.0), writes=[RANK])
                for tt in range(T0, NT):
                    def mmk(e):
                        e.matmul(pl[:, 0:32], lsb[:], Mh[:, tt, :], start=True, stop=True)
                        return e.matmul(pl[:, 32:64], ones_b[:], Mh[:, tt, :], start=True, stop=True)
                    p.op('pe', mmk, reads=[lsb, ones_b, Mh], writes=[pl])
                    p.op('dve', lambda e: e.tensor_add(RANK[:, tt, :], pl[:, 0:32], carry[:]), reads=[pl, carry], writes=[RANK])
                    p.op('dve', lambda e: e.tensor_add(carry[:], carry[:], pl[:, 32:64]), reads=[pl, carry], writes=[carry])
                NK = (2 * T) // MOEB + 2
                thr = p.sb(ph, [128, NK], F32)
                p.op('pool', lambda e: e.iota(thr[:], [[MOEB, NK]], base=0, channel_multiplier=0, allow_small_or_imprecise_dtypes=True), writes=[thr])
                cmp_ = p.sb(ph, [128, 32, NK], F32)
                padded = p.sb(ph, [128, 32], F32)
                pend = [p.sb(ph, [128, 32], F32) for _ in range(2)]
                p.op('dve', lambda e: e.tensor_tensor(cmp_[:], carry[:].unsqueeze(2).to_broadcast([128, 32, NK]), thr[:].unsqueeze(1).to_broadcast([128, 32, NK]), ALU.is_gt),
                     reads=[carry, thr], writes=[cmp_])
                p.op('dve', lambda e: e.tensor_reduce(padded[:], cmp_[:], AX.X, ALU.add), reads=[cmp_], writes=[padded])
                p.op('dve', lambda e: e.tensor_scalar(padded[:], padded[:], float(MOEB), None, ALU.mult), reads=[padded], writes=[padded])
                p.op('dve', lambda e: e.tensor_copy(pend[0][:], padded[:]), reads=[padded], writes=[pend[0]])
                cur = 0
                for sft in (1, 2, 4, 8, 16):
                    a = pend[cur]; b2 = pend[1 - cur]
                    p.op('dve', lambda e: e.tensor_copy(b2[:, 0:sft], a[:, 0:sft]), reads=[a], writes=[b2])
                    p.op('dve', lambda e: e.tensor_add(b2[:, sft:32], a[:, sft:32], a[:, 0:32 - sft]), reads=[a, b2], writes=[b2])
                    cur = 1 - cur
                pe_ = pend[cur]
                pstart = pend[1 - cur]
                p.op('dve', lambda e: e.tensor_sub(pstart[:], pe_[:], padded[:]), reads=[pe_, padded], writes=[pstart])
                destf = p.sb(ph, [128, NT, 2], F32)
                big = p.sb(ph, [128, NT, 32], F32)
                p.op('dve', lambda e: e.tensor_add(RANK[:], RANK[:], pstart[:].unsqueeze(1).to_broadcast([128, NT, 32])), reads=[RANK, pstart], writes=[RANK])
                for k_, Mk in ((0, M0), (1, M1)):
                    p.op('dve', lambda e: e.tensor_mul(big[:], RANK[:], Mk[:]), reads=[RANK, Mk], writes=[big])
                    p.op('dve', lambda e: e.tensor_reduce(destf[:, :, k_], big[:], AX.X, ALU.add), reads=[big], writes=[destf])
                p.op('dve', lambda e: e.tensor_copy(DESTi[:], destf[:]), reads=[destf], writes=[DESTi])
                bvals = p.sb(ph, [128, NBLK], F32)
                p.op('pool', lambda e: e.iota(bvals[:], [[MOEB, NBLK]], base=0, channel_multiplier=0, allow_small_or_imprecise_dtypes=True), writes=[bvals])
                cmpb = p.sb(ph, [128, NBLK, 32], F32)
                bex = p.sb(ph, [128, NBLK], F32)
                p.op('dve', lambda e: e.tensor_tensor(cmpb[:], pe_[:].unsqueeze(1).to_broadcast([128, NBLK, 32]), bvals[:].unsqueeze(2).to_broadcast([128, NBLK, 32]), ALU.is_le),
                     reads=[pe_, bvals], writes=[cmpb])
                p.op('dve', lambda e: e.tensor_reduce(bex[:], cmpb[:], AX.X, ALU.add), reads=[cmpb], writes=[bex])
                p.op('dve', lambda e: e.tensor_scalar(bex[:], bex[:], float(NEXP - 1), None, ALU.min), reads=[bex], writes=[bex])
                pcol = p.sb(ph, [128, 1], F32)
                p.dma('sp', pcol[:], pcol_d, writes=[pcol])
                idxf = p.sb(ph, [128, NBLK, 8], F32)
                base = p.sb(ph, [128, NBLK], F32)
                p.op('dve', lambda e: e.tensor_scalar(base[:], bex[:], float(D), float(layer * NEXP * D), ALU.mult, ALU.add), reads=[bex], writes=[base])
                for kc in range(8):
                    p.op('dve', lambda e: e.tensor_scalar(idxf[:, :, kc], base[:], pcol[:, 0:1], float(kc * 128), ALU.add, ALU.add), reads=[base, pcol], writes=[idxf])
                p.op('dve', lambda e: e.tensor_copy(IDXGU[:], idxf[:]), reads=[idxf], writes=[IDXGU])
                p.op('dve', lambda e: e.tensor_scalar(base[:], bex[:], 256.0, float(layer * NEXP * 256), ALU.mult, ALU.add), reads=[bex], writes=[base])
                for fc in range(2):
                    p.op('dve', lambda e: e.tensor_scalar(idxf[:, :, fc], base[:], pcol[:, 0:1], float(fc * 128), ALU.add, ALU.add), reads=[base, pcol, IDXGU], writes=[idxf])
                p.op('dve', lambda e: e.tensor_copy(IDXDN[:], idxf[:, :, 0:2]), reads=[idxf], writes=[IDXDN])
                zt = p.sb(ph, [128, 8, D], BF16)
                p.op('dve', lambda e: e.memset(zt[:], 0.0), writes=[zt])
                for s0 in range(0, NSLOT, 1024):
                    n_ = min(1024, NSLOT - s0) // 128
                    p.dma('sp', XS[s0:s0 + n_ * 128, :].rearrange("(n p) d -> p n d", p=128), zt[:, 0:n_, :], reads=[zt], writes=['XS'])
                hd = [p.sb(ph, [128, D], BF16) for _ in range(2)]
                for tt in range(T0, NT):
                    hb_ = hd[tt % 2]
                    p.dma('sp', hb_[:], H2[tt * 128:(tt + 1) * 128, :], reads=['H2'], writes=[hb_])
                    for k_ in range(2):
                        p.idma(XS[:, :], bass.IndirectOffsetOnAxis(ap=DESTi[:, tt, k_:k_ + 1], axis=0), hb_[:], None,
                               reads=[hb_, DESTi, 'XS'], writes=[p.uk()])
                p.barrier()
            if dbg == 'O':
                break

            with ExitStack() as ph:
                sets = []
                for si in range(2):
                    B_ = dict(
                        wf=p.sb(ph, [128, 8, 512], F32), df=p.sb(ph, [128, 2, D], F32),
                        wgub=p.sb(ph, [128, 8, 512], BF16), wdnb=p.sb(ph, [128, 2, D], BF16),
                        xb=p.sb(ph, [128, 2, D], BF16), xsT=p.sb(ph, [128, 8, 256], BF16),
                        sg=p.sb(ph, [128, 2, 256], F32), hT=p.sb(ph, [128, 2, 256], BF16),
                        ysb=[p.sb(ph, [128, D], F32) for _ in range(2)],
                        pg=p.ps(ph, [128, 2, 256]), py=p.ps(ph, [128, D]), pst=p.ps(ph, [128, 8, 128], BF16))
                    sets.append(B_)

                def blk(b, B_):
                    wf, df, wgub, wdnb, xb, xsT, sg, hT, ysb, pg, py, pst = (B_[k] for k in ('wf', 'df', 'wgub', 'wdnb', 'xb', 'xsT', 'sg', 'hT', 'ysb', 'pg', 'py', 'pst'))
                    for kc in range(8):
                        p.idma(wf[:, kc, :], None, w_gu[:, :], bass.IndirectOffsetOnAxis(ap=IDXGU[:, b, kc:kc + 1], axis=0), reads=[IDXGU], writes=[(wf.name, kc)])
                    for fc in range(2):
                        p.idma(df[:, fc, :], None, w_dn[:, :], bass.IndirectOffsetOnAxis(ap=IDXDN[:, b, fc:fc + 1], axis=0), reads=[IDXDN], writes=[(df.name, fc)])
                    s0 = b * MOEB
                    p.dma('sp', xb[:], XS[s0:s0 + 256, :].rearrange("(s p) d -> p s d", p=128), reads=['XS'], writes=[xb])
                    p.op('act', lambda e: e.copy(wgub[:, 0:4, :], wf[:, 0:4, :]), reads=[(wf.name, kc) for kc in range(4)], writes=[(wgub.name, 0)])
                    p.op('dve', lambda e: e.tensor_copy(wgub[:, 4:8, :], wf[:, 4:8, :]), reads=[(wf.name, kc) for kc in range(4, 8)], writes=[(wgub.name, 1)])
                    p.op('act', lambda e: e.copy(wdnb[:, 0, :], df[:, 0, :]), reads=[(df.name, 0)], writes=[(wdnb.name, 0)])
                    p.op('dve', lambda e: e.tensor_copy(wdnb[:, 1, :], df[:, 1, :]), reads=[(df.name, 1)], writes=[(wdnb.name, 1)])
                    for s in range(2):
                        def tr(e):
                            for kc in range(8):
                                r = e.transpose(pst[:, kc, :], xb[:, s, kc * 128:(kc + 1) * 128], ident_b[:])
                            return r
                        p.op('pe', tr, reads=[xb, ident_b], writes=[pst])
                        p.op('act', lambda e: e.copy(xsT[:, :, s * 128:(s + 1) * 128], pst[:]), reads=[pst], writes=[xsT])
                    for half in range(2):
                        def mmg(e):
                            for n in range(2):
                                for kc in range(8):
                                    c0 = (half * 2 + n) * 128
                                    r = e.matmul(pg[:, n, :], wgub[:, kc, c0:c0 + 128], xsT[:, kc, :], start=(kc == 0), stop=(kc == 7))
                            return r
                        p.op('pe', mmg, reads=[(wgub.name, 0), (wgub.name, 1), xsT], writes=[pg])
                        if half == 0:
                            p.op('act', lambda e: e.activation(sg[:], pg[:], AF.Silu), reads=[pg], writes=[sg])
                        else:
                            p.op('dve', lambda e: e.tensor_mul(hT[:], sg[:], pg[:]), reads=[sg, pg], writes=[hT])
                    for s in range(2):
                        yb_ = ysb[s]

                        def mmy(e):
                            for nb in range(2):
                                for fc in range(2):
                                    r = e.matmul(py[:, nb * 512:(nb + 1) * 512], hT[:, fc, s * 128:(s + 1) * 128], wdnb[:, fc, nb * 512:(nb + 1) * 512], start=(fc == 0), stop=(fc == 1))
                            return r
                        p.op('pe', mmy, reads=[hT, (wdnb.name, 0), (wdnb.name, 1)], writes=[py])
                        if s == 0:
                            p.op('act', lambda e: e.copy(yb_[:], py[:]), reads=[py], writes=[yb_])
                        else:
                            p.op('dve', lambda e: e.tensor_copy(yb_[:], py[:]), reads=[py], writes=[yb_])
                        p.dma('sp', YS[s0 + s * 128:s0 + (s + 1) * 128, :], yb_[:], reads=[yb_], writes=[p.uk()])

                def run_set(si):
                    for b in range(si, NBLK, 2):
                        blk(b, sets[si])
                p.interleave([lambda: run_set(0), lambda: run_set(1)])
                p.barrier()

            with ExitStack() as ph:
                M5 = [p.sb(ph, [128, D], F32) for _ in range(2)]
                for s in range(2):
                    p.dma('sp', M5[s][:], MOD[layer, s:s + 1, 5 * D:6 * D].to_broadcast([128, D]), writes=[M5[s]])
                fg = p.sb(ph, [128, D], F32)
                p.dma('sp', fg[:], final_g[0:1, :].to_broadcast([128, D]), writes=[fg])
                g0 = [p.sb(ph, [128, D], F32) for _ in range(2)]
                g1_ = [p.sb(ph, [128, D], F32) for _ in range(2)]
                xt = [p.sb(ph, [128, D], F32) for _ in range(2)]
                ys_ = [p.sb(ph, [128, D], F32) for _ in range(2)]
                sxs = [p.sb(ph, [128, 4], F32) for _ in range(2)]

                def cb_stream(si):
                    def run():
                        for tt in range(T0 + si, NT, 2):
                            s = 1 if tt < LT else 0
                            tok = slice(tt * 128, (tt + 1) * 128)
                            a = g0[si]; b2 = g1_[si]; xb = xt[si]; y = ys_[si]; sx = sxs[si]
                            p.idma(a[:], None, YS[:, :], bass.IndirectOffsetOnAxis(ap=DESTi[:, tt, 0:1], axis=0), reads=['YS', DESTi], writes=[a])
                            p.idma(b2[:], None, YS[:, :], bass.IndirectOffsetOnAxis(ap=DESTi[:, tt, 1:2], axis=0), reads=['YS', DESTi], writes=[b2])
                            p.dma('sp', xb[:], X[tok, :], reads=[('X', tt)], writes=[xb])
                            p.op('dve', lambda e: e.tensor_scalar(y[:], a[:], W01[:, tt, 0:1], None, ALU.mult), reads=[a, W01], writes=[y])
                            p.op('dve', lambda e: e.scalar_tensor_tensor(y[:], b2[:], W01[:, tt, 1:2], y[:], ALU.mult, ALU.add), reads=[b2, W01, y], writes=[y])
                            p.op('dve', lambda e: e.tensor_mul(y[:], y[:], M5[s][:]), reads=[y, M5[s]], writes=[y])
                            p.op('dve', lambda e: e.tensor_add(xb[:], xb[:], y[:]), reads=[xb, y], writes=[xb])
                            if layer == 0:
                                p.dma('sp', X[tok, :], xb[:], reads=[xb], writes=[('X', tt)])
                            else:
                                p.op('act', lambda e: e.activation(y[:], xb[:], AF.Square, accum_out=sx[:, 0:1]), reads=[xb], writes=[y, sx])
                                p.op('dve', lambda e: e.tensor_scalar(sx[:, 1:2], sx[:, 0:1], 1.0 / D, EPS, ALU.mult, ALU.add), reads=[sx], writes=[sx])
                                rsqrt(sx[:, 1:2], [sx])
                                p.op('dve', lambda e: e.scalar_tensor_tensor(y[:], xb[:], sx[:, 1:2], fg[:], ALU.mult, ALU.mult), reads=[xb, sx, fg], writes=[y])
                                p.dma('sp', out[(tt - LT) * 128:(tt - LT + 1) * 128, :], y[:], reads=[y], writes=[p.uk()])
                    return run
                p.interleave([cb_stream(0), cb_stream(1)])
                p.barrier()
    return nc


_L, _N = 256, 4096
_NC_CACHE = {}


def _core_inputs(inp, b):
    L, N = _L, _N
    T = L + N
    f = lambda a: np.ascontiguousarray(np.asarray(a, dtype=np.float32))
    m = {}
    m['xin'] = f(np.concatenate([inp['ctx'][b], inp['x'][b]], axis=0))
    c2 = np.stack([np.asarray(inp['c'][b]), np.asarray(inp['c_ctx'])], axis=0)
    m['c2T'] = f(c2.reshape(2, 8, 128).transpose(2, 1, 0))
    return m


def _shared_inputs(inp):
    L, N = _L, _N
    T = L + N
    f = lambda a: np.ascontiguousarray(np.asarray(a, dtype=np.float32))
    m = {}
    for k in ['w_mod', 'b_mod', 'norm1_g', 'norm2_g', 'w_in', 'w_out', 'hgrn_norm_g', 'gla_norm_g', 'mla_w_uq', 'mla_w_ukv']:
        m[k] = f(inp[k])
    m['final_norm_g'] = f(np.asarray(inp['final_norm_g']).reshape(1, -1))
    m['hgrn_lb_logits'] = f(np.asarray(inp['hgrn_lb_logits']).reshape(2, 512))
    m['na_tab'] = f(na_bias_table(np.asarray(inp['na_rpb'], dtype=np.float32)).reshape(2, 4, 8, 64, 512))
    wg = np.zeros((2, 32, 256), np.float32)
    wg[:, 0:16, 0:128] = np.asarray(inp['gla_wg_f'])
    wg[:, 16:32, 128:256] = np.asarray(inp['gla_wg_b'])
    m['gla_wg_bd'] = wg
    m['gla_bg_cat'] = f(np.concatenate([np.asarray(inp['gla_bg_f']), np.asarray(inp['gla_bg_b'])], axis=1))
    m['mla_q_norm_g'] = f(np.asarray(inp['mla_q_norm_g']).reshape(2, 192, 1))
    m['mla_kv_norm_g'] = f(np.asarray(inp['mla_kv_norm_g']).reshape(2, 128, 1))
    m['moe_w_r'] = f(np.concatenate([np.asarray(inp['moe_w_rg']), np.asarray(inp['moe_w_re'])], axis=2))
    m['moe_b_r'] = f(np.concatenate([np.asarray(inp['moe_b_rg']), np.asarray(inp['moe_b_re'])], axis=1))
    m['moe_w_gu'] = f(np.asarray(inp['moe_w_gu']).reshape(2 * 32 * 1024, 512))
    m['moe_w_dn'] = f(np.asarray(inp['moe_w_dn']).reshape(2 * 32 * 256, 1024))
    hc = host_consts(L, N)
    hc['rope'] = hc['rope'].reshape(T, 32)
    m.update(hc)
    return m


def kernel(**inputs):
    inp = {k: np.asarray(v) for k, v in inputs.items()}
    B = inp['x'].shape[0]
    if 'nc' not in _NC_CACHE:
        _NC_CACHE['nc'] = build(_L, _N)
    nc = _NC_CACHE['nc']
    shared = _shared_inputs(inp)
    in_maps = []
    for core in range(8):
        m = dict(shared)
        m.update(_core_inputs(inp, core % B))
        in_maps.append(m)
    res = run_bass_kernel_spmd(nc, in_maps, core_ids=list(range(8)))
    outs = [np.asarray(res.results[b]['out'], dtype=np.float32) for b in range(B)]
    return np.stack(outs, axis=0)
```

```python
import numpy as np
from contextlib import ExitStack
import ml_dtypes
import concourse.bass as bass
import concourse.mybir as mybir
from concourse.bass_utils import run_bass_kernel_spmd

F32 = mybir.dt.float32
BF16 = mybir.dt.bfloat16
I32 = mybir.dt.int32
AF = mybir.ActivationFunctionType
ALU = mybir.AluOpType
AX = mybir.AxisListType

D = 1024
DIN = 3200
EPS = 1e-6
NEG = -30000.0
MOEB = 256
NEXP = 32
RSTAGE = 99
SCL = float(np.exp(-30.0))


class Prog:
    def __init__(self, nc, es, n_dma_sems=14):
        self.nc = nc
        self.es = es
        self.engs = {'pe': nc.tensor, 'act': nc.scalar, 'dve': nc.vector, 'pool': nc.gpsimd, 'sp': nc.sync}
        self.sem = {}
        self.cnt = {}
        for e in ('pe', 'act', 'dve', 'pool'):
            self.sem[e] = es.enter_context(nc.semaphore('s_' + e))
            self.cnt[e] = 0
        self.dsems = {}
        self.dcnt = {}
        self.dnext = {}
        for q in ('sp', 'pool'):
            self.dsems[q] = [es.enter_context(nc.semaphore('d_%s%d' % (q, i))) for i in range(n_dma_sems)]
            self.dcnt[q] = [0] * n_dma_sems
            self.dnext[q] = 0
        self.waited = {}
        self.lastw = {}
        self.readers = {}
        self.nbuf = 0

    def sb(self, st, shape, dt, name=None):
        self.nbuf += 1
        return st.enter_context(self.nc.sbuf_tensor(name or ('sb%d' % self.nbuf), list(shape), dt))

    def ps(self, st, shape, dt=F32, name=None):
        self.nbuf += 1
        return st.enter_context(self.nc.psum_tensor(name or ('ps%d' % self.nbuf), list(shape), dt))

    def _key(self, k):
        if isinstance(k, (str, tuple)):
            return k
        t = getattr(k, 'tensor', k)
        return getattr(t, 'name', None) or id(t)

    def _wait(self, ename, ev):
        sem, val = ev
        k = (ename, sem.num)
        if self.waited.get(k, 0) >= val:
            return
        self.waited[k] = val
        self.engs[ename].wait_ge(sem, val)

    def _deps(self, ename, reads, writes):
        deps = []
        for k in reads:
            k = self._key(k)
            if k in self.lastw:
                deps.append(self.lastw[k])
        for k in writes:
            k = self._key(k)
            if k in self.lastw:
                deps.append(self.lastw[k])
            deps.extend(self.readers.get(k, []))
        for ev in deps:
            self._wait(ename, ev)

    def _record(self, ev, reads, writes):
        for k in reads:
            k = self._key(k)
            self.readers.setdefault(k, []).append(ev)
        for k in writes:
            k = self._key(k)
            self.lastw[k] = ev
            self.readers[k] = []

    def op(self, ename, fn, reads=(), writes=()):
        self._deps(ename, reads, writes)
        ins = fn(self.engs[ename])
        self.cnt[ename] += 1
        ins.then_inc(self.sem[ename], 1)
        ev = (self.sem[ename], self.cnt[ename])
        self._record(ev, reads, writes)
        if getattr(self, '_yield', None):
            self._yield()
        return ev

    def _dma_common(self, q, emit, reads, writes):
        i = self.dnext[q]
        self.dnext[q] = (i + 1) % len(self.dsems[q])
        sem = self.dsems[q][i]
        if self.dcnt[q][i] > 0:
            self._wait(q, (sem, self.dcnt[q][i]))
        self._deps(q, reads, writes)
        ins = emit(self.engs[q])
        self.dcnt[q][i] += 16
        ins.then_inc(sem, 16)
        ev = (sem, self.dcnt[q][i])
        self._record(ev, reads, writes)
        if getattr(self, '_yield', None):
            self._yield()
        return ev

    def dma(self, q, out, in_, reads=(), writes=(), **kw):
        return self._dma_common(q, lambda e: e.dma_start(out=out, in_=in_, **kw), reads, writes)

    def idma(self, out, out_off, in_, in_off, reads=(), writes=(), **kw):
        return self._dma_common('pool', lambda e: e.indirect_dma_start(out=out, out_offset=out_off, in_=in_,
                                                                      in_offset=in_off, **kw), reads, writes)

    def interleave(self, fns):
        import threading
        n = len(fns)
        il = {'turn': 0, 'alive': [True] * n, 'cond': threading.Condition(), 'exc': None}
        tl = threading.local()

        def advance(i):
            for k in range(1, n + 1):
                j = (i + k) % n
                if il['alive'][j]:
                    il['turn'] = j
                    break
            il['cond'].notify_all()

        def wait_turn(i):
            while il['turn'] != i and il['exc'] is None:
                il['cond'].wait()

        def yield_():
            i = getattr(tl, 'idx', None)
            if i is None:
                return
            advance(i)
            wait_turn(i)
            if il['exc'] is not None:
                raise RuntimeError('interleave peer failed')

        def runner(i):
            with il['cond']:
                wait_turn(i)
                tl.idx = i
                try:
                    if il['exc'] is None:
                        fns[i]()
                except BaseException as e:
                    if il['exc'] is None:
                        il['exc'] = e
                il['alive'][i] = False
                if any(il['alive']):
                    advance(i)
                il['cond'].notify_all()

        prev = getattr(self, '_yield', None)
        self._yield = yield_
        ths = [threading.Thread(target=runner, args=(i,)) for i in range(n)]
        for t in ths:
            t.start()
        for t in ths:
            t.join()
        self._yield = prev
        if il['exc'] is not None:
            raise il['exc']

    def uk(self):
        self._ukn = getattr(self, '_ukn', 0) + 1
        return ('uk', self._ukn)

    def barrier(self):
        evs = [(self.sem[e], self.cnt[e]) for e in self.sem if self.cnt[e] > 0]
        for q in self.dsems:
            for i, s in enumerate(self.dsems[q]):
                if self.dcnt[q][i] > 0:
                    evs.append((s, self.dcnt[q][i]))
        for e in self.engs:
            for ev in evs:
                self._wait(e, ev)
        self.lastw = {}
        self.readers = {}


def host_consts(L, N):
    T = L + N
    c = {}
    c['ident_f'] = np.eye(128, dtype=np.float32)
    j = np.arange(128)[:, None]
    i = np.arange(128)[None, :]
    same = (j // 64) == (i // 64)
    U2f = (same & (j <= i)).astype(np.float32)
    Umf = (same & (j % 64 <= 31)).astype(np.float32)
    U2b = (same & (j >= i)).astype(np.float32)
    Umb = (same & (j % 64 >= 32)).astype(np.float32)
    c['uw_f'] = np.concatenate([U2f, U2f - Umf], axis=1)
    c['uw_b'] = np.concatenate([U2b, U2b - Umb], axis=1)
    c['v2_f'] = (same & (j > i)).astype(np.float32)
    c['v2_b'] = (same & (j < i)).astype(np.float32)
    jj = np.arange(64)[:, None]
    ii = np.arange(64)[None, :]
    c['mask_f'] = ((jj <= ii).astype(np.float64) * np.exp(60.0)).astype(np.float32)
    c['mask_b'] = ((jj >= ii).astype(np.float64) * np.exp(60.0)).astype(np.float32)
    c['lstrict'] = (j < i).astype(np.float32)
    t = np.arange(N)
    inv = (10000.0 ** (-np.arange(8, dtype=np.float32) / 8)).astype(np.float32)
    ang = np.stack([(t // 64).astype(np.float32)[:, None] * inv, (t % 64).astype(np.float32)[:, None] * inv], axis=1)
    rope = np.zeros((T, 2, 2, 8), np.float32)
    rope[:L, 0] = 1.0
    rope[L:, 0] = np.cos(ang)
    rope[L:, 1] = np.sin(ang)
    c['rope'] = rope
    c['pcol'] = np.arange(128, dtype=np.float32)[:, None].copy()
    return c


def na_bias_table(rpb):
    cidx = np.arange(64)
    c_start = np.clip(cidx - 8, 0, 48)
    col_in = (cidx[None] >= c_start[:, None]) & (cidx[None] < c_start[:, None] + 16)
    dc = np.clip(cidx[None] - cidx[:, None], -15, 15) + 15
    out = np.full((2, 4, 8, 64, 8, 64), NEG, np.float32)
    for cls in range(8):
        for jx in range(8):
            dr = jx + 7 - cls
            g = rpb[:, :, dr, :][:, :, dc]
            g = np.where(col_in[None, None], g, NEG)
            out[:, :, cls, :, jx, :] = np.transpose(g, (0, 1, 3, 2))
    return out


def build(L, N, dbg=False):
    T = L + N
    NT = T // 128
    NCH = T // 64
    LT = L // 128
    ROWS = N // 64
    NBLK = (2 * T + MOEB - 1) // MOEB + NEXP
    NSLOT = NBLK * MOEB
    nc = bass.Bass("TRN2", target_bir_lowering=False)

    def din(name, shape, dt=F32):
        return nc.dram_tensor(name, list(shape), dt, kind="ExternalInput").ap()

    def dscr(name, shape, dt=F32):
        kind = "ExternalOutput" if dbg else "Internal"
        return nc.dram_tensor(name, list(shape), dt, kind=kind).ap()

    xin = din('xin', [T, D])
    c2T = din('c2T', [128, 8, 2])
    w_mod = din('w_mod', [2, D, 6 * D])
    b_mod = din('b_mod', [2, 6 * D])
    norm1_g = din('norm1_g', [2, D])
    norm2_g = din('norm2_g', [2, D])
    final_g = din('final_norm_g', [1, D])
    w_in = din('w_in', [2, D, DIN])
    w_out = din('w_out', [2, D, D])
    lb_logits = din('hgrn_lb_logits', [2, 512])
    hgrn_ng = din('hgrn_norm_g', [2, 256])
    na_tab = din('na_tab', [2, 4, 8, 64, 512])
    wg_bd = din('gla_wg_bd', [2, 32, 256])
    bg_cat = din('gla_bg_cat', [2, 256])
    gla_ng = din('gla_norm_g', [2, 256])
    mla_qg = din('mla_q_norm_g', [2, 192, 1])
    mla_wuq = din('mla_w_uq', [2, 192, 384])
    mla_kvg = din('mla_kv_norm_g', [2, 128, 1])
    mla_wukv = din('mla_w_ukv', [2, 128, 512])
    w_r = din('moe_w_r', [2, D, 36])
    b_r = din('moe_b_r', [2, 36])
    w_gu = din('moe_w_gu', [2 * NEXP * D, 512])
    w_dn = din('moe_w_dn', [2 * NEXP * 256, D])
    ident_f_d = din('ident_f', [128, 128])
    uw_d = {'f': din('uw_f', [128, 256]), 'b': din('uw_b', [128, 256])}
    v2_d = {'f': din('v2_f', [128, 128]), 'b': din('v2_b', [128, 128])}
    mask_d = {'f': din('mask_f', [64, 64]), 'b': din('mask_b', [64, 64])}
    lstrict_d = din('lstrict', [128, 128])
    rope_d = din('rope', [T, 32])
    pcol_d = din('pcol', [128, 1])
    out = nc.dram_tensor('out', [N, D], F32, kind="ExternalOutput").ap()

    X = dscr('X', [T, D])
    MOD = dscr('MOD', [2, 2, 6 * D])
    mix = {}
    for m in 'AC':
        for d in 'fb':
            mix[m + d + 'QT'] = dscr('s_%s%s_QT' % (m, d), [256, T], BF16)
            mix[m + d + 'KT'] = dscr('s_%s%s_KT' % (m, d), [256, T], BF16)
            mix[m + d + 'QH'] = dscr('s_%s%s_QH' % (m, d), [256, T], BF16)
            mix[m + d + 'KH'] = dscr('s_%s%s_KH' % (m, d), [T, 256], BF16)
            mix[m + d + 'DEC'] = dscr('s_%s%s_DEC' % (m, d), [256, NCH])
            mix[m + d + 'O'] = dscr('s_%s%s_O' % (m, d), [T, 256])
        mix[m + 'V'] = dscr('s_%s_V' % m, [T, 256], BF16)
        mix[m + 'G'] = dscr('s_%s_G' % m, [T, 256])
    BQT = dscr('s_B_QT', [256, T], BF16)
    BKT = dscr('s_B_KT', [256, T], BF16)
    BV = dscr('s_B_V', [T, 256], BF16)
    DQT = dscr('s_D_QT', [4, 96, T], BF16)
    DKT = dscr('s_D_KT', [4, 96, T], BF16)
    DV = dscr('s_D_V', [T, 256], BF16)
    CATT = dscr('s_CATT', [D, T], BF16)
    H2 = dscr('s_H2', [T, D], BF16)
    XS = dscr('s_XS', [NSLOT, D], BF16)
    YS = dscr('s_YS', [NSLOT, D])

    with ExitStack() as es:
        p = Prog(nc, es)
        ident_f = p.sb(es, [128, 128], F32, 'ident_f_sb')
        ident_b = p.sb(es, [128, 128], BF16, 'ident_b_sb')
        ones_f = p.sb(es, [128, 128], F32, 'ones_f')
        ones_b = p.sb(es, [128, 128], BF16, 'ones_b')
        p.dma('sp', ident_f[:], ident_f_d, writes=[ident_f])
        p.op('dve', lambda e: e.tensor_copy(ident_b[:], ident_f[:]), reads=[ident_f], writes=[ident_b])
        p.op('dve', lambda e: e.memset(ones_f[:], 1.0), writes=[ones_f])
        p.op('dve', lambda e: e.memset(ones_b[:], 1.0), writes=[ones_b])

        def transpose_f(ps_ap, in_ap, reads, writes):
            n = in_ap.shape[0]
            p.op('pe', lambda e: e.transpose(ps_ap, in_ap, ident_f[0:n, 0:n]), reads=list(reads) + [ident_f], writes=writes)

        def transpose_b(ps_ap, in_ap, reads, writes):
            n = in_ap.shape[0]
            p.op('pe', lambda e: e.transpose(ps_ap, in_ap, ident_b[0:n, 0:n]), reads=list(reads) + [ident_b], writes=writes)

        def rsqrt(ap, keys):
            p.op('act', lambda e: e.activation(ap, ap, AF.Ln), reads=keys, writes=keys)
            p.op('act', lambda e: e.activation(ap, ap, AF.Exp, scale=-0.5), reads=keys, writes=keys)

        with ExitStack() as ph:
            xt = [p.sb(ph, [128, D], F32) for _ in range(2)]
            for tt in range(NT):
                b = xt[tt % 2]
                p.dma('sp', b[:], xin[tt * 128:(tt + 1) * 128, :], writes=[b])
                p.dma('sp', X[tt * 128:(tt + 1) * 128, :], b[:], reads=[b], writes=[('X', tt)])
            cT = p.sb(ph, [128, 8, 2], F32)
            sg = p.sb(ph, [128, 8, 2], F32)
            p.dma('sp', cT[:], c2T, writes=[cT])
            p.op('act', lambda e: e.activation(sg[:], cT[:], AF.Sigmoid), reads=[cT], writes=[sg])
            p.op('dve', lambda e: e.tensor_mul(cT[:], cT[:], sg[:]), reads=[cT, sg], writes=[cT])
            wm = [p.sb(ph, [128, 8, 512], F32) for _ in range(2)]
            bm = [p.sb(ph, [2, 512], F32) for _ in range(2)]
            mo = [p.sb(ph, [2, 512], F32) for _ in range(2)]
            pm = [p.ps(ph, [2, 512]) for _ in range(2)]
            it = 0
            for l in range(2):
                for nb in range(12):
                    w = wm[it % 2]; bb = bm[it % 2]; o = mo[it % 2]; ps = pm[it % 2]
                    p.dma('sp', w[:], w_mod[l, :, nb * 512:(nb + 1) * 512].rearrange("(k p) n -> p k n", p=128), writes=[w])
                    p.dma('sp', bb[:], b_mod[l:l + 1, nb * 512:(nb + 1) * 512].to_broadcast([2, 512]), writes=[bb])

                    def mm(e, w=w, ps=ps):
                        for kc in range(8):
                            r = e.matmul(ps[:], cT[:, kc, :], w[:, kc, :], start=(kc == 0), stop=(kc == 7))
                        return r
                    p.op('pe', mm, reads=[cT, w], writes=[ps])
                    p.op('dve', lambda e, o=o, ps=ps, bb=bb: e.tensor_add(o[:], ps[:], bb[:]), reads=[ps, bb], writes=[o])
                    p.dma('sp', MOD[l, :, nb * 512:(nb + 1) * 512], o[:], reads=[o], writes=[p.uk()])
                    it += 1
            p.barrier()

        for layer in range(2):
          with ExitStack() as lay:
            keep_ctx = layer == 0
            T0 = 0 if keep_ctx else LT
            with ExitStack() as ph:
                winb = p.sb(ph, [128, 8, DIN], BF16)
                for kc in range(8):
                    p.dma('pool', winb[:, kc, :], w_in[layer, kc * 128:(kc + 1) * 128, :], writes=[(winb.name, kc)])
                G1 = [p.sb(ph, [128, D], F32) for _ in range(2)]
                B1 = [p.sb(ph, [128, D], F32) for _ in range(2)]
                g1 = p.sb(ph, [128, D], F32)
                p.dma('sp', g1[:], norm1_g[layer:layer + 1, :].to_broadcast([128, D]), writes=[g1])
                for s in range(2):
                    p.dma('sp', B1[s][:], MOD[layer, s:s + 1, 0:D].to_broadcast([128, D]), reads=['MOD'], writes=[B1[s]])
                    p.dma('sp', G1[s][:], MOD[layer, s:s + 1, D:2 * D].to_broadcast([128, D]), reads=['MOD'], writes=[G1[s]])
                    p.op('dve', lambda e, s=s: e.scalar_tensor_tensor(G1[s][:], G1[s][:], 1.0, g1[:], ALU.add, ALU.mult),
                         reads=[G1[s], g1], writes=[G1[s]])
                LB = p.sb(ph, [128, 512], F32)
                OMLB = p.sb(ph, [128, 512], F32)
                if layer == 0:
                    p.op('dve', lambda e: e.memset(LB[:], 0.0), writes=[LB])
                    p.op('dve', lambda e: e.memset(OMLB[:], 1.0), writes=[OMLB])
                else:
                    l0 = p.sb(ph, [128, 512], F32)
                    p.dma('sp', l0[:], lb_logits[0:1, :].to_broadcast([128, 512]), writes=[l0])
                    p.dma('sp', LB[:], lb_logits[1:2, :].to_broadcast([128, 512]), writes=[LB])
                    p.op('dve', lambda e: e.tensor_sub(LB[:], LB[:], l0[:]), reads=[LB, l0], writes=[LB])
                    p.op('act', lambda e: e.activation(LB[:], LB[:], AF.Sigmoid), reads=[LB], writes=[LB])
                    p.op('dve', lambda e: e.tensor_scalar(OMLB[:], LB[:], -1.0, 1.0, ALU.mult, ALU.add), reads=[LB], writes=[OMLB])
                wgbd = p.sb(ph, [32, 256], F32)
                bgc = p.sb(ph, [128, 256], F32)
                p.dma('sp', wgbd[:], wg_bd[layer], writes=[wgbd])
                p.dma('sp', bgc[:], bg_cat[layer:layer + 1, :].to_broadcast([128, 256]), writes=[bgc])
                wuq = p.sb(ph, [128, 2, 384], F32)
                wuqb = p.sb(ph, [128, 2, 384], BF16)
                qg = p.sb(ph, [128, 2], F32)
                wukv = p.sb(ph, [128, 512], F32)
                wukvb = p.sb(ph, [128, 512], BF16)
                kvg = p.sb(ph, [128, 1], F32)
                p.dma('sp', wuq[:, 0, :], mla_wuq[layer, 0:128, :], writes=[wuq])
                p.dma('sp', wuq[0:64, 1, :], mla_wuq[layer, 128:192, :], writes=[wuq])
                p.dma('sp', qg[:, 0:1], mla_qg[layer, 0:128, :], writes=[qg])
                p.dma('sp', qg[0:64, 1:2], mla_qg[layer, 128:192, :], writes=[qg])
                p.dma('sp', wukv[:], mla_wukv[layer], writes=[wukv])
                p.dma('sp', kvg[:], mla_kvg[layer], writes=[kvg])
                p.op('dve', lambda e: e.tensor_scalar(wuqb[:, 0, :], wuq[:, 0, :], qg[:, 0:1], None, ALU.mult), reads=[wuq, qg], writes=[wuqb])
                p.op('dve', lambda e: e.tensor_scalar(wuqb[0:64, 1, :], wuq[0:64, 1, :], qg[0:64, 1:2], None, ALU.mult), reads=[wuq, qg], writes=[wuqb])
                p.op('dve', lambda e: e.tensor_scalar(wukvb[:], wukv[:], kvg[:, 0:1], None, ALU.mult), reads=[wukv, kvg], writes=[wukvb])
                uw = {}; v2 = {}
                for d in 'fb':
                    uw[d] = p.sb(ph, [128, 256], F32)
                    v2[d] = p.sb(ph, [128, 128], F32)
                    p.dma('sp', uw[d][:], uw_d[d], writes=[uw[d]])
                    p.dma('sp', v2[d][:], v2_d[d], writes=[v2[d]])
                xt = [p.sb(ph, [128, D], F32) for _ in range(2)]
                junk = p.sb(ph, [128, D], F32)
                st = [p.sb(ph, [128, 8], F32) for _ in range(2)]
                hb = p.sb(ph, [128, D], BF16)
                hT = p.sb(ph, [128, 8, 128], BF16)
                pTs = [p.sb(ph, [128, DIN], F32) for _ in range(2)]
                junk2 = p.sb(ph, [128, 192], F32)
                ps_t = p.ps(ph, [128, 8, 128], BF16)
                ps_t2 = p.ps(ph, [128, 8, 128], BF16)
                ps_p = [p.ps(ph, [128, 512]) for _ in range(2)]
                ps_a = p.ps(ph, [128, 2, 256])
                ps_b = p.ps(ph, [128, 2, 256])
                ps_c = p.ps(ph, [128, 512])
                ps_d = p.ps(ph, [128, 512])
                qA = p.sb(ph, [128, 256], F32)
                kk = p.sb(ph, [128, 256], F32)
                la = p.sb(ph, [128, 256], F32)
                qTs = p.sb(ph, [128, 2, 128], F32)
                kTs = p.sb(ph, [128, 2, 128], F32)
                x2 = p.sb(ph, [128, 2, 128], F32)
                x1 = p.sb(ph, [128, 2, 128], F32)
                x1n = p.sb(ph, [128, 2, 128], F32)
                x3 = p.sb(ph, [128, 256], F32)
                o_qh = p.sb(ph, [128, 2, 128], BF16)
                o_qt = p.sb(ph, [128, 2, 128], BF16)
                o_kt = p.sb(ph, [128, 2, 128], BF16)
                o_kh = p.sb(ph, [128, 256], BF16)
                gs = p.sb(ph, [128, 256], F32)
                qpad = p.sb(ph, [128, 4, 64], F32)
                kpad = p.sb(ph, [128, 4, 64], F32)
                lapad = p.sb(ph, [128, 4, 64], F32)
                zT = p.sb(ph, [32, 128], F32)
                lac = p.sb(ph, [128, 256], F32)
                rin = p.sb(ph, [128, 5, 32], F32)
                rout = p.sb(ph, [128, 5, 32], F32)
                rtmp5 = p.sb(ph, [128, 5, 32], F32)
                rtmp = p.sb(ph, [128, 4, 32], F32)
                bq = p.sb(ph, [128, 256], BF16)
                bqT = p.sb(ph, [128, 2, 128], BF16)
                bkT = p.sb(ph, [128, 2, 128], BF16)
                bv = p.sb(ph, [128, 256], BF16)
                ropets = [p.sb(ph, [128, 32], F32) for _ in range(2)]
                cqn = p.sb(ph, [128, 192], BF16)
                ckvn = p.sb(ph, [128, 128], BF16)
                cqT = p.sb(ph, [128, 2, 128], BF16)
                ckvT = p.sb(ph, [128, 128], BF16)
                qd = p.sb(ph, [128, 4, 96], F32)
                qd2 = p.sb(ph, [128, 4, 96], F32)
                qdb = p.sb(ph, [128, 4, 96], BF16)
                kvd = p.sb(ph, [128, 4, 128], F32)
                kdb = p.sb(ph, [128, 4, 96], BF16)
                kr = p.sb(ph, [128, 32], F32)
                kr2 = p.sb(ph, [128, 32], F32)
                vdb = p.sb(ph, [128, 256], BF16)
                dT = p.sb(ph, [96, 4, 128], BF16)
                p.op('dve', lambda e: e.memset(qpad[:], 0.0), writes=[qpad])
                p.op('dve', lambda e: e.memset(kpad[:], 0.0), writes=[kpad])
                p.op('dve', lambda e: e.memset(lapad[:], 0.0), writes=[lapad])

                def gla_prep(m, tt, d, q_ap, k_ap, la_ap, rq, rk, rl, first):
                    tok = slice(tt * 128, (tt + 1) * 128)
                    if first:
                        for ct in range(2):
                            transpose_f(ps_a[:, ct, 0:128], q_ap[:, ct * 128:(ct + 1) * 128], rq, [ps_a])
                        p.op('act', lambda e: e.copy(qTs[:], ps_a[:, :, 0:128]), reads=[ps_a], writes=[qTs])
                    for ct in range(2):
                        transpose_f(ps_a[:, ct, 128:256], k_ap[:, ct * 128:(ct + 1) * 128], rk, [ps_a])
                    p.op('act', lambda e: e.copy(kTs[:], ps_a[:, :, 128:256]), reads=[ps_a], writes=[kTs])
                    for ct in range(2):
                        p.op('pe', lambda e, ct=ct: e.matmul(ps_b[:, ct, :], la_ap[:, ct * 128:(ct + 1) * 128], uw[d][:], start=True, stop=True),
                             reads=list(rl) + [uw[d]], writes=[ps_b])
                    p.op('pe', lambda e: e.matmul(ps_c[:, 0:256], v2[d][:], la_ap, start=True, stop=True), reads=list(rl) + [v2[d]], writes=[ps_c])
                    p.op('act', lambda e: e.activation(x2[:], ps_b[:, :, 0:128], AF.Exp), reads=[ps_b], writes=[x2])
                    p.op('act', lambda e: e.activation(x1[:], ps_b[:, :, 128:256], AF.Exp), reads=[ps_b], writes=[x1])
                    p.op('act', lambda e: e.activation(x1n[:], ps_b[:, :, 128:256], AF.Exp, scale=-1.0), reads=[ps_b], writes=[x1n])
                    p.op('act', lambda e: e.activation(x3[:], ps_c[:, 0:256], AF.Exp), reads=[ps_c], writes=[x3])
                    p.op('dve', lambda e: e.tensor_mul(o_qh[:], qTs[:], x2[:]), reads=[qTs, x2], writes=[o_qh])
                    p.op('dve', lambda e: e.scalar_tensor_tensor(o_qt[:], qTs[:], SCL, x1[:], ALU.mult, ALU.mult), reads=[qTs, x1], writes=[o_qt])
                    p.op('dve', lambda e: e.scalar_tensor_tensor(o_kt[:], kTs[:], SCL, x1n[:], ALU.mult, ALU.mult), reads=[kTs, x1n], writes=[o_kt])
                    p.op('dve', lambda e: e.tensor_mul(o_kh[:], k_ap, x3[:]), reads=list(rk) + [x3], writes=[o_kh])
                    md = m + d
                    p.dma('pool', mix[md + 'QH'][:, tok].rearrange("(c p) t -> p c t", p=128), o_qh[:], reads=[o_qh], writes=[p.uk()])
                    p.dma('pool', mix[md + 'QT'][:, tok].rearrange("(c p) t -> p c t", p=128), o_qt[:], reads=[o_qt], writes=[p.uk()])
                    p.dma('pool', mix[md + 'KT'][:, tok].rearrange("(c p) t -> p c t", p=128), o_kt[:], reads=[o_kt], writes=[p.uk()])
                    p.dma('pool', mix[md + 'KH'][tok, :], o_kh[:], reads=[o_kh], writes=[p.uk()])
                    cols = (63, 127) if d == 'f' else (0, 64)
                    for cc in range(2):
                        p.dma('pool', mix[md + 'DEC'][:, 2 * tt + cc:2 * tt + cc + 1].rearrange("(c p) t -> p c t", p=128),
                              x2[:, :, cols[cc]:cols[cc] + 1], reads=[x2], writes=[p.uk()], allow_slow_non_contiguous=True)

                ncb = (DIN + 511) // 512

                def front(tt):
                    s = 1 if tt < LT else 0
                    tok = slice(tt * 128, (tt + 1) * 128)
                    xb = xt[tt % 2]; sx = st[tt % 2]; pT = pTs[tt % 2]; ropet = ropets[tt % 2]
                    p.dma('sp', xb[:], X[tok, :], reads=[('X', tt)], writes=[xb])
                    p.dma('sp', ropet[:], rope_d[tok, :], writes=[ropet])
                    p.op('act', lambda e: e.activation(junk[:], xb[:], AF.Square, accum_out=sx[:, 0:1]), reads=[xb], writes=[junk, sx])
                    p.op('dve', lambda e: e.tensor_scalar(sx[:, 1:2], sx[:, 0:1], 1.0 / D, EPS, ALU.mult, ALU.add), reads=[sx], writes=[sx])
                    rsqrt(sx[:, 1:2], [sx])
                    p.op('dve', lambda e: e.scalar_tensor_tensor(junk[:], xb[:], sx[:, 1:2], G1[s][:], ALU.mult, ALU.mult), reads=[xb, sx, G1[s]], writes=[junk])
                    p.op('dve', lambda e: e.tensor_add(hb[:], junk[:], B1[s][:]), reads=[junk, B1[s]], writes=[hb])
                    for kc in range(8):
                        transpose_b(ps_t2[:, kc, :], hb[:, kc * 128:(kc + 1) * 128], [hb], [ps_t2])
                    p.op('act', lambda e: e.copy(hT[:], ps_t2[:]), reads=[ps_t2], writes=[hT])
                    for cb in range(ncb):
                        c0 = cb * 512; c1 = min(DIN, c0 + 512)
                        pp = ps_p[cb % 2]

                        def mm(e, pp=pp, c0=c0, c1=c1):
                            for kc in range(8):
                                r = e.matmul(pp[:, 0:c1 - c0], hT[:, kc, :], winb[:, kc, c0:c1], start=(kc == 0), stop=(kc == 7))
                            return r
                        p.op('pe', mm, reads=[hT] + [(winb.name, kc) for kc in range(8)], writes=[pp])
                        eng = 'act' if cb % 2 == 0 else 'dve'
                        if eng == 'act':
                            p.op('act', lambda e, pp=pp, c0=c0, c1=c1: e.copy(pT[:, c0:c1], pp[:, 0:c1 - c0]), reads=[pp], writes=[(pT.name, cb)])
                        else:
                            p.op('dve', lambda e, pp=pp, c0=c0, c1=c1: e.tensor_copy(pT[:, c0:c1], pp[:, 0:c1 - c0]), reads=[pp], writes=[(pT.name, cb)])

                def back(tt):
                    s = 1 if tt < LT else 0
                    tok = slice(tt * 128, (tt + 1) * 128)
                    sx = st[tt % 2]; pT = pTs[tt % 2]; ropet = ropets[tt % 2]
                    RP = [(pT.name, cb) for cb in range(ncb)]
                    def chain_x():
                        p.op('act', lambda e: e.activation(qA[:], pT[:, 0:256], AF.Silu), reads=RP, writes=[qA])
                        p.op('dve', lambda e: e.tensor_scalar(qA[:], qA[:], 0.125, None, ALU.mult), reads=[qA], writes=[qA])
                        p.dma('pool', mix['AV'][tok, :], pT[:, 256:512], reads=RP, writes=[p.uk()])
                        p.op('act', lambda e: e.activation(gs[:], pT[:, 1024:1280], AF.Silu), reads=RP, writes=[gs])
                        p.dma('pool', mix['AG'][tok, :], gs[:], reads=[gs], writes=[p.uk()])
                        for di, d in enumerate('fb'):
                            zc = slice(512 + 256 * di, 768 + 256 * di)
                            lc = slice(256 * di, 256 * di + 256)
                            p.op('act', lambda e: e.activation(la[:], pT[:, zc], AF.Sigmoid), reads=RP, writes=[la])
                            p.op('dve', lambda e: e.tensor_mul(la[:], la[:], OMLB[:, lc]), reads=[la, OMLB], writes=[la])
                            p.op('dve', lambda e: e.tensor_add(la[:], la[:], LB[:, lc]), reads=[la, LB], writes=[la])
                            p.op('dve', lambda e: e.tensor_scalar(kk[:], la[:], -1.0, 1.0, ALU.mult, ALU.add), reads=[la], writes=[kk])
                            p.op('act', lambda e: e.activation(la[:], la[:], AF.Ln), reads=[la], writes=[la])
                            gla_prep('A', tt, d, qA[:], kk[:], la[:], [qA], [kk], [la], di == 0)
                        p.op('dve', lambda e: e.tensor_scalar(qpad[:, :, 0:32], pT[:, 2048:2176].rearrange("p (h k) -> p h k", h=4), 32.0 ** -0.5, None, ALU.mult),
                             reads=RP, writes=[qpad])
                        p.op('dve', lambda e: e.tensor_copy(kpad[:, :, 0:32], pT[:, 2176:2304].rearrange("p (h k) -> p h k", h=4)), reads=RP, writes=[kpad])
                        p.dma('pool', mix['CV'][tok, :], pT[:, 2304:2560], reads=RP, writes=[p.uk()])
                        p.op('act', lambda e: e.activation(gs[:], pT[:, 2560:2816], AF.Silu), reads=RP, writes=[gs])
                        p.dma('pool', mix['CG'][tok, :], gs[:], reads=[gs], writes=[p.uk()])
                        transpose_f(ps_c[0:32, 256:384], pT[:, 2816:2848], RP, [ps_c])
                        p.op('act', lambda e: e.copy(zT[:], ps_c[0:32, 256:384]), reads=[ps_c], writes=[zT])
                        p.op('pe', lambda e: e.matmul(ps_c[:, 0:256], zT[:], wgbd[:], start=True, stop=True), reads=[zT, wgbd], writes=[ps_c])
                        p.op('dve', lambda e: e.tensor_add(lac[:], ps_c[:, 0:256], bgc[:]), reads=[ps_c, bgc], writes=[lac])
                        p.op('act', lambda e: e.activation(lac[:], lac[:], AF.Sigmoid), reads=[lac], writes=[lac])
                        p.op('act', lambda e: e.activation(lac[:], lac[:], AF.Ln), reads=[lac], writes=[lac])
                        for di, d in enumerate('fb'):
                            p.op('dve', lambda e: e.tensor_scalar(lapad[:, :, 0:32], lac[:, 128 * di:128 * di + 128].rearrange("p (h k) -> p h k", h=4), 1.0 / 16.0, None, ALU.mult),
                                 reads=[lac], writes=[lapad])
                            gla_prep('C', tt, d, qpad[:].rearrange("p h k -> p (h k)"), kpad[:].rearrange("p h k -> p (h k)"),
                                     lapad[:].rearrange("p h k -> p (h k)"), [qpad], [kpad], [lapad], di == 0)

                    def chain_y():
                        p.op('dve', lambda e: e.tensor_scalar(bq[:], pT[:, 1280:1536], 0.125, None, ALU.mult), reads=RP, writes=[bq])
                        for ct in range(2):
                            transpose_b(ps_t[:, ct, :], bq[:, ct * 128:(ct + 1) * 128], [bq], [ps_t])
                        p.op('act', lambda e: e.copy(bqT[:], ps_t[:, 0:2, :]), reads=[ps_t], writes=[bqT])
                        p.dma('sp', BQT[:, tok].rearrange("(c p) t -> p c t", p=128), bqT[:], reads=[bqT], writes=[p.uk()])
                        p.op('dve', lambda e: e.tensor_copy(bq[:], pT[:, 1536:1792]), reads=RP + [bqT], writes=[bq])
                        for ct in range(2):
                            transpose_b(ps_t[:, 2 + ct, :], bq[:, ct * 128:(ct + 1) * 128], [bq], [ps_t])
                        p.op('act', lambda e: e.copy(bkT[:], ps_t[:, 2:4, :]), reads=[ps_t], writes=[bkT])
                        p.dma('sp', BKT[:, tok].rearrange("(c p) t -> p c t", p=128), bkT[:], reads=[bkT], writes=[p.uk()])
                        p.op('dve', lambda e: e.tensor_copy(bv[:], pT[:, 1792:2048]), reads=RP, writes=[bv])
                        p.dma('sp', BV[tok, :], bv[:], reads=[bv], writes=[p.uk()])
                        p.op('act', lambda e: e.activation(junk2[:, 0:192], pT[:, 2848:3040], AF.Square, accum_out=sx[:, 2:3]), reads=RP, writes=[junk2, sx])
                        p.op('act', lambda e: e.activation(junk2[:, 0:128], pT[:, 3040:3168], AF.Square, accum_out=sx[:, 3:4]), reads=RP, writes=[junk2, sx])
                        p.op('dve', lambda e: e.tensor_scalar(sx[:, 4:5], sx[:, 2:3], 1.0 / 192, EPS, ALU.mult, ALU.add), reads=[sx], writes=[sx])
                        rsqrt(sx[:, 4:5], [sx])
                        p.op('dve', lambda e: e.tensor_scalar(sx[:, 5:6], sx[:, 3:4], 1.0 / 128, EPS, ALU.mult, ALU.add), reads=[sx], writes=[sx])
                        rsqrt(sx[:, 5:6], [sx])
                        p.op('dve', lambda e: e.tensor_scalar(cqn[:], pT[:, 2848:3040], sx[:, 4:5], None, ALU.mult), reads=RP + [sx], writes=[cqn])
                        p.op('dve', lambda e: e.tensor_scalar(ckvn[:], pT[:, 3040:3168], sx[:, 5:6], None, ALU.mult), reads=RP + [sx], writes=[ckvn])
                        transpose_b(ps_t[:, 4, :], cqn[:, 0:128], [cqn], [ps_t])
                        transpose_b(ps_t[0:64, 5, :], cqn[:, 128:192], [cqn], [ps_t])
                        transpose_b(ps_t[:, 6, :], ckvn[:], [ckvn], [ps_t])
                        p.op('act', lambda e: e.copy(cqT[:, 0, :], ps_t[:, 4, :]), reads=[ps_t], writes=[cqT])
                        p.op('act', lambda e: e.copy(cqT[0:64, 1, :], ps_t[0:64, 5, :]), reads=[ps_t], writes=[cqT])
                        p.op('act', lambda e: e.copy(ckvT[:], ps_t[:, 6, :]), reads=[ps_t], writes=[ckvT])

                        def mmq(e):
                            e.matmul(ps_d[:, 0:384], cqT[:, 0, :], wuqb[:, 0, :], start=True, stop=False)
                            return e.matmul(ps_d[:, 0:384], cqT[0:64, 1, :], wuqb[0:64, 1, :], start=False, stop=True)
                        p.op('pe', mmq, reads=[cqT, wuqb], writes=[ps_d])
                        p.op('act', lambda e: e.activation(qd[:].rearrange("p h k -> p (h k)"), ps_d[:, 0:384], AF.Copy, scale=96.0 ** -0.5), reads=[ps_d], writes=[qd])
                        p.op('pe', lambda e: e.matmul(ps_d[:, 0:512], ckvT[:], wukvb[:], start=True, stop=True), reads=[ckvT, wukvb], writes=[ps_d])
                        p.op('act', lambda e: e.copy(kvd[:].rearrange("p h k -> p (h k)"), ps_d[:, 0:512]), reads=[ps_d], writes=[kvd])
                        cosb = ropet[:, 0:16].rearrange("p (a f) -> p a f", a=2)
                        sinb = ropet[:, 16:32].rearrange("p (a f) -> p a f", a=2)

                        p.op('dve', lambda e: e.tensor_copy(rin[:, 0:4, :], qd[:, :, 64:96]), reads=[qd], writes=[rin])
                        p.op('dve', lambda e: e.tensor_copy(rin[:, 4, :], pT[:, 3168:3200]), reads=RP + [rin], writes=[rin])
                        s5 = rin[:].rearrange("p h (a x f) -> p h a x f", a=2, x=2)
                        d5 = rout[:].rearrange("p h (a x f) -> p h a x f", a=2, x=2)
                        t5 = rtmp5[:].rearrange("p h (a x f) -> p h a x f", a=2, x=2)
                        u1 = s5[:, :, :, 0, :]; u2 = s5[:, :, :, 1, :]
                        cos5 = cosb.unsqueeze(1).to_broadcast([128, 5, 2, 8])
                        sin5 = sinb.unsqueeze(1).to_broadcast([128, 5, 2, 8])
                        p.op('dve', lambda e: e.tensor_mul(d5[:, :, :, 0, :], u1, cos5), reads=[rin, ropet], writes=[rout])
                        p.op('dve', lambda e: e.tensor_mul(t5[:, :, :, 0, :], u2, sin5), reads=[rin, ropet], writes=[rtmp5])
                        p.op('dve', lambda e: e.tensor_mul(d5[:, :, :, 1, :], u1, sin5), reads=[rin, ropet, rout], writes=[rout])
                        p.op('dve', lambda e: e.tensor_mul(t5[:, :, :, 1, :], u2, cos5), reads=[rin, ropet, rtmp5], writes=[rtmp5])
                        p.op('dve', lambda e: e.tensor_sub(d5[:, :, :, 0, :], d5[:, :, :, 0, :], t5[:, :, :, 0, :]), reads=[rout, rtmp5], writes=[rout])
                        p.op('dve', lambda e: e.tensor_add(d5[:, :, :, 1, :], d5[:, :, :, 1, :], t5[:, :, :, 1, :]), reads=[rout, rtmp5], writes=[rout])
                        p.op('dve', lambda e: e.tensor_copy(qdb[:, :, 0:64], qd[:, :, 0:64]), reads=[qd], writes=[qdb])
                        p.op('dve', lambda e: e.tensor_copy(qdb[:, :, 64:96], rout[:, 0:4, :]), reads=[rout, qdb], writes=[qdb])
                        p.op('dve', lambda e: e.tensor_copy(kdb[:, :, 0:64], kvd[:, :, 0:64]), reads=[kvd], writes=[kdb])
                        p.op('dve', lambda e: e.tensor_copy(kdb[:, :, 64:96], rout[:, 4:5, :].to_broadcast([128, 4, 32])), reads=[rout, kdb], writes=[kdb])
                        p.op('dve', lambda e: e.tensor_copy(vdb[:].rearrange("p (h k) -> p h k", h=4), kvd[:, :, 64:128]), reads=[kvd], writes=[vdb])
                        p.dma('sp', DV[tok, :], vdb[:], reads=[vdb], writes=[p.uk()])
                        for h in range(4):
                            transpose_b(ps_t[0:96, h, :], qdb[:, h, :], [qdb], [ps_t])
                        p.op('act', lambda e: e.copy(dT[:], ps_t[0:96, 0:4, :]), reads=[ps_t], writes=[dT])
                        p.dma('sp', DQT[:, :, tok].rearrange("h k t -> k h t"), dT[:], reads=[dT], writes=[p.uk()])
                        for h in range(4):
                            transpose_b(ps_t[0:96, 4 + h, :], kdb[:, h, :], [kdb], [ps_t])
                        p.op('act', lambda e: e.copy(dT[:], ps_t[0:96, 4:8, :]), reads=[ps_t], writes=[dT])
                        p.dma('sp', DKT[:, :, tok].rearrange("h k t -> k h t"), dT[:], reads=[dT], writes=[p.uk()])

                    if tt + 1 < NT:
                        p.interleave([lambda: front(tt + 1), chain_x, chain_y])
                    else:
                        p.interleave([chain_x, chain_y])

                front(0)
                for tt in range(NT):
                    back(tt)
                p.barrier()
            if dbg == 'P':
                break

            def normalize(ph_bufs, OT, LTp, n, dst_ap, dst_key):
                rl, on = ph_bufs
                p.op('dve', lambda e: e.reciprocal(rl[:, 0:n], LTp[:, 0:n]), reads=[LTp], writes=[rl])
                p.op('dve', lambda e: e.tensor_mul(on[:, 0:n], OT[:, 0:n], rl[:, 0:n]), reads=[OT, rl], writes=[on])
                p.dma('pool', dst_ap, on[:, 0:n], reads=[on], writes=[p.uk()])

            with ExitStack() as ph:
                GC = 4
                NG = NCH // GC
                LG = (L // 64) // GC
                masks = {}
                for d in 'fb':
                    masks[d] = p.sb(ph, [64, 64], F32)
                    p.dma('sp', masks[d][:], mask_d[d], writes=[masks[d]])
                T2 = [p.ps(ph, [64, 512]) for _ in range(2)]
                streams = []
                for si, (m, d) in enumerate((('A', 'f'), ('A', 'b'), ('C', 'f'), ('C', 'b'))):
                    st_ = dict(m=m, d=d, md=m + d)
                    for nm in ('qt', 'kt', 'qh', 'kh', 'v'):
                        st_[nm] = [p.sb(ph, [64, 4, 256], BF16) for _ in range(2)]
                    st_['dec'] = [p.sb(ph, [64, 4, 4], F32) for _ in range(2)]
                    st_['ob'] = [p.sb(ph, [64, 4, 256], F32) for _ in range(2)]
                    st_['at'] = p.sb(ph, [64, 256], BF16)
                    st_['S'] = p.sb(ph, [64, 4, 64], F32)
                    st_['Sb'] = p.sb(ph, [64, 4, 64], BF16)
                    st_['T1'] = p.ps(ph, [64, 512])
                    st_['pS'] = T2[si // 2][:, (si % 2) * 256:(si % 2) * 256 + 256]
                    st_['pSk'] = T2[si // 2]
                    if d == 'f':
                        st_['gorder'] = list(range(NG))
                    else:
                        st_['gorder'] = list(range(LG - 1, -1, -1)) + list(range(NG - 1, LG - 1, -1))
                    p.op('dve', lambda e: e.memset(st_['S'][:], 0.0), writes=[st_['S']])
                    p.op('dve', lambda e: e.memset(st_['Sb'][:], 0.0), writes=[st_['Sb']])
                    streams.append(st_)
                for gi in range(NG):
                    b2 = gi % 2
                    for st_ in streams:
                        m, d, md = st_['m'], st_['d'], st_['md']
                        g = st_['gorder'][gi]
                        tk = slice(g * 256, (g + 1) * 256)
                        p.dma('sp', st_['qt'][b2][:], mix[md + 'QT'][:, tk].rearrange("(h k) t -> k h t", k=64), reads=[md + 'QT'], writes=[st_['qt'][b2]])
                        p.dma('sp', st_['kt'][b2][:], mix[md + 'KT'][:, tk].rearrange("(h k) t -> k h t", k=64), reads=[md + 'KT'], writes=[st_['kt'][b2]])
                        p.dma('sp', st_['qh'][b2][:], mix[md + 'QH'][:, tk].rearrange("(h k) t -> k h t", k=64), reads=[md + 'QH'], writes=[st_['qh'][b2]])
                        p.dma('sp', st_['kh'][b2][:], mix[md + 'KH'][tk, :].rearrange("(c p) n -> p c n", p=64), reads=[md + 'KH'], writes=[st_['kh'][b2]])
                        p.dma('sp', st_['v'][b2][:], mix[m + 'V'][tk, :].rearrange("(c p) n -> p c n", p=64), reads=[m + 'V'], writes=[st_['v'][b2]])
                        p.dma('sp', st_['dec'][b2][:], mix[md + 'DEC'][:, g * 4:(g + 1) * 4].rearrange("(h k) t -> k h t", k=64), reads=[md + 'DEC'], writes=[st_['dec'][b2]])
                    for ci in range(GC):
                        for st_ in streams:
                            d = st_['d']
                            c = ci if d == 'f' else GC - 1 - ci
                            cs = slice(c * 64, (c + 1) * 64)
                            qt, kt, qh, kh, v, dec, ob = (st_[n][b2] for n in ('qt', 'kt', 'qh', 'kh', 'v', 'dec', 'ob'))
                            at, S, Sb, T1, pS, pSk = st_['at'], st_['S'], st_['Sb'], st_['T1'], st_['pS'], st_['pSk']
                            pa = T1[:, 0:256]; po = T1[:, 256:512]
                            ka = T1; ko = T1

                            def mmA(e):
                                for h in range(4):
                                    r = e.matmul(pa[:, h * 64:(h + 1) * 64], kt[:, h, cs], qt[:, h, cs], start=True, stop=True)
                                return r
                            p.op('pe', mmA, reads=[kt, qt], writes=[ka])
                            p.op('dve', lambda e: e.tensor_scalar(at[:], pa, 1e30, -1e30, ALU.min, ALU.max), reads=[ka], writes=[at])
                            p.op('dve', lambda e: e.tensor_mul(at[:].rearrange("p (h i) -> p h i", h=4), at[:].rearrange("p (h i) -> p h i", h=4),
                                                              masks[d][:].unsqueeze(1).to_broadcast([64, 4, 64])), reads=[at, masks[d]], writes=[at])

                            def mmO(e):
                                for h in range(4):
                                    e.matmul(po[:, h * 64:(h + 1) * 64], at[:, h * 64:(h + 1) * 64], v[:, c, h * 64:(h + 1) * 64], start=True, stop=False)
                                    r = e.matmul(po[:, h * 64:(h + 1) * 64], qh[:, h, cs], Sb[:, h, :], start=False, stop=True)
                                return r
                            p.op('pe', mmO, reads=[at, v, qh, Sb], writes=[ko])
                            p.op('act', lambda e: e.copy(ob[:, c, :], po), reads=[ko], writes=[ob])

                            def mmS(e):
                                for h in range(4):
                                    r = e.matmul(pS[:, h * 64:(h + 1) * 64], kh[:, c, h * 64:(h + 1) * 64], v[:, c, h * 64:(h + 1) * 64], start=True, stop=True)
                                return r
                            p.op('pe', mmS, reads=[kh, v], writes=[pSk])
                            p.op('dve', lambda e: e.tensor_mul(S[:], S[:], dec[:, :, c:c + 1].to_broadcast([64, 4, 64])), reads=[S, dec], writes=[S])
                            p.op('dve', lambda e: e.tensor_add(S[:], S[:], pS.rearrange("p (h v) -> p h v", h=4)), reads=[S, pSk], writes=[S])
                            p.op('act', lambda e: e.copy(Sb[:], S[:]), reads=[S], writes=[Sb])
                    for st_ in streams:
                        g = st_['gorder'][gi]
                        tk = slice(g * 256, (g + 1) * 256)
                        p.dma('pool', mix[st_['md'] + 'O'][tk, :].rearrange("(c p) n -> p c n", p=64), st_['ob'][b2][:], reads=[st_['ob'][b2]], writes=[p.uk()])
                p.barrier()
            if dbg == 'R':
                break
            with ExitStack() as ph:
                ngb = {}
                for m, src in (('A', hgrn_ng), ('C', gla_ng)):
                    ngb[m] = p.sb(ph, [128, 256], F32)
                    p.dma('sp', ngb[m][:], src[layer:layer + 1, :].to_broadcast([128, 256]), writes=[ngb[m]])
                def ro_stream(m, roff):
                    of = [p.sb(ph, [128, 256], F32) for _ in range(2)]
                    ob_ = [p.sb(ph, [128, 256], F32) for _ in range(2)]
                    gg = [p.sb(ph, [128, 256], F32) for _ in range(2)]
                    sq = p.sb(ph, [128, 256], F32)
                    ss = p.sb(ph, [128, 4], F32)
                    yb = p.sb(ph, [128, 256], BF16)
                    yT = p.sb(ph, [128, 2, 128], BF16)
                    pst = p.ps(ph, [128, 2, 128], BF16)

                    def run():
                        it = 0
                        for tt in range(T0, NT):
                            tok = slice(tt * 128, (tt + 1) * 128)
                            a = of[it % 2]; b2 = ob_[it % 2]; g = gg[it % 2]
                            it += 1
                            p.dma('sp', a[:], mix[m + 'fO'][tok, :], reads=[m + 'fO'], writes=[a])
                            p.dma('sp', b2[:], mix[m + 'bO'][tok, :], reads=[m + 'bO'], writes=[b2])
                            p.dma('sp', g[:], mix[m + 'G'][tok, :], reads=[m + 'G'], writes=[g])
                            p.op('dve', lambda e: e.tensor_add(a[:], a[:], b2[:]), reads=[a, b2], writes=[a])
                            p.op('dve', lambda e: e.tensor_mul(sq[:], a[:], a[:]), reads=[a], writes=[sq])
                            p.op('dve', lambda e: e.tensor_reduce(ss[:], sq[:].rearrange("p (h v) -> p h v", h=4), AX.X, ALU.add), reads=[sq], writes=[ss])
                            p.op('dve', lambda e: e.tensor_scalar(ss[:], ss[:], 1.0 / 64, EPS, ALU.mult, ALU.add), reads=[ss], writes=[ss])
                            rsqrt(ss[:], [ss])
                            p.op('dve', lambda e: e.tensor_mul(a[:].rearrange("p (h v) -> p h v", h=4), a[:].rearrange("p (h v) -> p h v", h=4),
                                                              ss[:].unsqueeze(2).to_broadcast([128, 4, 64])), reads=[a, ss], writes=[a])
                            p.op('dve', lambda e: e.tensor_mul(a[:], a[:], ngb[m][:]), reads=[a, ngb[m]], writes=[a])
                            p.op('dve', lambda e: e.tensor_mul(yb[:], a[:], g[:]), reads=[a, g], writes=[yb])

                            def tr(e):
                                for ct in range(2):
                                    r = e.transpose(pst[:, ct, :], yb[:, ct * 128:(ct + 1) * 128], ident_b[:])
                                return r
                            p.op('pe', tr, reads=[yb, ident_b], writes=[pst])
                            p.op('act', lambda e: e.copy(yT[:], pst[:]), reads=[pst], writes=[yT])
                            p.dma('pool', CATT[roff:roff + 256, tok].rearrange("(c p) t -> p c t", p=128), yT[:], reads=[yT], writes=[p.uk()])
                    return run
                p.interleave([ro_stream('A', 0), ro_stream('C', 512)])
                p.barrier()

            with ExitStack() as ph:
                KTn = p.sb(ph, [64, 4, T], BF16)
                QTn = p.sb(ph, [64, 4, T], BF16)
                p.dma('sp', KTn[:], BKT.rearrange("(h d) t -> d h t", d=64), reads=['BKT'], writes=[KTn])
                p.dma('sp', QTn[:], BQT.rearrange("(h d) t -> d h t", d=64), reads=['BQT'], writes=[QTn])
                V1l = p.sb(ph, [64, ROWS, 256], BF16)
                V1c = p.sb(ph, [128, LT, 256], BF16)
                for r8 in range(0, ROWS, 8):
                    p.dma('sp', V1l[:, r8:r8 + 8, :], BV[L + r8 * 64:L + (r8 + 8) * 64, :].rearrange("(r w) c -> w r c", w=64), reads=['BV'], writes=[V1l])
                p.dma('sp', V1c[:], BV[0:L, :].rearrange("(n p) c -> p n c", p=128), reads=['BV'], writes=[V1c])
                EBs = [p.sb(ph, [64, 8, 512], F32) for _ in range(2)]
                psS = [p.ps(ph, [64, 512]) for _ in range(2)]
                psC = [p.ps(ph, [128, 512]) for _ in range(2)]
                OTp = p.ps(ph, [64, 512])
                LTp = p.ps(ph, [64, 512])
                nb_ = (p.sb(ph, [64, 512], F32), p.sb(ph, [64, 512], BF16))
                pe_ = [p.sb(ph, [64, 512], F32) for _ in range(2)]
                pb16 = [p.sb(ph, [64, 512], BF16) for _ in range(2)]
                pc16 = [p.sb(ph, [128, 512], BF16) for _ in range(2)]
                rows_ = [(h, r) for h in range(4) for r in range(ROWS)]

                def na_front(i):
                    h, r = rows_[i]
                    EB = EBs[h % 2]
                    if r == 0:
                        p.dma('sp', EB[:], na_tab[layer, h].rearrange("c w x -> w c x"), writes=[EB])
                        p.op('act', lambda e: e.activation(EB[:], EB[:], AF.Exp), reads=[EB], writes=[EB])
                    rs = min(max(r - 4, 0), ROWS - 8)
                    cls = r - rs
                    qc = slice(L + r * 64, L + (r + 1) * 64)
                    ps = psS[i % 2]; pc = psC[i % 2]; pe = pe_[i % 2]; pbb = pb16[i % 2]; pcs = pc16[i % 2]

                    def mmS(e):
                        for j in range(8):
                            kc = slice(L + (rs + j) * 64, L + (rs + j + 1) * 64)
                            r_ = e.matmul(ps[:, j * 64:(j + 1) * 64], KTn[:, h, kc], QTn[:, h, qc], start=True, stop=True)
                        for kt in range(LT):
                            r_ = e.matmul(pc[:, kt * 64:(kt + 1) * 64], KTn[:, h, kt * 128:(kt + 1) * 128], QTn[:, h, qc], start=True, stop=True)
                        return r_
                    p.op('pe', mmS, reads=[KTn, QTn], writes=[ps, pc])
                    p.op('act', lambda e: e.activation(pe[:], ps[:], AF.Exp), reads=[ps], writes=[pe])
                    p.op('act', lambda e: e.activation(pcs[:, 0:LT * 64], pc[:, 0:LT * 64], AF.Exp), reads=[pc], writes=[pcs])
                    p.op('dve', lambda e: e.tensor_mul(pbb[:], pe[:], EB[:, cls, :]), reads=[pe, EB], writes=[pbb])

                def na_back(i):
                    h, r = rows_[i]
                    hv = slice(h * 64, (h + 1) * 64)
                    rs = min(max(r - 4, 0), ROWS - 8)
                    ri = r % 8
                    r0 = r - ri
                    pbb = pb16[i % 2]; pcs = pc16[i % 2]

                    def mmV(e):
                        for j in range(8):
                            e.matmul(OTp[:, ri * 64:(ri + 1) * 64], V1l[:, rs + j, hv], pbb[:, j * 64:(j + 1) * 64], start=(j == 0), stop=False)
                        for kt in range(LT):
                            e.matmul(OTp[:, ri * 64:(ri + 1) * 64], V1c[:, kt, hv], pcs[:, kt * 64:(kt + 1) * 64], start=False, stop=(kt == LT - 1))
                        for j in range(8):
                            e.matmul(LTp[:, ri * 64:(ri + 1) * 64], ones_b[0:64, 0:64], pbb[:, j * 64:(j + 1) * 64], start=(j == 0), stop=False)
                        for kt in range(LT):
                            r_ = e.matmul(LTp[:, ri * 64:(ri + 1) * 64], ones_b[:, 0:64], pcs[:, kt * 64:(kt + 1) * 64], start=False, stop=(kt == LT - 1))
                        return r_
                    p.op('pe', mmV, reads=[V1l, V1c, pbb, pcs, ones_b], writes=[OTp, LTp])
                    if ri == 7:
                        normalize(nb_, OTp, LTp, 512, CATT[256 + h * 64:256 + (h + 1) * 64, L + r0 * 64:L + r0 * 64 + 512], 'CATT')


                def na_ctx(h):
                    hv = slice(h * 64, (h + 1) * 64)
                    for kt in range(LT):
                        pc = psC[kt % 2]; pcs = pc16[kt % 2]
                        p.op('pe', lambda e: e.matmul(pc[:, 0:L], KTn[:, h, kt * 128:(kt + 1) * 128], QTn[:, h, 0:L], start=True, stop=True),
                             reads=[KTn, QTn], writes=[pc])
                        p.op('act', lambda e: e.activation(pcs[:, 0:L], pc[:, 0:L], AF.Exp), reads=[pc], writes=[pcs])
                        p.op('pe', lambda e: e.matmul(OTp[:, 0:L], V1c[:, kt, hv], pcs[:, 0:L], start=(kt == 0), stop=(kt == LT - 1)),
                             reads=[V1c, pcs], writes=[OTp])
                        p.op('pe', lambda e: e.matmul(LTp[:, 0:L], ones_b[:, 0:64], pcs[:, 0:L], start=(kt == 0), stop=(kt == LT - 1)),
                             reads=[ones_b, pcs], writes=[LTp])
                    normalize(nb_, OTp, LTp, L, CATT[256 + h * 64:256 + (h + 1) * 64, 0:L], 'CATT')

                if keep_ctx:
                    for h in range(4):
                        na_ctx(h)
                na_front(0)
                for i in range(len(rows_)):
                    if i + 1 < len(rows_):
                        na_front(i + 1)
                    na_back(i)
                p.barrier()

            with ExitStack() as ph:
                KTd = p.sb(ph, [96, 4, T], BF16)
                p.dma('sp', KTd[:], DKT.rearrange("h k t -> k h t"), reads=['DKT'], writes=[KTd])
                V1 = p.sb(ph, [128, NT, 256], BF16)
                for n8 in range(0, NT, 8):
                    n9 = min(NT, n8 + 8)
                    p.dma('sp', V1[:, n8:n9, :], DV[n8 * 128:n9 * 128, :].rearrange("(n p) c -> p n c", p=128), reads=['DV'], writes=[V1])
                QTt = [p.sb(ph, [96, 512], BF16) for _ in range(2)]
                psS = [p.ps(ph, [128, 2, 512]) for _ in range(3)]
                OTp = p.ps(ph, [64, 512])
                LTp = p.ps(ph, [64, 512])
                nb_ = (p.sb(ph, [64, 512], F32), p.sb(ph, [64, 512], BF16))
                pe16 = [p.sb(ph, [128, 2, 512], BF16) for _ in range(3)]
                steps = []
                qn = 0
                for h in range(4):
                    qtiles = []
                    if keep_ctx:
                        qtiles.append((0, L, list(range(LT))))
                    for q0 in range(L, T, 512):
                        qtiles.append((q0, min(512, T - q0), list(range(NT))))
                    for (q0, nq, kts) in qtiles:
                        pairs = [kts[i:i + 2] for i in range(0, len(kts), 2)]
                        for ki, kp in enumerate(pairs):
                            steps.append((h, q0, nq, kp, ki == 0, ki == len(pairs) - 1, qn))
                        qn += 1

                def mla_front(i):
                    h, q0, nq, kp, first, last, qi = steps[i]
                    qb = QTt[qi % 2]
                    if first:
                        p.dma('sp', qb[:, 0:nq], DQT[h, :, q0:q0 + nq], reads=['DQT'], writes=[qb])
                    ps = psS[i % 3]; pe = pe16[i % 3]
                    nk = len(kp)

                    def mms(e):
                        for j, kt in enumerate(kp):
                            r = e.matmul(ps[:, j, 0:nq], KTd[:, h, kt * 128:(kt + 1) * 128], qb[:, 0:nq], start=True, stop=True)
                        return r
                    p.op('pe', mms, reads=[KTd, qb], writes=[ps])
                    p.op('act', lambda e: e.activation(pe[:, 0:nk, 0:nq], ps[:, 0:nk, 0:nq], AF.Exp), reads=[ps], writes=[pe])

                def mla_back(i):
                    h, q0, nq, kp, first, last, qi = steps[i]
                    pe = pe16[i % 3]
                    nk = len(kp)

                    def mmv(e):
                        for j, kt in enumerate(kp):
                            e.matmul(OTp[:, 0:nq], V1[:, kt, h * 64:(h + 1) * 64], pe[:, j, 0:nq], start=(first and j == 0), stop=(last and j == nk - 1))
                        for j, kt in enumerate(kp):
                            r = e.matmul(LTp[:, 0:nq], ones_b[:, 0:64], pe[:, j, 0:nq], start=(first and j == 0), stop=(last and j == nk - 1))
                        return r
                    p.op('pe', mmv, reads=[V1, pe, ones_b], writes=[OTp, LTp])
                    if last:
                        normalize(nb_, OTp, LTp, nq, CATT[768 + h * 64:768 + (h + 1) * 64, q0:q0 + nq], 'CATT')

                LOOK = 2
                for i in range(min(LOOK, len(steps))):
                    mla_front(i)
                for i in range(len(steps)):
                    if i + LOOK < len(steps):
                        mla_front(i + LOOK)
                    mla_back(i)
                p.barrier()
            if dbg == 'M':
                break

            W01 = p.sb(lay, [128, NT, 2], F32)
            DESTi = p.sb(lay, [128, NT, 2], I32)
            IDXGU = p.sb(lay, [128, NBLK, 8], I32)
            IDXDN = p.sb(lay, [128, NBLK, 2], I32)
            with ExitStack() as ph:
                woutb = p.sb(ph, [128, 8, D], BF16)
                for kc in range(8):
                    p.dma('pool', woutb[:, kc, :], w_out[layer, kc * 128:(kc + 1) * 128, :], writes=[(woutb.name, kc)])
                RW = [(woutb.name, kc) for kc in range(8)]
                wr = p.sb(ph, [128, 8, 36], F32)
                p.dma('sp', wr[:], w_r[layer].rearrange("(k p) n -> p k n", p=128), writes=[wr])
                brb = p.sb(ph, [128, 36], F32)
                p.dma('sp', brb[:], b_r[layer:layer + 1, :].to_broadcast([128, 36]), writes=[brb])
                g2 = p.sb(ph, [128, D], F32)
                p.dma('sp', g2[:], norm2_g[layer:layer + 1, :].to_broadcast([128, D]), writes=[g2])
                M2 = [p.sb(ph, [128, D], F32) for _ in range(2)]
                G2 = [p.sb(ph, [128, D], F32) for _ in range(2)]
                B2 = [p.sb(ph, [128, D], F32) for _ in range(2)]
                for s in range(2):
                    p.dma('sp', M2[s][:], MOD[layer, s:s + 1, 2 * D:3 * D].to_broadcast([128, D]), writes=[M2[s]])
                    p.dma('sp', B2[s][:], MOD[layer, s:s + 1, 3 * D:4 * D].to_broadcast([128, D]), writes=[B2[s]])
                    p.dma('sp', G2[s][:], MOD[layer, s:s + 1, 4 * D:5 * D].to_broadcast([128, D]), writes=[G2[s]])
                    p.op('dve', lambda e: e.scalar_tensor_tensor(G2[s][:], G2[s][:], 1.0, g2[:], ALU.add, ALU.mult), reads=[G2[s], g2], writes=[G2[s]])
                M0 = p.sb(ph, [128, NT, 32], F32)
                M1 = p.sb(ph, [128, NT, 32], F32)
                Mh = p.sb(ph, [128, NT, 32], BF16)
                p.op('dve', lambda e: e.memset(M0[:], 0.0), writes=[M0])
                p.op('dve', lambda e: e.memset(M1[:], 0.0), writes=[M1])
                p.op('dve', lambda e: e.memset(Mh[:], 0.0), writes=[Mh])
                p.op('dve', lambda e: e.memset(W01[:], 0.0), writes=[W01])
                Bs = []
                for si in range(2):
                    Bs.append(dict(cb=p.sb(ph, [128, 8, 128], BF16), xb=p.sb(ph, [128, D], F32), tmp=p.sb(ph, [128, D], F32), xn=p.sb(ph, [128, D], F32),
                                   h2=p.sb(ph, [128, D], F32), h2b=p.sb(ph, [128, D], BF16), h2T=p.sb(ph, [128, 8, 128], F32), sx=p.sb(ph, [128, 16], F32),
                                   lg=p.sb(ph, [128, 36], F32), r8=p.sb(ph, [128, 4, 8], F32), sel=p.sb(ph, [128, 8], F32), sel2=p.sb(ph, [128, 8], F32),
                                   oh=p.sb(ph, [128, 3, 8], F32), po=p.ps(ph, [128, 512]), pt=p.ps(ph, [128, 4, 128]), pl=p.ps(ph, [128, 64])))
                pl = Bs[0]['pl']

                def o_stream(si):
                    def run():
                        for tt in range(T0 + si, NT, 2):
                            s = 1 if tt < LT else 0
                            tok = slice(tt * 128, (tt + 1) * 128)
                            cb, xb, tmp, xn, h2, h2b, h2T, sx, lg, r8, sel, sel2, oh, po, pt, pl = (Bs[si][k] for k in ('cb', 'xb', 'tmp', 'xn', 'h2', 'h2b', 'h2T', 'sx', 'lg', 'r8', 'sel', 'sel2', 'oh', 'po', 'pt', 'pl'))
                            p.dma('sp', cb[:], CATT[:, tok].rearrange("(c p) t -> p c t", p=128), reads=['CATT'], writes=[cb])
                            p.dma('sp', xb[:], X[tok, :], reads=[('X', tt)], writes=[xb])

                            for nb in range(2):
                                def mm(e):
                                    for c in range(8):
                                        r = e.matmul(po[:], cb[:, c, :], woutb[:, c, nb * 512:(nb + 1) * 512], start=(c == 0), stop=(c == 7))
                                    return r
                                p.op('pe', mm, reads=[cb] + RW, writes=[po])
                                p.op('dve', lambda e: e.tensor_mul(tmp[:, nb * 512:(nb + 1) * 512], po[:], M2[s][:, nb * 512:(nb + 1) * 512]), reads=[po, M2[s]], writes=[tmp])
                            p.op('dve', lambda e: e.tensor_add(xn[:], xb[:], tmp[:]), reads=[xb, tmp], writes=[xn])
                            p.dma('pool', X[tok, :], xn[:], reads=[xn], writes=[('X', tt)])
                            p.op('act', lambda e: e.activation(tmp[:], xn[:], AF.Square, accum_out=sx[:, 0:1]), reads=[xn], writes=[tmp, sx])
                            p.op('dve', lambda e: e.tensor_scalar(sx[:, 1:2], sx[:, 0:1], 1.0 / D, EPS, ALU.mult, ALU.add), reads=[sx], writes=[sx])
                            rsqrt(sx[:, 1:2], [sx])
                            p.op('dve', lambda e: e.scalar_tensor_tensor(tmp[:], xn[:], sx[:, 1:2], G2[s][:], ALU.mult, ALU.mult), reads=[xn, sx, G2[s]], writes=[tmp])
                            p.op('dve', lambda e: e.tensor_add(h2[:], tmp[:], B2[s][:]), reads=[tmp, B2[s]], writes=[h2])
                            p.op('act', lambda e: e.copy(h2b[:], h2[:]), reads=[h2], writes=[h2b])
                            p.dma('pool', H2[tok, :], h2b[:], reads=[h2b], writes=['H2'])
                            for rnd in range(2):
                                def tr(e):
                                    for k4 in range(4):
                                        kc = rnd * 4 + k4
                                        r = e.transpose(pt[:, k4, :], h2[:, kc * 128:(kc + 1) * 128], ident_f[:])
                                    return r
                                p.op('pe', tr, reads=[h2, ident_f], writes=[pt])
                                p.op('act', lambda e: e.copy(h2T[:, rnd * 4:(rnd + 1) * 4, :], pt[:]), reads=[pt], writes=[h2T])

                            def mmr(e):
                                for kc in range(8):
                                    r = e.matmul(pl[:, 0:36], h2T[:, kc, :], wr[:, kc, :], start=(kc == 0), stop=(kc == 7))
                                return r
                            p.op('pe', mmr, reads=[h2T, wr], writes=[pl])
                            p.op('dve', lambda e: e.tensor_add(lg[:], pl[:, 0:36], brb[:]), reads=[pl, brb], writes=[lg])
                            K_ = [sx, lg, r8, sel, sel2, oh]
                            p.op('dve', lambda e: e.tensor_reduce(sx[:, 2:3], lg[:, 0:4], AX.X, ALU.max), reads=K_, writes=[sx])
                            p.op('dve', lambda e: e.tensor_scalar(oh[:, 0, 0:4], lg[:, 0:4], sx[:, 2:3], None, ALU.is_equal), reads=K_, writes=[oh])
                            p.op('dve', lambda e: e.tensor_scalar(sx[:, 3:4], sx[:, 2:3], -1.0, None, ALU.mult), reads=K_, writes=[sx])
                            p.op('act', lambda e: e.activation(sel2[:, 0:4], lg[:, 0:4], AF.Exp, bias=sx[:, 3:4], accum_out=sx[:, 4:5]), reads=K_, writes=[sel2, sx])
                            p.op('dve', lambda e: e.reciprocal(sx[:, 5:6], sx[:, 4:5]), reads=K_, writes=[sx])
                            p.op('dve', lambda e: e.tensor_mul(r8[:], lg[:, 4:36].rearrange("p (g j) -> p g j", g=4), oh[:, 0, 0:4].unsqueeze(2).to_broadcast([128, 4, 8])), reads=K_, writes=[r8])
                            p.op('dve', lambda e: e.tensor_reduce(sel[:], r8[:].rearrange("p g j -> p j g"), AX.X, ALU.add), reads=K_, writes=[sel])
                            p.op('dve', lambda e: e.tensor_reduce(sx[:, 6:7], sel[:], AX.X, ALU.max), reads=K_, writes=[sx])
                            p.op('dve', lambda e: e.tensor_scalar(oh[:, 1, :], sel[:], sx[:, 6:7], None, ALU.is_equal), reads=K_, writes=[oh])
                            p.op('dve', lambda e: e.scalar_tensor_tensor(sel2[:], oh[:, 1, :], -1e30, sel[:], ALU.mult, ALU.add), reads=K_, writes=[sel2])
                            p.op('dve', lambda e: e.tensor_reduce(sx[:, 7:8], sel2[:], AX.X, ALU.max), reads=K_, writes=[sx])
                            p.op('dve', lambda e: e.tensor_scalar(oh[:, 2, :], sel2[:], sx[:, 7:8], None, ALU.is_equal), reads=K_, writes=[oh])
                            p.op('dve', lambda e: e.tensor_sub(sx[:, 8:9], sx[:, 6:7], sx[:, 7:8]), reads=K_, writes=[sx])
                            p.op('act', lambda e: e.activation(sx[:, 9:10], sx[:, 8:9], AF.Sigmoid), reads=K_, writes=[sx])
                            p.op('dve', lambda e: e.tensor_mul(W01[:, tt, 0:1], sx[:, 9:10], sx[:, 5:6]), reads=K_, writes=[W01])
                            p.op('dve', lambda e: e.tensor_sub(W01[:, tt, 1:2], sx[:, 5:6], W01[:, tt, 0:1]), reads=K_ + [W01], writes=[W01])
                            for k_, Mk in ((1, M0), (2, M1)):
                                p.op('dve', lambda e: e.tensor_mul(Mk[:, tt, :].rearrange("p (g j) -> p g j", g=4), oh[:, 0, 0:4].unsqueeze(2).to_broadcast([128, 4, 8]),
                                                                  oh[:, k_, :].unsqueeze(1).to_broadcast([128, 4, 8])), reads=K_, writes=[Mk])
                            p.op('dve', lambda e: e.tensor_add(Mh[:, tt, :], M0[:, tt, :], M1[:, tt, :]), reads=[M0, M1], writes=[Mh])
                    return run
                p.interleave([o_stream(0), o_stream(1)])
                lsb = p.sb(ph, [128, 128], BF16)
                lsf = p.sb(ph, [128, 128], F32)
                p.dma('sp', lsf[:], lstrict_d, writes=[lsf])
                p.op('dve', lambda e: e.tensor_copy(lsb[:], lsf[:]), reads=[lsf], writes=[lsb])
                carry = p.sb(ph, [128, 32], F32)
                RANK = p.sb(ph, [128, NT, 32], F32)
                p.op('dve', lambda e: e.memset(carry[:], 0.0), writes=[carry])
                p.op('dve', lambda e: e.memset(RANK[:], 0.0), writes=[RANK])
                for tt in range(T0, NT):
                    def mmk(e):
                        e.matmul(pl[:, 0:32], lsb[:], Mh[:, tt, :], start=True, stop=True)
                        return e.matmul(pl[:, 32:64], ones_b[:], Mh[:, tt, :], start=True, stop=True)
                    p.op('pe', mmk, reads=[lsb, ones_b, Mh], writes=[pl])
                    p.op('dve', lambda e: e.tensor_add(RANK[:, tt, :], pl[:, 0:32], carry[:]), reads=[pl, carry], writes=[RANK])
                    p.op('dve', lambda e: e.tensor_add(carry[:], carry[:], pl[:, 32:64]), reads=[pl, carry], writes=[carry])
                NK = (2 * T) // MOEB + 2
                thr = p.sb(ph, [128, NK], F32)
                p.op('pool', lambda e: e.iota(thr[:], [[MOEB, NK]], base=0, channel_multiplier=0, allow_small_or_imprecise_dtypes=True), writes=[thr])
                cmp_ = p.sb(ph, [128, 32, NK], F32)
                padded = p.sb(ph, [128, 32], F32)
                pend = [p.sb(ph, [128, 32], F32) for _ in range(2)]
                p.op('dve', lambda e: e.tensor_tensor(cmp_[:], carry[:].unsqueeze(2).to_broadcast([128, 32, NK]), thr[:].unsqueeze(1).to_broadcast([128, 32, NK]), ALU.is_gt),
                     reads=[carry, thr], writes=[cmp_])
                p.op('dve', lambda e: e.tensor_reduce(padded[:], cmp_[:], AX.X, ALU.add), reads=[cmp_], writes=[padded])
                p.op('dve', lambda e: e.tensor_scalar(padded[:], padded[:], float(MOEB), None, ALU.mult), reads=[padded], writes=[padded])
                p.op('dve', lambda e: e.tensor_copy(pend[0][:], padded[:]), reads=[padded], writes=[pend[0]])
                cur = 0
                for sft in (1, 2, 4, 8, 16):
                    a = pend[cur]; b2 = pend[1 - cur]
                    p.op('dve', lambda e: e.tensor_copy(b2[:, 0:sft], a[:, 0:sft]), reads=[a], writes=[b2])
                    p.op('dve', lambda e: e.tensor_add(b2[:, sft:32], a[:, sft:32], a[:, 0:32 - sft]), reads=[a, b2], writes=[b2])
                    cur = 1 - cur
                pe_ = pend[cur]
                pstart = pend[1 - cur]
                p.op('dve', lambda e: e.tensor_sub(pstart[:], pe_[:], padded[:]), reads=[pe_, padded], writes=[pstart])
                destf = p.sb(ph, [128, NT, 2], F32)
                big = p.sb(ph, [128, NT, 32], F32)
                p.op('dve', lambda e: e.tensor_add(RANK[:], RANK[:], pstart[:].unsqueeze(1).to_broadcast([128, NT, 32])), reads=[RANK, pstart], writes=[RANK])
                for k_, Mk in ((0, M0), (1, M1)):
                    p.op('dve', lambda e: e.tensor_mul(big[:], RANK[:], Mk[:]), reads=[RANK, Mk], writes=[big])
                    p.op('dve', lambda e: e.tensor_reduce(destf[:, :, k_], big[:], AX.X, ALU.add), reads=[big], writes=[destf])
                p.op('dve', lambda e: e.tensor_copy(DESTi[:], destf[:]), reads=[destf], writes=[DESTi])
                bvals = p.sb(ph, [128, NBLK], F32)
                p.op('pool', lambda e: e.iota(bvals[:], [[MOEB, NBLK]], base=0, channel_multiplier=0, allow_small_or_imprecise_dtypes=True), writes=[bvals])
                cmpb = p.sb(ph, [128, NBLK, 32], F32)
                bex = p.sb(ph, [128, NBLK], F32)
                p.op('dve', lambda e: e.tensor_tensor(cmpb[:], pe_[:].unsqueeze(1).to_broadcast([128, NBLK, 32]), bvals[:].unsqueeze(2).to_broadcast([128, NBLK, 32]), ALU.is_le),
                     reads=[pe_, bvals], writes=[cmpb])
                p.op('dve', lambda e: e.tensor_reduce(bex[:], cmpb[:], AX.X, ALU.add), reads=[cmpb], writes=[bex])
                p.op('dve', lambda e: e.tensor_scalar(bex[:], bex[:], float(NEXP - 1), None, ALU.min), reads=[bex], writes=[bex])
                pcol = p.sb(ph, [128, 1], F32)
                p.dma('sp', pcol[:], pcol_d, writes=[pcol])
                idxf = p.sb(ph, [128, NBLK, 8], F32)
                base = p.sb(ph, [128, NBLK], F32)
                p.op('dve', lambda e: e.tensor_scalar(base[:], bex[:], float(D), float(layer * NEXP * D), ALU.mult, ALU.add), reads=[bex], writes=[base])
                for kc in range(8):
                    p.op('dve', lambda e: e.tensor_scalar(idxf[:, :, kc], base[:], pcol[:, 0:1], float(kc * 128), ALU.add, ALU.add), reads=[base, pcol], writes=[idxf])
                p.op('dve', lambda e: e.tensor_copy(IDXGU[:], idxf[:]), reads=[idxf], writes=[IDXGU])
                p.op('dve', lambda e: e.tensor_scalar(base[:], bex[:], 256.0, float(layer * NEXP * 256), ALU.mult, ALU.add), reads=[bex], writes=[base])
                for fc in range(2):
                    p.op('dve', lambda e: e.tensor_scalar(idxf[:, :, fc], base[:], pcol[:, 0:1], float(fc * 128), ALU.add, ALU.add), reads=[base, pcol, IDXGU], writes=[idxf])
                p.op('dve', lambda e: e.tensor_copy(IDXDN[:], idxf[:, :, 0:2]), reads=[idxf], writes=[IDXDN])
                zt = p.sb(ph, [128, 8, D], BF16)
                p.op('dve', lambda e: e.memset(zt[:], 0.0), writes=[zt])
                for s0 in range(0, NSLOT, 1024):
                    n_ = min(1024, NSLOT - s0) // 128
                    p.dma('sp', XS[s0:s0 + n_ * 128, :].rearrange("(n p) d -> p n d", p=128), zt[:, 0:n_, :], reads=[zt], writes=['XS'])
                hd = [p.sb(ph, [128, D], BF16) for _ in range(2)]
                for tt in range(T0, NT):
                    hb_ = hd[tt % 2]
                    p.dma('sp', hb_[:], H2[tt * 128:(tt + 1) * 128, :], reads=['H2'], writes=[hb_])
                    for k_ in range(2):
                        p.idma(XS[:, :], bass.IndirectOffsetOnAxis(ap=DESTi[:, tt, k_:k_ + 1], axis=0), hb_[:], None,
                               reads=[hb_, DESTi, 'XS'], writes=[p.uk()])
                p.barrier()
            if dbg == 'O':
                break

            with ExitStack() as ph:
                sets = []
                for si in range(2):
                    B_ = dict(
                        wf=p.sb(ph, [128, 8, 512], F32), df=p.sb(ph, [128, 2, D], F32),
                        wgub=p.sb(ph, [128, 8, 512], BF16), wdnb=p.sb(ph, [128, 2, D], BF16),
                        xb=p.sb(ph, [128, 2, D], BF16), xsT=p.sb(ph, [128, 8, 256], BF16),
                        sg=p.sb(ph, [128, 2, 256], F32), hT=p.sb(ph, [128, 2, 256], BF16),
                        ysb=[p.sb(ph, [128, D], F32) for _ in range(2)],
                        pg=p.ps(ph, [128, 2, 256]), py=p.ps(ph, [128, D]), pst=p.ps(ph, [128, 8, 128], BF16))
                    sets.append(B_)

                def blk(b, B_):
                    wf, df, wgub, wdnb, xb, xsT, sg, hT, ysb, pg, py, pst = (B_[k] for k in ('wf', 'df', 'wgub', 'wdnb', 'xb', 'xsT', 'sg', 'hT', 'ysb', 'pg', 'py', 'pst'))
                    for kc in range(8):
                        p.idma(wf[:, kc, :], None, w_gu[:, :], bass.IndirectOffsetOnAxis(ap=IDXGU[:, b, kc:kc + 1], axis=0), reads=[IDXGU], writes=[(wf.name, kc)])
                    for fc in range(2):
                        p.idma(df[:, fc, :], None, w_dn[:, :], bass.IndirectOffsetOnAxis(ap=IDXDN[:, b, fc:fc + 1], axis=0), reads=[IDXDN], writes=[(df.name, fc)])
                    s0 = b * MOEB
                    p.dma('sp', xb[:], XS[s0:s0 + 256, :].rearrange("(s p) d -> p s d", p=128), reads=['XS'], writes=[xb])
                    p.op('act', lambda e: e.copy(wgub[:, 0:4, :], wf[:, 0:4, :]), reads=[(wf.name, kc) for kc in range(4)], writes=[(wgub.name, 0)])
                    p.op('dve', lambda e: e.tensor_copy(wgub[:, 4:8, :], wf[:, 4:8, :]), reads=[(wf.name, kc) for kc in range(4, 8)], writes=[(wgub.name, 1)])
                    p.op('act', lambda e: e.copy(wdnb[:, 0, :], df[:, 0, :]), reads=[(df.name, 0)], writes=[(wdnb.name, 0)])
                    p.op('dve', lambda e: e.tensor_copy(wdnb[:, 1, :], df[:, 1, :]), reads=[(df.name, 1)], writes=[(wdnb.name, 1)])
                    for s in range(2):
                        def tr(e):
                            for kc in range(8):
                                r = e.transpose(pst[:, kc, :], xb[:, s, kc * 128:(kc + 1) * 128], ident_b[:])
                            return r
                        p.op('pe', tr, reads=[xb, ident_b], writes=[pst])
                        p.op('act', lambda e: e.copy(xsT[:, :, s * 128:(s + 1) * 128], pst[:]), reads=[pst], writes=[xsT])
                    for half in range(2):
                        def mmg(e):
                            for n in range(2):
                                for kc in range(8):
                                    c0 = (half * 2 + n) * 128
                                    r = e.matmul(pg[:, n, :], wgub[:, kc, c0:c0 + 128], xsT[:, kc, :], start=(kc == 0), stop=(kc == 7))
                            return r
                        p.op('pe', mmg, reads=[(wgub.name, 0), (wgub.name, 1), xsT], writes=[pg])
                        if half == 0:
                            p.op('act', lambda e: e.activation(sg[:], pg[:], AF.Silu), reads=[pg], writes=[sg])
                        else:
                            p.op('dve', lambda e: e.tensor_mul(hT[:], sg[:], pg[:]), reads=[sg, pg], writes=[hT])
                    for s in range(2):
                        yb_ = ysb[s]

                        def mmy(e):
                            for nb in range(2):
                                for fc in range(2):
                                    r = e.matmul(py[:, nb * 512:(nb + 1) * 512], hT[:, fc, s * 128:(s + 1) * 128], wdnb[:, fc, nb * 512:(nb + 1) * 512], start=(fc == 0), stop=(fc == 1))
                            return r
                        p.op('pe', mmy, reads=[hT, (wdnb.name, 0), (wdnb.name, 1)], writes=[py])
                        if s == 0:
                            p.op('act', lambda e: e.copy(yb_[:], py[:]), reads=[py], writes=[yb_])
                        else:
                            p.op('dve', lambda e: e.tensor_copy(yb_[:], py[:]), reads=[py], writes=[yb_])
                        p.dma('sp', YS[s0 + s * 128:s0 + (s + 1) * 128, :], yb_[:], reads=[yb_], writes=[p.uk()])

                def run_set(si):
                    for b in range(si, NBLK, 2):
                        blk(b, sets[si])
                p.interleave([lambda: run_set(0), lambda: run_set(1)])
                p.barrier()

            with ExitStack() as ph:
                M5 = [p.sb(ph, [128, D], F32) for _ in range(2)]
                for s in range(2):
                    p.dma('sp', M5[s][:], MOD[layer, s:s + 1, 5 * D:6 * D].to_broadcast([128, D]), writes=[M5[s]])
                fg = p.sb(ph, [128, D], F32)
                p.dma('sp', fg[:], final_g[0:1, :].to_broadcast([128, D]), writes=[fg])
                g0 = [p.sb(ph, [128, D], F32) for _ in range(2)]
                g1_ = [p.sb(ph, [128, D], F32) for _ in range(2)]
                xt = [p.sb(ph, [128, D], F32) for _ in range(2)]
                ys_ = [p.sb(ph, [128, D], F32) for _ in range(2)]
                sxs = [p.sb(ph, [128, 4], F32) for _ in range(2)]

                def cb_stream(si):
                    def run():
                        for tt in range(T0 + si, NT, 2):
                            s = 1 if tt < LT else 0
                            tok = slice(tt * 128, (tt + 1) * 128)
                            a = g0[si]; b2 = g1_[si]; xb = xt[si]; y = ys_[si]; sx = sxs[si]
                            p.idma(a[:], None, YS[:, :], bass.IndirectOffsetOnAxis(ap=DESTi[:, tt, 0:1], axis=0), reads=['YS', DESTi], writes=[a])
                            p.idma(b2[:], None, YS[:, :], bass.IndirectOffsetOnAxis(ap=DESTi[:, tt, 1:2], axis=0), reads=['YS', DESTi], writes=[b2])
                            p.dma('sp', xb[:], X[tok, :], reads=[('X', tt)], writes=[xb])
                            p.op('dve', lambda e: e.tensor_scalar(y[:], a[:], W01[:, tt, 0:1], None, ALU.mult), reads=[a, W01], writes=[y])
                            p.op('dve', lambda e: e.scalar_tensor_tensor(y[:], b2[:], W01[:, tt, 1:2], y[:], ALU.mult, ALU.add), reads=[b2, W01, y], writes=[y])
                            p.op('dve', lambda e: e.tensor_mul(y[:], y[:], M5[s][:]), reads=[y, M5[s]], writes=[y])
                            p.op('dve', lambda e: e.tensor_add(xb[:], xb[:], y[:]), reads=[xb, y], writes=[xb])
                            if layer == 0:
                                p.dma('sp', X[tok, :], xb[:], reads=[xb], writes=[('X', tt)])
                            else:
                                p.op('act', lambda e: e.activation(y[:], xb[:], AF.Square, accum_out=sx[:, 0:1]), reads=[xb], writes=[y, sx])
                                p.op('dve', lambda e: e.tensor_scalar(sx[:, 1:2], sx[:, 0:1], 1.0 / D, EPS, ALU.mult, ALU.add), reads=[sx], writes=[sx])
                                rsqrt(sx[:, 1:2], [sx])
                                p.op('dve', lambda e: e.scalar_tensor_tensor(y[:], xb[:], sx[:, 1:2], fg[:], ALU.mult, ALU.mult), reads=[xb, sx, fg], writes=[y])
                                p.dma('sp', out[(tt - LT) * 128:(tt - LT + 1) * 128, :], y[:], reads=[y], writes=[p.uk()])
                    return run
                p.interleave([cb_stream(0), cb_stream(1)])
                p.barrier()
    return nc


_L, _N = 256, 4096
_NC_CACHE = {}


def _core_inputs(inp, b):
    L, N = _L, _N
    T = L + N
    f = lambda a: np.ascontiguousarray(np.asarray(a, dtype=np.float32))
    m = {}
    m['xin'] = f(np.concatenate([inp['ctx'][b], inp['x'][b]], axis=0))
    c2 = np.stack([np.asarray(inp['c'][b]), np.asarray(inp['c_ctx'])], axis=0)
    m['c2T'] = f(c2.reshape(2, 8, 128).transpose(2, 1, 0))
    return m


def _shared_inputs(inp):
    L, N = _L, _N
    T = L + N
    f = lambda a: np.ascontiguousarray(np.asarray(a, dtype=np.float32))
    m = {}
    for k in ['w_mod', 'b_mod', 'norm1_g', 'norm2_g', 'w_in', 'w_out', 'hgrn_norm_g', 'gla_norm_g', 'mla_w_uq', 'mla_w_ukv']:
        m[k] = f(inp[k])
    m['final_norm_g'] = f(np.asarray(inp['final_norm_g']).reshape(1, -1))
    m['hgrn_lb_logits'] = f(np.asarray(inp['hgrn_lb_logits']).reshape(2, 512))
    m['na_tab'] = f(na_bias_table(np.asarray(inp['na_rpb'], dtype=np.float32)).reshape(2, 4, 8, 64, 512))
    wg = np.zeros((2, 32, 256), np.float32)
    wg[:, 0:16, 0:128] = np.asarray(inp['gla_wg_f'])
    wg[:, 16:32, 128:256] = np.asarray(inp['gla_wg_b'])
    m['gla_wg_bd'] = wg
    m['gla_bg_cat'] = f(np.concatenate([np.asarray(inp['gla_bg_f']), np.asarray(inp['gla_bg_b'])], axis=1))
    m['mla_q_norm_g'] = f(np.asarray(inp['mla_q_norm_g']).reshape(2, 192, 1))
    m['mla_kv_norm_g'] = f(np.asarray(inp['mla_kv_norm_g']).reshape(2, 128, 1))
    m['moe_w_r'] = f(np.concatenate([np.asarray(inp['moe_w_rg']), np.asarray(inp['moe_w_re'])], axis=2))
    m['moe_b_r'] = f(np.concatenate([np.asarray(inp['moe_b_rg']), np.asarray(inp['moe_b_re'])], axis=1))
    m['moe_w_gu'] = f(np.asarray(inp['moe_w_gu']).reshape(2 * 32 * 1024, 512))
    m['moe_w_dn'] = f(np.asarray(inp['moe_w_dn']).reshape(2 * 32 * 256, 1024))
    hc = host_consts(L, N)
    hc['rope'] = hc['rope'].reshape(T, 32)
    m.update(hc)
    return m


def kernel(**inputs):
    inp = {k: np.asarray(v) for k, v in inputs.items()}
    B = inp['x'].shape[0]
    if 'nc' not in _NC_CACHE:
        _NC_CACHE['nc'] = build(_L, _N)
    nc = _NC_CACHE['nc']
    shared = _shared_inputs(inp)
    in_maps = []
    for core in range(8):
        m = dict(shared)
        m.update(_core_inputs(inp, core % B))
        in_maps.append(m)
    res = run_bass_kernel_spmd(nc, in_maps, core_ids=list(range(8)))
    outs = [np.asarray(res.results[b]['out'], dtype=np.float32) for b in range(B)]
    return np.stack(outs, axis=0)
```

```python
import numpy as np
from contextlib import ExitStack
import ml_dtypes
import concourse.bass as bass
import concourse.mybir as mybir
from concourse.bass_utils import run_bass_kernel_spmd

F32 = mybir.dt.float32
BF16 = mybir.dt.bfloat16
I32 = mybir.dt.int32
AF = mybir.ActivationFunctionType
ALU = mybir.AluOpType
AX = mybir.AxisListType

D = 1024
DIN = 3200
EPS = 1e-6
NEG = -30000.0
MOEB = 256
NEXP = 32
RSTAGE = 99
SCL = float(np.exp(-30.0))


class Prog:
    def __init__(self, nc, es, n_dma_sems=14):
        self.nc = nc
        self.es = es
        self.engs = {'pe': nc.tensor, 'act': nc.scalar, 'dve': nc.vector, 'pool': nc.gpsimd, 'sp': nc.sync}
        self.sem = {}
        self.cnt = {}
        for e in ('pe', 'act', 'dve', 'pool'):
            self.sem[e] = es.enter_context(nc.semaphore('s_' + e))
            self.cnt[e] = 0
        self.dsems = {}
        self.dcnt = {}
        self.dnext = {}
        for q in ('sp', 'pool'):
            self.dsems[q] = [es.enter_context(nc.semaphore('d_%s%d' % (q, i))) for i in range(n_dma_sems)]
            self.dcnt[q] = [0] * n_dma_sems
            self.dnext[q] = 0
        self.waited = {}
        self.lastw = {}
        self.readers = {}
        self.nbuf = 0

    def sb(self, st, shape, dt, name=None):
        self.nbuf += 1
        return st.enter_context(self.nc.sbuf_tensor(name or ('sb%d' % self.nbuf), list(shape), dt))

    def ps(self, st, shape, dt=F32, name=None):
        self.nbuf += 1
        return st.enter_context(self.nc.psum_tensor(name or ('ps%d' % self.nbuf), list(shape), dt))

    def _key(self, k):
        if isinstance(k, (str, tuple)):
            return k
        t = getattr(k, 'tensor', k)
        return getattr(t, 'name', None) or id(t)

    def _wait(self, ename, ev):
        sem, val = ev
        k = (ename, sem.num)
        if self.waited.get(k, 0) >= val:
            return
        self.waited[k] = val
        self.engs[ename].wait_ge(sem, val)

    def _deps(self, ename, reads, writes):
        deps = []
        for k in reads:
            k = self._key(k)
            if k in self.lastw:
                deps.append(self.lastw[k])
        for k in writes:
            k = self._key(k)
            if k in self.lastw:
                deps.append(self.lastw[k])
            deps.extend(self.readers.get(k, []))
        for ev in deps:
            self._wait(ename, ev)

    def _record(self, ev, reads, writes):
        for k in reads:
            k = self._key(k)
            self.readers.setdefault(k, []).append(ev)
        for k in writes:
            k = self._key(k)
            self.lastw[k] = ev
            self.readers[k] = []

    def op(self, ename, fn, reads=(), writes=()):
        self._deps(ename, reads, writes)
        ins = fn(self.engs[ename])
        self.cnt[ename] += 1
        ins.then_inc(self.sem[ename], 1)
        ev = (self.sem[ename], self.cnt[ename])
        self._record(ev, reads, writes)
        if getattr(self, '_yield', None):
            self._yield()
        return ev

    def _dma_common(self, q, emit, reads, writes):
        i = self.dnext[q]
        self.dnext[q] = (i + 1) % len(self.dsems[q])
        sem = self.dsems[q][i]
        if self.dcnt[q][i] > 0:
            self._wait(q, (sem, self.dcnt[q][i]))
        self._deps(q, reads, writes)
        ins = emit(self.engs[q])
        self.dcnt[q][i] += 16
        ins.then_inc(sem, 16)
        ev = (sem, self.dcnt[q][i])
        self._record(ev, reads, writes)
        if getattr(self, '_yield', None):
            self._yield()
        return ev

    def dma(self, q, out, in_, reads=(), writes=(), **kw):
        return self._dma_common(q, lambda e: e.dma_start(out=out, in_=in_, **kw), reads, writes)

    def idma(self, out, out_off, in_, in_off, reads=(), writes=(), **kw):
        return self._dma_common('pool', lambda e: e.indirect_dma_start(out=out, out_offset=out_off, in_=in_,
                                                                      in_offset=in_off, **kw), reads, writes)

    def interleave(self, fns):
        import threading
        n = len(fns)
        il = {'turn': 0, 'alive': [True] * n, 'cond': threading.Condition(), 'exc': None}
        tl = threading.local()

        def advance(i):
            for k in range(1, n + 1):
                j = (i + k) % n
                if il['alive'][j]:
                    il['turn'] = j
                    break
            il['cond'].notify_all()

        def wait_turn(i):
            while il['turn'] != i and il['exc'] is None:
                il['cond'].wait()

        def yield_():
            i = getattr(tl, 'idx', None)
            if i is None:
                return
            advance(i)
            wait_turn(i)
            if il['exc'] is not None:
                raise RuntimeError('interleave peer failed')

        def runner(i):
            with il['cond']:
                wait_turn(i)
                tl.idx = i
                try:
                    if il['exc'] is None:
                        fns[i]()
                except BaseException as e:
                    if il['exc'] is None:
                        il['exc'] = e
                il['alive'][i] = False
                if any(il['alive']):
                    advance(i)
                il['cond'].notify_all()

        prev = getattr(self, '_yield', None)
        self._yield = yield_
        ths = [threading.Thread(target=runner, args=(i,)) for i in range(n)]
        for t in ths:
            t.start()
        for t in ths:
            t.join()
        self._yield = prev
        if il['exc'] is not None:
            raise il['exc']

    def uk(self):
        self._ukn = getattr(self, '_ukn', 0) + 1
        return ('uk', self._ukn)

    def barrier(self):
        evs = [(self.sem[e], self.cnt[e]) for e in self.sem if self.cnt[e] > 0]
        for q in self.dsems:
            for i, s in enumerate(self.dsems[q]):
                if self.dcnt[q][i] > 0:
                    evs.append((s, self.dcnt[q][i]))
        for e in self.engs:
            for ev in evs:
                self._wait(e, ev)
        self.lastw = {}
        self.readers = {}


def host_consts(L, N):
    T = L + N
    c = {}
    c['ident_f'] = np.eye(128, dtype=np.float32)
    j = np.arange(128)[:, None]
    i = np.arange(128)[None, :]
    same = (j // 64) == (i // 64)
    U2f = (same & (j <= i)).astype(np.float32)
    Umf = (same & (j % 64 <= 31)).astype(np.float32)
    U2b = (same & (j >= i)).astype(np.float32)
    Umb = (same & (j % 64 >= 32)).astype(np.float32)
    c['uw_f'] = np.concatenate([U2f, U2f - Umf], axis=1)
    c['uw_b'] = np.concatenate([U2b, U2b - Umb], axis=1)
    c['v2_f'] = (same & (j > i)).astype(np.float32)
    c['v2_b'] = (same & (j < i)).astype(np.float32)
    jj = np.arange(64)[:, None]
    ii = np.arange(64)[None, :]
    c['mask_f'] = ((jj <= ii).astype(np.float64) * np.exp(60.0)).astype(np.float32)
    c['mask_b'] = ((jj >= ii).astype(np.float64) * np.exp(60.0)).astype(np.float32)
    c['lstrict'] = (j < i).astype(np.float32)
    t = np.arange(N)
    inv = (10000.0 ** (-np.arange(8, dtype=np.float32) / 8)).astype(np.float32)
    ang = np.stack([(t // 64).astype(np.float32)[:, None] * inv, (t % 64).astype(np.float32)[:, None] * inv], axis=1)
    rope = np.zeros((T, 2, 2, 8), np.float32)
    rope[:L, 0] = 1.0
    rope[L:, 0] = np.cos(ang)
    rope[L:, 1] = np.sin(ang)
    c['rope'] = rope
    c['pcol'] = np.arange(128, dtype=np.float32)[:, None].copy()
    return c


def na_bias_table(rpb):
    cidx = np.arange(64)
    c_start = np.clip(cidx - 8, 0, 48)
    col_in = (cidx[None] >= c_start[:, None]) & (cidx[None] < c_start[:, None] + 16)
    dc = np.clip(cidx[None] - cidx[:, None], -15, 15) + 15
    out = np.full((2, 4, 8, 64, 8, 64), NEG, np.float32)
    for cls in range(8):
        for jx in range(8):
            dr = jx + 7 - cls
            g = rpb[:, :, dr, :][:, :, dc]
            g = np.where(col_in[None, None], g, NEG)
            out[:, :, cls, :, jx, :] = np.transpose(g, (0, 1, 3, 2))
    return out


def build(L, N, dbg=False):
    T = L + N
    NT = T // 128
    NCH = T // 64
    LT = L // 128
    ROWS = N // 64
    NBLK = (2 * T + MOEB - 1) // MOEB + NEXP
    NSLOT = NBLK * MOEB
    nc = bass.Bass("TRN2", target_bir_lowering=False)

    def din(name, shape, dt=F32):
        return nc.dram_tensor(name, list(shape), dt, kind="ExternalInput").ap()

    def dscr(name, shape, dt=F32):
        kind = "ExternalOutput" if dbg else "Internal"
        return nc.dram_tensor(name, list(shape), dt, kind=kind).ap()

    xin = din('xin', [T, D])
    c2T = din('c2T', [128, 8, 2])
    w_mod = din('w_mod', [2, D, 6 * D])
    b_mod = din('b_mod', [2, 6 * D])
    norm1_g = din('norm1_g', [2, D])
    norm2_g = din('norm2_g', [2, D])
    final_g = din('final_norm_g', [1, D])
    w_in = din('w_in', [2, D, DIN])
    w_out = din('w_out', [2, D, D])
    lb_logits = din('hgrn_lb_logits', [2, 512])
    hgrn_ng = din('hgrn_norm_g', [2, 256])
    na_tab = din('na_tab', [2, 4, 8, 64, 512])
    wg_bd = din('gla_wg_bd', [2, 32, 256])
    bg_cat = din('gla_bg_cat', [2, 256])
    gla_ng = din('gla_norm_g', [2, 256])
    mla_qg = din('mla_q_norm_g', [2, 192, 1])
    mla_wuq = din('mla_w_uq', [2, 192, 384])
    mla_kvg = din('mla_kv_norm_g', [2, 128, 1])
    mla_wukv = din('mla_w_ukv', [2, 128, 512])
    w_r = din('moe_w_r', [2, D, 36])
    b_r = din('moe_b_r', [2, 36])
    w_gu = din('moe_w_gu', [2 * NEXP * D, 512])
    w_dn = din('moe_w_dn', [2 * NEXP * 256, D])
    ident_f_d = din('ident_f', [128, 128])
    uw_d = {'f': din('uw_f', [128, 256]), 'b': din('uw_b', [128, 256])}
    v2_d = {'f': din('v2_f', [128, 128]), 'b': din('v2_b', [128, 128])}
    mask_d = {'f': din('mask_f', [64, 64]), 'b': din('mask_b', [64, 64])}
    lstrict_d = din('lstrict', [128, 128])
    rope_d = din('rope', [T, 32])
    pcol_d = din('pcol', [128, 1])
    out = nc.dram_tensor('out', [N, D], F32, kind="ExternalOutput").ap()

    X = dscr('X', [T, D])
    MOD = dscr('MOD', [2, 2, 6 * D])
    mix = {}
    for m in 'AC':
        for d in 'fb':
            mix[m + d + 'QT'] = dscr('s_%s%s_QT' % (m, d), [256, T], BF16)
            mix[m + d + 'KT'] = dscr('s_%s%s_KT' % (m, d), [256, T], BF16)
            mix[m + d + 'QH'] = dscr('s_%s%s_QH' % (m, d), [256, T], BF16)
            mix[m + d + 'KH'] = dscr('s_%s%s_KH' % (m, d), [T, 256], BF16)
            mix[m + d + 'DEC'] = dscr('s_%s%s_DEC' % (m, d), [256, NCH])
            mix[m + d + 'O'] = dscr('s_%s%s_O' % (m, d), [T, 256])
        mix[m + 'V'] = dscr('s_%s_V' % m, [T, 256], BF16)
        mix[m + 'G'] = dscr('s_%s_G' % m, [T, 256])
    BQT = dscr('s_B_QT', [256, T], BF16)
    BKT = dscr('s_B_KT', [256, T], BF16)
    BV = dscr('s_B_V', [T, 256], BF16)
    DQT = dscr('s_D_QT', [4, 96, T], BF16)
    DKT = dscr('s_D_KT', [4, 96, T], BF16)
    DV = dscr('s_D_V', [T, 256], BF16)
    CATT = dscr('s_CATT', [D, T], BF16)
    H2 = dscr('s_H2', [T, D], BF16)
    XS = dscr('s_XS', [NSLOT, D], BF16)
    YS = dscr('s_YS', [NSLOT, D])

    with ExitStack() as es:
        p = Prog(nc, es)
        ident_f = p.sb(es, [128, 128], F32, 'ident_f_sb')
        ident_b = p.sb(es, [128, 128], BF16, 'ident_b_sb')
        ones_f = p.sb(es, [128, 128], F32, 'ones_f')
        ones_b = p.sb(es, [128, 128], BF16, 'ones_b')
        p.dma('sp', ident_f[:], ident_f_d, writes=[ident_f])
        p.op('dve', lambda e: e.tensor_copy(ident_b[:], ident_f[:]), reads=[ident_f], writes=[ident_b])
        p.op('dve', lambda e: e.memset(ones_f[:], 1.0), writes=[ones_f])
        p.op('dve', lambda e: e.memset(ones_b[:], 1.0), writes=[ones_b])

        def transpose_f(ps_ap, in_ap, reads, writes):
            n = in_ap.shape[0]
            p.op('pe', lambda e: e.transpose(ps_ap, in_ap, ident_f[0:n, 0:n]), reads=list(reads) + [ident_f], writes=writes)

        def transpose_b(ps_ap, in_ap, reads, writes):
            n = in_ap.shape[0]
            p.op('pe', lambda e: e.transpose(ps_ap, in_ap, ident_b[0:n, 0:n]), reads=list(reads) + [ident_b], writes=writes)

        def rsqrt(ap, keys):
            p.op('act', lambda e: e.activation(ap, ap, AF.Ln), reads=keys, writes=keys)
            p.op('act', lambda e: e.activation(ap, ap, AF.Exp, scale=-0.5), reads=keys, writes=keys)

        with ExitStack() as ph:
            xt = [p.sb(ph, [128, D], F32) for _ in range(2)]
            for tt in range(NT):
                b = xt[tt % 2]
                p.dma('sp', b[:], xin[tt * 128:(tt + 1) * 128, :], writes=[b])
                p.dma('sp', X[tt * 128:(tt + 1) * 128, :], b[:], reads=[b], writes=[('X', tt)])
            cT = p.sb(ph, [128, 8, 2], F32)
            sg = p.sb(ph, [128, 8, 2], F32)
            p.dma('sp', cT[:], c2T, writes=[cT])
            p.op('act', lambda e: e.activation(sg[:], cT[:], AF.Sigmoid), reads=[cT], writes=[sg])
            p.op('dve', lambda e: e.tensor_mul(cT[:], cT[:], sg[:]), reads=[cT, sg], writes=[cT])
            wm = [p.sb(ph, [128, 8, 512], F32) for _ in range(2)]
            bm = [p.sb(ph, [2, 512], F32) for _ in range(2)]
            mo = [p.sb(ph, [2, 512], F32) for _ in range(2)]
            pm = [p.ps(ph, [2, 512]) for _ in range(2)]
            it = 0
            for l in range(2):
                for nb in range(12):
                    w = wm[it % 2]; bb = bm[it % 2]; o = mo[it % 2]; ps = pm[it % 2]
                    p.dma('sp', w[:], w_mod[l, :, nb * 512:(nb + 1) * 512].rearrange("(k p) n -> p k n", p=128), writes=[w])
                    p.dma('sp', bb[:], b_mod[l:l + 1, nb * 512:(nb + 1) * 512].to_broadcast([2, 512]), writes=[bb])

                    def mm(e, w=w, ps=ps):
                        for kc in range(8):
                            r = e.matmul(ps[:], cT[:, kc, :], w[:, kc, :], start=(kc == 0), stop=(kc == 7))
                        return r
                    p.op('pe', mm, reads=[cT, w], writes=[ps])
                    p.op('dve', lambda e, o=o, ps=ps, bb=bb: e.tensor_add(o[:], ps[:], bb[:]), reads=[ps, bb], writes=[o])
                    p.dma('sp', MOD[l, :, nb * 512:(nb + 1) * 512], o[:], reads=[o], writes=[p.uk()])
                    it += 1
            p.barrier()

        for layer in range(2):
          with ExitStack() as lay:
            keep_ctx = layer == 0
            T0 = 0 if keep_ctx else LT
            with ExitStack() as ph:
                winb = p.sb(ph, [128, 8, DIN], BF16)
                for kc in range(8):
                    p.dma('pool', winb[:, kc, :], w_in[layer, kc * 128:(kc + 1) * 128, :], writes=[(winb.name, kc)])
                G1 = [p.sb(ph, [128, D], F32) for _ in range(2)]
                B1 = [p.sb(ph, [128, D], F32) for _ in range(2)]
                g1 = p.sb(ph, [128, D], F32)
                p.dma('sp', g1[:], norm1_g[layer:layer + 1, :].to_broadcast([128, D]), writes=[g1])
                for s in range(2):
                    p.dma('sp', B1[s][:], MOD[layer, s:s + 1, 0:D].to_broadcast([128, D]), reads=['MOD'], writes=[B1[s]])
                    p.dma('sp', G1[s][:], MOD[layer, s:s + 1, D:2 * D].to_broadcast([128, D]), reads=['MOD'], writes=[G1[s]])
                    p.op('dve', lambda e, s=s: e.scalar_tensor_tensor(G1[s][:], G1[s][:], 1.0, g1[:], ALU.add, ALU.mult),
                         reads=[G1[s], g1], writes=[G1[s]])
                LB = p.sb(ph, [128, 512], F32)
                OMLB = p.sb(ph, [128, 512], F32)
                if layer == 0:
                    p.op('dve', lambda e: e.memset(LB[:], 0.0), writes=[LB])
                    p.op('dve', lambda e: e.memset(OMLB[:], 1.0), writes=[OMLB])
                else:
                    l0 = p.sb(ph, [128, 512], F32)
                    p.dma('sp', l0[:], lb_logits[0:1, :].to_broadcast([128, 512]), writes=[l0])
                    p.dma('sp', LB[:], lb_logits[1:2, :].to_broadcast([128, 512]), writes=[LB])
                    p.op('dve', lambda e: e.tensor_sub(LB[:], LB[:], l0[:]), reads=[LB, l0], writes=[LB])
                    p.op('act', lambda e: e.activation(LB[:], LB[:], AF.Sigmoid), reads=[LB], writes=[LB])
                    p.op('dve', lambda e: e.tensor_scalar(OMLB[:], LB[:], -1.0, 1.0, ALU.mult, ALU.add), reads=[LB], writes=[OMLB])
                wgbd = p.sb(ph, [32, 256], F32)
                bgc = p.sb(ph, [128, 256], F32)
                p.dma('sp', wgbd[:], wg_bd[layer], writes=[wgbd])
                p.dma('sp', bgc[:], bg_cat[layer:layer + 1, :].to_broadcast([128, 256]), writes=[bgc])
                wuq = p.sb(ph, [128, 2, 384], F32)
                wuqb = p.sb(ph, [128, 2, 384], BF16)
                qg = p.sb(ph, [128, 2], F32)
                wukv = p.sb(ph, [128, 512], F32)
                wukvb = p.sb(ph, [128, 512], BF16)
                kvg = p.sb(ph, [128, 1], F32)
                p.dma('sp', wuq[:, 0, :], mla_wuq[layer, 0:128, :], writes=[wuq])
                p.dma('sp', wuq[0:64, 1, :], mla_wuq[layer, 128:192, :], writes=[wuq])
                p.dma('sp', qg[:, 0:1], mla_qg[layer, 0:128, :], writes=[qg])
                p.dma('sp', qg[0:64, 1:2], mla_qg[layer, 128:192, :], writes=[qg])
                p.dma('sp', wukv[:], mla_wukv[layer], writes=[wukv])
                p.dma('sp', kvg[:], mla_kvg[layer], writes=[kvg])
                p.op('dve', lambda e: e.tensor_scalar(wuqb[:, 0, :], wuq[:, 0, :], qg[:, 0:1], None, ALU.mult), reads=[wuq, qg], writes=[wuqb])
                p.op('dve', lambda e: e.tensor_scalar(wuqb[0:64, 1, :], wuq[0:64, 1, :], qg[0:64, 1:2], None, ALU.mult), reads=[wuq, qg], writes=[wuqb])
                p.op('dve', lambda e: e.tensor_scalar(wukvb[:], wukv[:], kvg[:, 0:1], None, ALU.mult), reads=[wukv, kvg], writes=[wukvb])
                uw = {}; v2 = {}
                for d in 'fb':
                    uw[d] = p.sb(ph, [128, 256], F32)
                    v2[d] = p.sb(ph, [128, 128], F32)
                    p.dma('sp', uw[d][:], uw_d[d], writes=[uw[d]])
                    p.dma('sp', v2[d][:], v2_d[d], writes=[v2[d]])
                xt = [p.sb(ph, [128, D], F32) for _ in range(2)]
                junk = p.sb(ph, [128, D], F32)
                st = [p.sb(ph, [128, 8], F32) for _ in range(2)]
                hb = p.sb(ph, [128, D], BF16)
                hT = p.sb(ph, [128, 8, 128], BF16)
                pTs = [p.sb(ph, [128, DIN], F32) for _ in range(2)]
                junk2 = p.sb(ph, [128, 192], F32)
                ps_t = p.ps(ph, [128, 8, 128], BF16)
                ps_t2 = p.ps(ph, [128, 8, 128], BF16)
                ps_p = [p.ps(ph, [128, 512]) for _ in range(2)]
                ps_a = p.ps(ph, [128, 2, 256])
                ps_b = p.ps(ph, [128, 2, 256])
                ps_c = p.ps(ph, [128, 512])
                ps_d = p.ps(ph, [128, 512])
                qA = p.sb(ph, [128, 256], F32)
                kk = p.sb(ph, [128, 256], F32)
                la = p.sb(ph, [128, 256], F32)
                qTs = p.sb(ph, [128, 2, 128], F32)
                kTs = p.sb(ph, [128, 2, 128], F32)
                x2 = p.sb(ph, [128, 2, 128], F32)
                x1 = p.sb(ph, [128, 2, 128], F32)
                x1n = p.sb(ph, [128, 2, 128], F32)
                x3 = p.sb(ph, [128, 256], F32)
                o_qh = p.sb(ph, [128, 2, 128], BF16)
                o_qt = p.sb(ph, [128, 2, 128], BF16)
                o_kt = p.sb(ph, [128, 2, 128], BF16)
                o_kh = p.sb(ph, [128, 256], BF16)
                gs = p.sb(ph, [128, 256], F32)
                qpad = p.sb(ph, [128, 4, 64], F32)
                kpad = p.sb(ph, [128, 4, 64], F32)
                lapad = p.sb(ph, [128, 4, 64], F32)
                zT = p.sb(ph, [32, 128], F32)
                lac = p.sb(ph, [128, 256], F32)
                rin = p.sb(ph, [128, 5, 32], F32)
                rout = p.sb(ph, [128, 5, 32], F32)
                rtmp5 = p.sb(ph, [128, 5, 32], F32)
                rtmp = p.sb(ph, [128, 4, 32], F32)
                bq = p.sb(ph, [128, 256], BF16)
                bqT = p.sb(ph, [128, 2, 128], BF16)
                bkT = p.sb(ph, [128, 2, 128], BF16)
                bv = p.sb(ph, [128, 256], BF16)
                ropets = [p.sb(ph, [128, 32], F32) for _ in range(2)]
                cqn = p.sb(ph, [128, 192], BF16)
                ckvn = p.sb(ph, [128, 128], BF16)
                cqT = p.sb(ph, [128, 2, 128], BF16)
                ckvT = p.sb(ph, [128, 128], BF16)
                qd = p.sb(ph, [128, 4, 96], F32)
                qd2 = p.sb(ph, [128, 4, 96], F32)
                qdb = p.sb(ph, [128, 4, 96], BF16)
                kvd = p.sb(ph, [128, 4, 128], F32)
                kdb = p.sb(ph, [128, 4, 96], BF16)
                kr = p.sb(ph, [128, 32], F32)
                kr2 = p.sb(ph, [128, 32], F32)
                vdb = p.sb(ph, [128, 256], BF16)
                dT = p.sb(ph, [96, 4, 128], BF16)
                p.op('dve', lambda e: e.memset(qpad[:], 0.0), writes=[qpad])
                p.op('dve', lambda e: e.memset(kpad[:], 0.0), writes=[kpad])
                p.op('dve', lambda e: e.memset(lapad[:], 0.0), writes=[lapad])

                def gla_prep(m, tt, d, q_ap, k_ap, la_ap, rq, rk, rl, first):
                    tok = slice(tt * 128, (tt + 1) * 128)
                    if first:
                        for ct in range(2):
                            transpose_f(ps_a[:, ct, 0:128], q_ap[:, ct * 128:(ct + 1) * 128], rq, [ps_a])
                        p.op('act', lambda e: e.copy(qTs[:], ps_a[:, :, 0:128]), reads=[ps_a], writes=[qTs])
                    for ct in range(2):
                        transpose_f(ps_a[:, ct, 128:256], k_ap[:, ct * 128:(ct + 1) * 128], rk, [ps_a])
                    p.op('act', lambda e: e.copy(kTs[:], ps_a[:, :, 128:256]), reads=[ps_a], writes=[kTs])
                    for ct in range(2):
                        p.op('pe', lambda e, ct=ct: e.matmul(ps_b[:, ct, :], la_ap[:, ct * 128:(ct + 1) * 128], uw[d][:], start=True, stop=True),
                             reads=list(rl) + [uw[d]], writes=[ps_b])
                    p.op('pe', lambda e: e.matmul(ps_c[:, 0:256], v2[d][:], la_ap, start=True, stop=True), reads=list(rl) + [v2[d]], writes=[ps_c])
                    p.op('act', lambda e: e.activation(x2[:], ps_b[:, :, 0:128], AF.Exp), reads=[ps_b], writes=[x2])
                    p.op('act', lambda e: e.activation(x1[:], ps_b[:, :, 128:256], AF.Exp), reads=[ps_b], writes=[x1])
                    p.op('act', lambda e: e.activation(x1n[:], ps_b[:, :, 128:256], AF.Exp, scale=-1.0), reads=[ps_b], writes=[x1n])
                    p.op('act', lambda e: e.activation(x3[:], ps_c[:, 0:256], AF.Exp), reads=[ps_c], writes=[x3])
                    p.op('dve', lambda e: e.tensor_mul(o_qh[:], qTs[:], x2[:]), reads=[qTs, x2], writes=[o_qh])
                    p.op('dve', lambda e: e.scalar_tensor_tensor(o_qt[:], qTs[:], SCL, x1[:], ALU.mult, ALU.mult), reads=[qTs, x1], writes=[o_qt])
                    p.op('dve', lambda e: e.scalar_tensor_tensor(o_kt[:], kTs[:], SCL, x1n[:], ALU.mult, ALU.mult), reads=[kTs, x1n], writes=[o_kt])
                    p.op('dve', lambda e: e.tensor_mul(o_kh[:], k_ap, x3[:]), reads=list(rk) + [x3], writes=[o_kh])
                    md = m + d
                    p.dma('pool', mix[md + 'QH'][:, tok].rearrange("(c p) t -> p c t", p=128), o_qh[:], reads=[o_qh], writes=[p.uk()])
                    p.dma('pool', mix[md + 'QT'][:, tok].rearrange("(c p) t -> p c t", p=128), o_qt[:], reads=[o_qt], writes=[p.uk()])
                    p.dma('pool', mix[md + 'KT'][:, tok].rearrange("(c p) t -> p c t", p=128), o_kt[:], reads=[o_kt], writes=[p.uk()])
                    p.dma('pool', mix[md + 'KH'][tok, :], o_kh[:], reads=[o_kh], writes=[p.uk()])
                    cols = (63, 127) if d == 'f' else (0, 64)
                    for cc in range(2):
                        p.dma('pool', mix[md + 'DEC'][:, 2 * tt + cc:2 * tt + cc + 1].rearrange("(c p) t -> p c t", p=128),
                              x2[:, :, cols[cc]:cols[cc] + 1], reads=[x2], writes=[p.uk()], allow_slow_non_contiguous=True)

                ncb = (DIN + 511) // 512

                def front(tt):
                    s = 1 if tt < LT else 0
                    tok = slice(tt * 128, (tt + 1) * 128)
                    xb = xt[tt % 2]; sx = st[tt % 2]; pT = pTs[tt % 2]; ropet = ropets[tt % 2]
                    p.dma('sp', xb[:], X[tok, :], reads=[('X', tt)], writes=[xb])
                    p.dma('sp', ropet[:], rope_d[tok, :], writes=[ropet])
                    p.op('act', lambda e: e.activation(junk[:], xb[:], AF.Square, accum_out=sx[:, 0:1]), reads=[xb], writes=[junk, sx])
                    p.op('dve', lambda e: e.tensor_scalar(sx[:, 1:2], sx[:, 0:1], 1.0 / D, EPS, ALU.mult, ALU.add), reads=[sx], writes=[sx])
                    rsqrt(sx[:, 1:2], [sx])
                    p.op('dve', lambda e: e.scalar_tensor_tensor(junk[:], xb[:], sx[:, 1:2], G1[s][:], ALU.mult, ALU.mult), reads=[xb, sx, G1[s]], writes=[junk])
                    p.op('dve', lambda e: e.tensor_add(hb[:], junk[:], B1[s][:]), reads=[junk, B1[s]], writes=[hb])
                    for kc in range(8):
                        transpose_b(ps_t2[:, kc, :], hb[:, kc * 128:(kc + 1) * 128], [hb], [ps_t2])
                    p.op('act', lambda e: e.copy(hT[:], ps_t2[:]), reads=[ps_t2], writes=[hT])
                    for cb in range(ncb):
                        c0 = cb * 512; c1 = min(DIN, c0 + 512)
                        pp = ps_p[cb % 2]

                        def mm(e, pp=pp, c0=c0, c1=c1):
                            for kc in range(8):
                                r = e.matmul(pp[:, 0:c1 - c0], hT[:, kc, :], winb[:, kc, c0:c1], start=(kc == 0), stop=(kc == 7))
                            return r
                        p.op('pe', mm, reads=[hT] + [(winb.name, kc) for kc in range(8)], writes=[pp])
                        eng = 'act' if cb % 2 == 0 else 'dve'
                        if eng == 'act':
                            p.op('act', lambda e, pp=pp, c0=c0, c1=c1: e.copy(pT[:, c0:c1], pp[:, 0:c1 - c0]), reads=[pp], writes=[(pT.name, cb)])
                        else:
                            p.op('dve', lambda e, pp=pp, c0=c0, c1=c1: e.tensor_copy(pT[:, c0:c1], pp[:, 0:c1 - c0]), reads=[pp], writes=[(pT.name, cb)])

                def back(tt):
                    s = 1 if tt < LT else 0
                    tok = slice(tt * 128, (tt + 1) * 128)
                    sx = st[tt % 2]; pT = pTs[tt % 2]; ropet = ropets[tt % 2]
                    RP = [(pT.name, cb) for cb in range(ncb)]
                    def chain_x():
                        p.op('act', lambda e: e.activation(qA[:], pT[:, 0:256], AF.Silu), reads=RP, writes=[qA])
                        p.op('dve', lambda e: e.tensor_scalar(qA[:], qA[:], 0.125, None, ALU.mult), reads=[qA], writes=[qA])
                        p.dma('pool', mix['AV'][tok, :], pT[:, 256:512], reads=RP, writes=[p.uk()])
                        p.op('act', lambda e: e.activation(gs[:], pT[:, 1024:1280], AF.Silu), reads=RP, writes=[gs])
                        p.dma('pool', mix['AG'][tok, :], gs[:], reads=[gs], writes=[p.uk()])
                        for di, d in enumerate('fb'):
                            zc = slice(512 + 256 * di, 768 + 256 * di)
                            lc = slice(256 * di, 256 * di + 256)
                            p.op('act', lambda e: e.activation(la[:], pT[:, zc], AF.Sigmoid), reads=RP, writes=[la])
                            p.op('dve', lambda e: e.tensor_mul(la[:], la[:], OMLB[:, lc]), reads=[la, OMLB], writes=[la])
                            p.op('dve', lambda e: e.tensor_add(la[:], la[:], LB[:, lc]), reads=[la, LB], writes=[la])
                            p.op('dve', lambda e: e.tensor_scalar(kk[:], la[:], -1.0, 1.0, ALU.mult, ALU.add), reads=[la], writes=[kk])
                            p.op('act', lambda e: e.activation(la[:], la[:], AF.Ln), reads=[la], writes=[la])
                            gla_prep('A', tt, d, qA[:], kk[:], la[:], [qA], [kk], [la], di == 0)
                        p.op('dve', lambda e: e.tensor_scalar(qpad[:, :, 0:32], pT[:, 2048:2176].rearrange("p (h k) -> p h k", h=4), 32.0 ** -0.5, None, ALU.mult),
                             reads=RP, writes=[qpad])
                        p.op('dve', lambda e: e.tensor_copy(kpad[:, :, 0:32], pT[:, 2176:2304].rearrange("p (h k) -> p h k", h=4)), reads=RP, writes=[kpad])
                        p.dma('pool', mix['CV'][tok, :], pT[:, 2304:2560], reads=RP, writes=[p.uk()])
                        p.op('act', lambda e: e.activation(gs[:], pT[:, 2560:2816], AF.Silu), reads=RP, writes=[gs])
                        p.dma('pool', mix['CG'][tok, :], gs[:], reads=[gs], writes=[p.uk()])
                        transpose_f(ps_c[0:32, 256:384], pT[:, 2816:2848], RP, [ps_c])
                        p.op('act', lambda e: e.copy(zT[:], ps_c[0:32, 256:384]), reads=[ps_c], writes=[zT])
                        p.op('pe', lambda e: e.matmul(ps_c[:, 0:256], zT[:], wgbd[:], start=True, stop=True), reads=[zT, wgbd], writes=[ps_c])
                        p.op('dve', lambda e: e.tensor_add(lac[:], ps_c[:, 0:256], bgc[:]), reads=[ps_c, bgc], writes=[lac])
                        p.op('act', lambda e: e.activation(lac[:], lac[:], AF.Sigmoid), reads=[lac], writes=[lac])
                        p.op('act', lambda e: e.activation(lac[:], lac[:], AF.Ln), reads=[lac], writes=[lac])
                        for di, d in enumerate('fb'):
                            p.op('dve', lambda e: e.tensor_scalar(lapad[:, :, 0:32], lac[:, 128 * di:128 * di + 128].rearrange("p (h k) -> p h k", h=4), 1.0 / 16.0, None, ALU.mult),
                                 reads=[lac], writes=[lapad])
                            gla_prep('C', tt, d, qpad[:].rearrange("p h k -> p (h k)"), kpad[:].rearrange("p h k -> p (h k)"),
                                     lapad[:].rearrange("p h k -> p (h k)"), [qpad], [kpad], [lapad], di == 0)

                    def chain_y():
                        p.op('dve', lambda e: e.tensor_scalar(bq[:], pT[:, 1280:1536], 0.125, None, ALU.mult), reads=RP, writes=[bq])
                        for ct in range(2):
                            transpose_b(ps_t[:, ct, :], bq[:, ct * 128:(ct + 1) * 128], [bq], [ps_t])
                        p.op('act', lambda e: e.copy(bqT[:], ps_t[:, 0:2, :]), reads=[ps_t], writes=[bqT])
                        p.dma('pool', BQT[:, tok].rearrange("(c p) t -> p c t", p=128), bqT[:], reads=[bqT], writes=[p.uk()])
                        p.op('dve', lambda e: e.tensor_copy(bq[:], pT[:, 1536:1792]), reads=RP + [bqT], writes=[bq])
                        for ct in range(2):
                            transpose_b(ps_t[:, 2 + ct, :], bq[:, ct * 128:(ct + 1) * 128], [bq], [ps_t])
                        p.op('act', lambda e: e.copy(bkT[:], ps_t[:, 2:4, :]), reads=[ps_t], writes=[bkT])
                        p.dma('pool', BKT[:, tok].rearrange("(c p) t -> p c t", p=128), bkT[:], reads=[bkT], writes=[p.uk()])
                        p.op('dve', lambda e: e.tensor_copy(bv[:], pT[:, 1792:2048]), reads=RP, writes=[bv])
                        p.dma('pool', BV[tok, :], bv[:], reads=[bv], writes=[p.uk()])
                        p.op('act', lambda e: e.activation(junk2[:, 0:192], pT[:, 2848:3040], AF.Square, accum_out=sx[:, 2:3]), reads=RP, writes=[junk2, sx])
                        p.op('act', lambda e: e.activation(junk2[:, 0:128], pT[:, 3040:3168], AF.Square, accum_out=sx[:, 3:4]), reads=RP, writes=[junk2, sx])
                        p.op('dve', lambda e: e.tensor_scalar(sx[:, 4:5], sx[:, 2:3], 1.0 / 192, EPS, ALU.mult, ALU.add), reads=[sx], writes=[sx])
                        rsqrt(sx[:, 4:5], [sx])
                        p.op('dve', lambda e: e.tensor_scalar(sx[:, 5:6], sx[:, 3:4], 1.0 / 128, EPS, ALU.mult, ALU.add), reads=[sx], writes=[sx])
                        rsqrt(sx[:, 5:6], [sx])
                        p.op('dve', lambda e: e.tensor_scalar(cqn[:], pT[:, 2848:3040], sx[:, 4:5], None, ALU.mult), reads=RP + [sx], writes=[cqn])
                        p.op('dve', lambda e: e.tensor_scalar(ckvn[:], pT[:, 3040:3168], sx[:, 5:6], None, ALU.mult), reads=RP + [sx], writes=[ckvn])
                        transpose_b(ps_t[:, 4, :], cqn[:, 0:128], [cqn], [ps_t])
                        transpose_b(ps_t[0:64, 5, :], cqn[:, 128:192], [cqn], [ps_t])
                        transpose_b(ps_t[:, 6, :], ckvn[:], [ckvn], [ps_t])
                        p.op('act', lambda e: e.copy(cqT[:, 0, :], ps_t[:, 4, :]), reads=[ps_t], writes=[cqT])
                        p.op('act', lambda e: e.copy(cqT[0:64, 1, :], ps_t[0:64, 5, :]), reads=[ps_t], writes=[cqT])
                        p.op('act', lambda e: e.copy(ckvT[:], ps_t[:, 6, :]), reads=[ps_t], writes=[ckvT])

                        def mmq(e):
                            e.matmul(ps_d[:, 0:384], cqT[:, 0, :], wuqb[:, 0, :], start=True, stop=False)
                            return e.matmul(ps_d[:, 0:384], cqT[0:64, 1, :], wuqb[0:64, 1, :], start=False, stop=True)
                        p.op('pe', mmq, reads=[cqT, wuqb], writes=[ps_d])
                        p.op('act', lambda e: e.activation(qd[:].rearrange("p h k -> p (h k)"), ps_d[:, 0:384], AF.Copy, scale=96.0 ** -0.5), reads=[ps_d], writes=[qd])
                        p.op('pe', lambda e: e.matmul(ps_d[:, 0:512], ckvT[:], wukvb[:], start=True, stop=True), reads=[ckvT, wukvb], writes=[ps_d])
                        p.op('act', lambda e: e.copy(kvd[:].rearrange("p h k -> p (h k)"), ps_d[:, 0:512]), reads=[ps_d], writes=[kvd])
                        cosb = ropet[:, 0:16].rearrange("p (a f) -> p a f", a=2)
                        sinb = ropet[:, 16:32].rearrange("p (a f) -> p a f", a=2)

                        p.op('dve', lambda e: e.tensor_copy(rin[:, 0:4, :], qd[:, :, 64:96]), reads=[qd], writes=[rin])
                        p.op('dve', lambda e: e.tensor_copy(rin[:, 4, :], pT[:, 3168:3200]), reads=RP + [rin], writes=[rin])
                        s5 = rin[:].rearrange("p h (a x f) -> p h a x f", a=2, x=2)
                        d5 = rout[:].rearrange("p h (a x f) -> p h a x f", a=2, x=2)
                        t5 = rtmp5[:].rearrange("p h (a x f) -> p h a x f", a=2, x=2)
                        u1 = s5[:, :, :, 0, :]; u2 = s5[:, :, :, 1, :]
                        cos5 = cosb.unsqueeze(1).to_broadcast([128, 5, 2, 8])
                        sin5 = sinb.unsqueeze(1).to_broadcast([128, 5, 2, 8])
                        p.op('dve', lambda e: e.tensor_mul(d5[:, :, :, 0, :], u1, cos5), reads=[rin, ropet], writes=[rout])
                        p.op('dve', lambda e: e.tensor_mul(t5[:, :, :, 0, :], u2, sin5), reads=[rin, ropet], writes=[rtmp5])
                        p.op('dve', lambda e: e.tensor_mul(d5[:, :, :, 1, :], u1, sin5), reads=[rin, ropet, rout], writes=[rout])
                        p.op('dve', lambda e: e.tensor_mul(t5[:, :, :, 1, :], u2, cos5), reads=[rin, ropet, rtmp5], writes=[rtmp5])
                        p.op('dve', lambda e: e.tensor_sub(d5[:, :, :, 0, :], d5[:, :, :, 0, :], t5[:, :, :, 0, :]), reads=[rout, rtmp5], writes=[rout])
                        p.op('dve', lambda e: e.tensor_add(d5[:, :, :, 1, :], d5[:, :, :, 1, :], t5[:, :, :, 1, :]), reads=[rout, rtmp5], writes=[rout])
                        p.op('dve', lambda e: e.tensor_copy(qdb[:, :, 0:64], qd[:, :, 0:64]), reads=[qd], writes=[qdb])
                        p.op('dve', lambda e: e.tensor_copy(qdb[:, :, 64:96], rout[:, 0:4, :]), reads=[rout, qdb], writes=[qdb])
                        p.op('dve', lambda e: e.tensor_copy(kdb[:, :, 0:64], kvd[:, :, 0:64]), reads=[kvd], writes=[kdb])
                        p.op('dve', lambda e: e.tensor_copy(kdb[:, :, 64:96], rout[:, 4:5, :].to_broadcast([128, 4, 32])), reads=[rout, kdb], writes=[kdb])
                        p.op('dve', lambda e: e.tensor_copy(vdb[:].rearrange("p (h k) -> p h k", h=4), kvd[:, :, 64:128]), reads=[kvd], writes=[vdb])
                        p.dma('pool', DV[tok, :], vdb[:], reads=[vdb], writes=[p.uk()])
                        for h in range(4):
                            transpose_b(ps_t[0:96, h, :], qdb[:, h, :], [qdb], [ps_t])
                        p.op('act', lambda e: e.copy(dT[:], ps_t[0:96, 0:4, :]), reads=[ps_t], writes=[dT])
                        p.dma('pool', DQT[:, :, tok].rearrange("h k t -> k h t"), dT[:], reads=[dT], writes=[p.uk()])
                        for h in range(4):
                            transpose_b(ps_t[0:96, 4 + h, :], kdb[:, h, :], [kdb], [ps_t])
                        p.op('act', lambda e: e.copy(dT[:], ps_t[0:96, 4:8, :]), reads=[ps_t], writes=[dT])
                        p.dma('pool', DKT[:, :, tok].rearrange("h k t -> k h t"), dT[:], reads=[dT], writes=[p.uk()])

                    if tt + 1 < NT:
                        p.interleave([lambda: front(tt + 1), chain_x, chain_y])
                    else:
                        p.interleave([chain_x, chain_y])

                front(0)
                for tt in range(NT):
                    back(tt)
                p.barrier()
            if dbg == 'P':
                break

            def normalize(ph_bufs, OT, LTp, n, dst_ap, dst_key):
                rl, on = ph_bufs
                p.op('dve', lambda e: e.reciprocal(rl[:, 0:n], LTp[:, 0:n]), reads=[LTp], writes=[rl])
                p.op('dve', lambda e: e.tensor_mul(on[:, 0:n], OT[:, 0:n], rl[:, 0:n]), reads=[OT, rl], writes=[on])
                p.dma('pool', dst_ap, on[:, 0:n], reads=[on], writes=[p.uk()])

            with ExitStack() as ph:
                GC = 4
                NG = NCH // GC
                LG = (L // 64) // GC
                masks = {}
                for d in 'fb':
                    masks[d] = p.sb(ph, [64, 64], F32)
                    p.dma('sp', masks[d][:], mask_d[d], writes=[masks[d]])
                T2 = [p.ps(ph, [64, 512]) for _ in range(2)]
                streams = []
                for si, (m, d) in enumerate((('A', 'f'), ('A', 'b'), ('C', 'f'), ('C', 'b'))):
                    st_ = dict(m=m, d=d, md=m + d)
                    for nm in ('qt', 'kt', 'qh', 'kh', 'v'):
                        st_[nm] = [p.sb(ph, [64, 4, 256], BF16) for _ in range(2)]
                    st_['dec'] = [p.sb(ph, [64, 4, 4], F32) for _ in range(2)]
                    st_['ob'] = [p.sb(ph, [64, 4, 256], F32) for _ in range(2)]
                    st_['at'] = p.sb(ph, [64, 256], BF16)
                    st_['S'] = p.sb(ph, [64, 4, 64], F32)
                    st_['Sb'] = p.sb(ph, [64, 4, 64], BF16)
                    st_['T1'] = p.ps(ph, [64, 512])
                    st_['pS'] = T2[si // 2][:, (si % 2) * 256:(si % 2) * 256 + 256]
                    st_['pSk'] = T2[si // 2]
                    if d == 'f':
                        st_['gorder'] = list(range(NG))
                    else:
                        st_['gorder'] = list(range(LG - 1, -1, -1)) + list(range(NG - 1, LG - 1, -1))
                    p.op('dve', lambda e: e.memset(st_['S'][:], 0.0), writes=[st_['S']])
                    p.op('dve', lambda e: e.memset(st_['Sb'][:], 0.0), writes=[st_['Sb']])
                    streams.append(st_)
                for gi in range(NG):
                    b2 = gi % 2
                    for st_ in streams:
                        m, d, md = st_['m'], st_['d'], st_['md']
                        g = st_['gorder'][gi]
                        tk = slice(g * 256, (g + 1) * 256)
                        p.dma('sp', st_['qt'][b2][:], mix[md + 'QT'][:, tk].rearrange("(h k) t -> k h t", k=64), reads=[md + 'QT'], writes=[st_['qt'][b2]])
                        p.dma('sp', st_['kt'][b2][:], mix[md + 'KT'][:, tk].rearrange("(h k) t -> k h t", k=64), reads=[md + 'KT'], writes=[st_['kt'][b2]])
                        p.dma('sp', st_['qh'][b2][:], mix[md + 'QH'][:, tk].rearrange("(h k) t -> k h t", k=64), reads=[md + 'QH'], writes=[st_['qh'][b2]])
                        p.dma('sp', st_['kh'][b2][:], mix[md + 'KH'][tk, :].rearrange("(c p) n -> p c n", p=64), reads=[md + 'KH'], writes=[st_['kh'][b2]])
                        p.dma('sp', st_['v'][b2][:], mix[m + 'V'][tk, :].rearrange("(c p) n -> p c n", p=64), reads=[m + 'V'], writes=[st_['v'][b2]])
                        p.dma('sp', st_['dec'][b2][:], mix[md + 'DEC'][:, g * 4:(g + 1) * 4].rearrange("(h k) t -> k h t", k=64), reads=[md + 'DEC'], writes=[st_['dec'][b2]])
                    for ci in range(GC):
                        for st_ in streams:
                            d = st_['d']
                            c = ci if d == 'f' else GC - 1 - ci
                            cs = slice(c * 64, (c + 1) * 64)
                            qt, kt, qh, kh, v, dec, ob = (st_[n][b2] for n in ('qt', 'kt', 'qh', 'kh', 'v', 'dec', 'ob'))
                            at, S, Sb, T1, pS, pSk = st_['at'], st_['S'], st_['Sb'], st_['T1'], st_['pS'], st_['pSk']
                            pa = T1[:, 0:256]; po = T1[:, 256:512]
                            ka = T1; ko = T1

                            def mmA(e):
                                for h in range(4):
                                    r = e.matmul(pa[:, h * 64:(h + 1) * 64], kt[:, h, cs], qt[:, h, cs], start=True, stop=True)
                                return r
                            p.op('pe', mmA, reads=[kt, qt], writes=[ka])
                            p.op('dve', lambda e: e.tensor_scalar(at[:], pa, 1e30, -1e30, ALU.min, ALU.max), reads=[ka], writes=[at])
                            p.op('dve', lambda e: e.tensor_mul(at[:].rearrange("p (h i) -> p h i", h=4), at[:].rearrange("p (h i) -> p h i", h=4),
                                                              masks[d][:].unsqueeze(1).to_broadcast([64, 4, 64])), reads=[at, masks[d]], writes=[at])

                            def mmO(e):
                                for h in range(4):
                                    e.matmul(po[:, h * 64:(h + 1) * 64], at[:, h * 64:(h + 1) * 64], v[:, c, h * 64:(h + 1) * 64], start=True, stop=False)
                                    r = e.matmul(po[:, h * 64:(h + 1) * 64], qh[:, h, cs], Sb[:, h, :], start=False, stop=True)
                                return r
                            p.op('pe', mmO, reads=[at, v, qh, Sb], writes=[ko])
                            p.op('act', lambda e: e.copy(ob[:, c, :], po), reads=[ko], writes=[ob])

                            def mmS(e):
                                for h in range(4):
                                    r = e.matmul(pS[:, h * 64:(h + 1) * 64], kh[:, c, h * 64:(h + 1) * 64], v[:, c, h * 64:(h + 1) * 64], start=True, stop=True)
                                return r
                            p.op('pe', mmS, reads=[kh, v], writes=[pSk])
                            p.op('dve', lambda e: e.tensor_mul(S[:], S[:], dec[:, :, c:c + 1].to_broadcast([64, 4, 64])), reads=[S, dec], writes=[S])
                            p.op('dve', lambda e: e.tensor_add(S[:], S[:], pS.rearrange("p (h v) -> p h v", h=4)), reads=[S, pSk], writes=[S])
                            p.op('act', lambda e: e.copy(Sb[:], S[:]), reads=[S], writes=[Sb])
                    for st_ in streams:
                        g = st_['gorder'][gi]
                        tk = slice(g * 256, (g + 1) * 256)
                        p.dma('pool', mix[st_['md'] + 'O'][tk, :].rearrange("(c p) n -> p c n", p=64), st_['ob'][b2][:], reads=[st_['ob'][b2]], writes=[p.uk()])
                p.barrier()
            if dbg == 'R':
                break
            with ExitStack() as ph:
                ngb = {}
                for m, src in (('A', hgrn_ng), ('C', gla_ng)):
                    ngb[m] = p.sb(ph, [128, 256], F32)
                    p.dma('sp', ngb[m][:], src[layer:layer + 1, :].to_broadcast([128, 256]), writes=[ngb[m]])
                def ro_stream(m, roff):
                    of = [p.sb(ph, [128, 256], F32) for _ in range(2)]
                    ob_ = [p.sb(ph, [128, 256], F32) for _ in range(2)]
                    gg = [p.sb(ph, [128, 256], F32) for _ in range(2)]
                    sq = p.sb(ph, [128, 256], F32)
                    ss = p.sb(ph, [128, 4], F32)
                    yb = p.sb(ph, [128, 256], BF16)
                    yT = p.sb(ph, [128, 2, 128], BF16)
                    pst = p.ps(ph, [128, 2, 128], BF16)

                    def run():
                        it = 0
                        for tt in range(T0, NT):
                            tok = slice(tt * 128, (tt + 1) * 128)
                            a = of[it % 2]; b2 = ob_[it % 2]; g = gg[it % 2]
                            it += 1
                            p.dma('sp', a[:], mix[m + 'fO'][tok, :], reads=[m + 'fO'], writes=[a])
                            p.dma('sp', b2[:], mix[m + 'bO'][tok, :], reads=[m + 'bO'], writes=[b2])
                            p.dma('sp', g[:], mix[m + 'G'][tok, :], reads=[m + 'G'], writes=[g])
                            p.op('dve', lambda e: e.tensor_add(a[:], a[:], b2[:]), reads=[a, b2], writes=[a])
                            p.op('dve', lambda e: e.tensor_mul(sq[:], a[:], a[:]), reads=[a], writes=[sq])
                            p.op('dve', lambda e: e.tensor_reduce(ss[:], sq[:].rearrange("p (h v) -> p h v", h=4), AX.X, ALU.add), reads=[sq], writes=[ss])
                            p.op('dve', lambda e: e.tensor_scalar(ss[:], ss[:], 1.0 / 64, EPS, ALU.mult, ALU.add), reads=[ss], writes=[ss])
                            rsqrt(ss[:], [ss])
                            p.op('dve', lambda e: e.tensor_mul(a[:].rearrange("p (h v) -> p h v", h=4), a[:].rearrange("p (h v) -> p h v", h=4),
                                                              ss[:].unsqueeze(2).to_broadcast([128, 4, 64])), reads=[a, ss], writes=[a])
                            p.op('dve', lambda e: e.tensor_mul(a[:], a[:], ngb[m][:]), reads=[a, ngb[m]], writes=[a])
                            p.op('dve', lambda e: e.tensor_mul(yb[:], a[:], g[:]), reads=[a, g], writes=[yb])

                            def tr(e):
                                for ct in range(2):
                                    r = e.transpose(pst[:, ct, :], yb[:, ct * 128:(ct + 1) * 128], ident_b[:])
                                return r
                            p.op('pe', tr, reads=[yb, ident_b], writes=[pst])
                            p.op('act', lambda e: e.copy(yT[:], pst[:]), reads=[pst], writes=[yT])
                            p.dma('pool', CATT[roff:roff + 256, tok].rearrange("(c p) t -> p c t", p=128), yT[:], reads=[yT], writes=[p.uk()])
                    return run
                p.interleave([ro_stream('A', 0), ro_stream('C', 512)])
                p.barrier()

            with ExitStack() as ph:
                KTn = p.sb(ph, [64, 4, T], BF16)
                QTn = p.sb(ph, [64, 4, T], BF16)
                p.dma('sp', KTn[:], BKT.rearrange("(h d) t -> d h t", d=64), reads=['BKT'], writes=[KTn])
                p.dma('sp', QTn[:], BQT.rearrange("(h d) t -> d h t", d=64), reads=['BQT'], writes=[QTn])
                V1l = p.sb(ph, [64, ROWS, 256], BF16)
                V1c = p.sb(ph, [128, LT, 256], BF16)
                for r8 in range(0, ROWS, 8):
                    p.dma('sp', V1l[:, r8:r8 + 8, :], BV[L + r8 * 64:L + (r8 + 8) * 64, :].rearrange("(r w) c -> w r c", w=64), reads=['BV'], writes=[V1l])
                p.dma('sp', V1c[:], BV[0:L, :].rearrange("(n p) c -> p n c", p=128), reads=['BV'], writes=[V1c])
                EBs = [p.sb(ph, [64, 8, 512], F32) for _ in range(2)]
                psS = [p.ps(ph, [64, 512]) for _ in range(2)]
                psC = [p.ps(ph, [128, 512]) for _ in range(2)]
                OTp = p.ps(ph, [64, 512])
                LTp = p.ps(ph, [64, 512])
                nb_ = (p.sb(ph, [64, 512], F32), p.sb(ph, [64, 512], BF16))
                pe_ = [p.sb(ph, [64, 512], F32) for _ in range(2)]
                pb16 = [p.sb(ph, [64, 512], BF16) for _ in range(2)]
                pc16 = [p.sb(ph, [128, 512], BF16) for _ in range(2)]
                rows_ = [(h, r) for h in range(4) for r in range(ROWS)]

                def na_front(i):
                    h, r = rows_[i]
                    EB = EBs[h % 2]
                    if r == 0:
                        p.dma('sp', EB[:], na_tab[layer, h].rearrange("c w x -> w c x"), writes=[EB])
                        p.op('act', lambda e: e.activation(EB[:], EB[:], AF.Exp), reads=[EB], writes=[EB])
                    rs = min(max(r - 4, 0), ROWS - 8)
                    cls = r - rs
                    qc = slice(L + r * 64, L + (r + 1) * 64)
                    ps = psS[i % 2]; pc = psC[i % 2]; pe = pe_[i % 2]; pbb = pb16[i % 2]; pcs = pc16[i % 2]

                    def mmS(e):
                        for j in range(8):
                            kc = slice(L + (rs + j) * 64, L + (rs + j + 1) * 64)
                            r_ = e.matmul(ps[:, j * 64:(j + 1) * 64], KTn[:, h, kc], QTn[:, h, qc], start=True, stop=True)
                        for kt in range(LT):
                            r_ = e.matmul(pc[:, kt * 64:(kt + 1) * 64], KTn[:, h, kt * 128:(kt + 1) * 128], QTn[:, h, qc], start=True, stop=True)
                        return r_
                    p.op('pe', mmS, reads=[KTn, QTn], writes=[ps, pc])
                    p.op('act', lambda e: e.activation(pe[:], ps[:], AF.Exp), reads=[ps], writes=[pe])
                    p.op('act', lambda e: e.activation(pcs[:, 0:LT * 64], pc[:, 0:LT * 64], AF.Exp), reads=[pc], writes=[pcs])
                    p.op('dve', lambda e: e.tensor_mul(pbb[:], pe[:], EB[:, cls, :]), reads=[pe, EB], writes=[pbb])

                def na_back(i):
                    h, r = rows_[i]
                    hv = slice(h * 64, (h + 1) * 64)
                    rs = min(max(r - 4, 0), ROWS - 8)
                    ri = r % 8
                    r0 = r - ri
                    pbb = pb16[i % 2]; pcs = pc16[i % 2]

                    def mmV(e):
                        for j in range(8):
                            e.matmul(OTp[:, ri * 64:(ri + 1) * 64], V1l[:, rs + j, hv], pbb[:, j * 64:(j + 1) * 64], start=(j == 0), stop=False)
                        for kt in range(LT):
                            e.matmul(OTp[:, ri * 64:(ri + 1) * 64], V1c[:, kt, hv], pcs[:, kt * 64:(kt + 1) * 64], start=False, stop=(kt == LT - 1))
                        for j in range(8):
                            e.matmul(LTp[:, ri * 64:(ri + 1) * 64], ones_b[0:64, 0:64], pbb[:, j * 64:(j + 1) * 64], start=(j == 0), stop=False)
                        for kt in range(LT):
                            r_ = e.matmul(LTp[:, ri * 64:(ri + 1) * 64], ones_b[:, 0:64], pcs[:, kt * 64:(kt + 1) * 64], start=False, stop=(kt == LT - 1))
                        return r_
                    p.op('pe', mmV, reads=[V1l, V1c, pbb, pcs, ones_b], writes=[OTp, LTp])
                    if ri == 7:
                        normalize(nb_, OTp, LTp, 512, CATT[256 + h * 64:256 + (h + 1) * 64, L + r0 * 64:L + r0 * 64 + 512], 'CATT')


                def na_ctx(h):
                    hv = slice(h * 64, (h + 1) * 64)
                    for kt in range(LT):
                        pc = psC[kt % 2]; pcs = pc16[kt % 2]
                        p.op('pe', lambda e: e.matmul(pc[:, 0:L], KTn[:, h, kt * 128:(kt + 1) * 128], QTn[:, h, 0:L], start=True, stop=True),
                             reads=[KTn, QTn], writes=[pc])
                        p.op('act', lambda e: e.activation(pcs[:, 0:L], pc[:, 0:L], AF.Exp), reads=[pc], writes=[pcs])
                        p.op('pe', lambda e: e.matmul(OTp[:, 0:L], V1c[:, kt, hv], pcs[:, 0:L], start=(kt == 0), stop=(kt == LT - 1)),
                             reads=[V1c, pcs], writes=[OTp])
                        p.op('pe', lambda e: e.matmul(LTp[:, 0:L], ones_b[:, 0:64], pcs[:, 0:L], start=(kt == 0), stop=(kt == LT - 1)),
                             reads=[ones_b, pcs], writes=[LTp])
                    normalize(nb_, OTp, LTp, L, CATT[256 + h * 64:256 + (h + 1) * 64, 0:L], 'CATT')

                if keep_ctx:
                    for h in range(4):
                        na_ctx(h)
                na_front(0)
                for i in range(len(rows_)):
                    if i + 1 < len(rows_):
                        na_front(i + 1)
                    na_back(i)
                p.barrier()

            with ExitStack() as ph:
                KTd = p.sb(ph, [96, 4, T], BF16)
                p.dma('sp', KTd[:], DKT.rearrange("h k t -> k h t"), reads=['DKT'], writes=[KTd])
                V1 = p.sb(ph, [128, NT, 256], BF16)
                for n8 in range(0, NT, 8):
                    n9 = min(NT, n8 + 8)
                    p.dma('sp', V1[:, n8:n9, :], DV[n8 * 128:n9 * 128, :].rearrange("(n p) c -> p n c", p=128), reads=['DV'], writes=[V1])
                QTt = [p.sb(ph, [96, 512], BF16) for _ in range(2)]
                psS = [p.ps(ph, [128, 2, 512]) for _ in range(3)]
                OTp = p.ps(ph, [64, 512])
                LTp = p.ps(ph, [64, 512])
                nb_ = (p.sb(ph, [64, 512], F32), p.sb(ph, [64, 512], BF16))
                pe16 = [p.sb(ph, [128, 2, 512], BF16) for _ in range(3)]
                steps = []
                qn = 0
                for h in range(4):
                    qtiles = []
                    if keep_ctx:
                        qtiles.append((0, L, list(range(LT))))
                    for q0 in range(L, T, 512):
                        qtiles.append((q0, min(512, T - q0), list(range(NT))))
                    for (q0, nq, kts) in qtiles:
                        pairs = [kts[i:i + 2] for i in range(0, len(kts), 2)]
                        for ki, kp in enumerate(pairs):
                            steps.append((h, q0, nq, kp, ki == 0, ki == len(pairs) - 1, qn))
                        qn += 1

                def mla_front(i):
                    h, q0, nq, kp, first, last, qi = steps[i]
                    qb = QTt[qi % 2]
                    if first:
                        p.dma('sp', qb[:, 0:nq], DQT[h, :, q0:q0 + nq], reads=['DQT'], writes=[qb])
                    ps = psS[i % 3]; pe = pe16[i % 3]
                    nk = len(kp)

                    def mms(e):
                        for j, kt in enumerate(kp):
                            r = e.matmul(ps[:, j, 0:nq], KTd[:, h, kt * 128:(kt + 1) * 128], qb[:, 0:nq], start=True, stop=True)
                        return r
                    p.op('pe', mms, reads=[KTd, qb], writes=[ps])
                    p.op('act', lambda e: e.activation(pe[:, 0:nk, 0:nq], ps[:, 0:nk, 0:nq], AF.Exp), reads=[ps], writes=[pe])

                def mla_back(i):
                    h, q0, nq, kp, first, last, qi = steps[i]
                    pe = pe16[i % 3]
                    nk = len(kp)

                    def mmv(e):
                        for j, kt in enumerate(kp):
                            e.matmul(OTp[:, 0:nq], V1[:, kt, h * 64:(h + 1) * 64], pe[:, j, 0:nq], start=(first and j == 0), stop=(last and j == nk - 1))
                        for j, kt in enumerate(kp):
                            r = e.matmul(LTp[:, 0:nq], ones_b[:, 0:64], pe[:, j, 0:nq], start=(first and j == 0), stop=(last and j == nk - 1))
                        return r
                    p.op('pe', mmv, reads=[V1, pe, ones_b], writes=[OTp, LTp])
                    if last:
                        normalize(nb_, OTp, LTp, nq, CATT[768 + h * 64:768 + (h + 1) * 64, q0:q0 + nq], 'CATT')

                LOOK = 2
                for i in range(min(LOOK, len(steps))):
                    mla_front(i)
                for i in range(len(steps)):
                    if i + LOOK < len(steps):
                        mla_front(i + LOOK)
                    mla_back(i)
                p.barrier()
            if dbg == 'M':
                break

            W01 = p.sb(lay, [128, NT, 2], F32)
            DESTi = p.sb(lay, [128, NT, 2], I32)
            IDXGU = p.sb(lay, [128, NBLK, 8], I32)
            IDXDN = p.sb(lay, [128, NBLK, 2], I32)
            with ExitStack() as ph:
                woutb = p.sb(ph, [128, 8, D], BF16)
                for kc in range(8):
                    p.dma('pool', woutb[:, kc, :], w_out[layer, kc * 128:(kc + 1) * 128, :], writes=[(woutb.name, kc)])
                RW = [(woutb.name, kc) for kc in range(8)]
                wr = p.sb(ph, [128, 8, 36], F32)
                p.dma('sp', wr[:], w_r[layer].rearrange("(k p) n -> p k n", p=128), writes=[wr])
                brb = p.sb(ph, [128, 36], F32)
                p.dma('sp', brb[:], b_r[layer:layer + 1, :].to_broadcast([128, 36]), writes=[brb])
                g2 = p.sb(ph, [128, D], F32)
                p.dma('sp', g2[:], norm2_g[layer:layer + 1, :].to_broadcast([128, D]), writes=[g2])
                M2 = [p.sb(ph, [128, D], F32) for _ in range(2)]
                G2 = [p.sb(ph, [128, D], F32) for _ in range(2)]
                B2 = [p.sb(ph, [128, D], F32) for _ in range(2)]
                for s in range(2):
                    p.dma('sp', M2[s][:], MOD[layer, s:s + 1, 2 * D:3 * D].to_broadcast([128, D]), writes=[M2[s]])
                    p.dma('sp', B2[s][:], MOD[layer, s:s + 1, 3 * D:4 * D].to_broadcast([128, D]), writes=[B2[s]])
                    p.dma('sp', G2[s][:], MOD[layer, s:s + 1, 4 * D:5 * D].to_broadcast([128, D]), writes=[G2[s]])
                    p.op('dve', lambda e: e.scalar_tensor_tensor(G2[s][:], G2[s][:], 1.0, g2[:], ALU.add, ALU.mult), reads=[G2[s], g2], writes=[G2[s]])
                M0 = p.sb(ph, [128, NT, 32], F32)
                M1 = p.sb(ph, [128, NT, 32], F32)
                Mh = p.sb(ph, [128, NT, 32], BF16)
                p.op('dve', lambda e: e.memset(M0[:], 0.0), writes=[M0])
                p.op('dve', lambda e: e.memset(M1[:], 0.0), writes=[M1])
                p.op('dve', lambda e: e.memset(Mh[:], 0.0), writes=[Mh])
                p.op('dve', lambda e: e.memset(W01[:], 0.0), writes=[W01])
                Bs = []
                for si in range(2):
                    Bs.append(dict(cb=p.sb(ph, [128, 8, 128], BF16), xb=p.sb(ph, [128, D], F32), tmp=p.sb(ph, [128, D], F32), xn=p.sb(ph, [128, D], F32),
                                   h2=p.sb(ph, [128, D], F32), h2b=p.sb(ph, [128, D], BF16), h2T=p.sb(ph, [128, 8, 128], F32), sx=p.sb(ph, [128, 16], F32),
                                   lg=p.sb(ph, [128, 36], F32), r8=p.sb(ph, [128, 4, 8], F32), sel=p.sb(ph, [128, 8], F32), sel2=p.sb(ph, [128, 8], F32),
                                   oh=p.sb(ph, [128, 3, 8], F32), po=p.ps(ph, [128, 512]), pt=p.ps(ph, [128, 4, 128]), pl=p.ps(ph, [128, 64])))
                pl = Bs[0]['pl']

                def o_stream(si):
                    def run():
                        for tt in range(T0 + si, NT, 2):
                            s = 1 if tt < LT else 0
                            tok = slice(tt * 128, (tt + 1) * 128)
                            cb, xb, tmp, xn, h2, h2b, h2T, sx, lg, r8, sel, sel2, oh, po, pt, pl = (Bs[si][k] for k in ('cb', 'xb', 'tmp', 'xn', 'h2', 'h2b', 'h2T', 'sx', 'lg', 'r8', 'sel', 'sel2', 'oh', 'po', 'pt', 'pl'))
                            p.dma('sp', cb[:], CATT[:, tok].rearrange("(c p) t -> p c t", p=128), reads=['CATT'], writes=[cb])
                            p.dma('sp', xb[:], X[tok, :], reads=[('X', tt)], writes=[xb])

                            for nb in range(2):
                                def mm(e):
                                    for c in range(8):
                                        r = e.matmul(po[:], cb[:, c, :], woutb[:, c, nb * 512:(nb + 1) * 512], start=(c == 0), stop=(c == 7))
                                    return r
                                p.op('pe', mm, reads=[cb] + RW, writes=[po])
                                p.op('dve', lambda e: e.tensor_mul(tmp[:, nb * 512:(nb + 1) * 512], po[:], M2[s][:, nb * 512:(nb + 1) * 512]), reads=[po, M2[s]], writes=[tmp])
                            p.op('dve', lambda e: e.tensor_add(xn[:], xb[:], tmp[:]), reads=[xb, tmp], writes=[xn])
                            p.dma('pool', X[tok, :], xn[:], reads=[xn], writes=[('X', tt)])
                            p.op('act', lambda e: e.activation(tmp[:], xn[:], AF.Square, accum_out=sx[:, 0:1]), reads=[xn], writes=[tmp, sx])
                            p.op('dve', lambda e: e.tensor_scalar(sx[:, 1:2], sx[:, 0:1], 1.0 / D, EPS, ALU.mult, ALU.add), reads=[sx], writes=[sx])
                            rsqrt(sx[:, 1:2], [sx])
                            p.op('dve', lambda e: e.scalar_tensor_tensor(tmp[:], xn[:], sx[:, 1:2], G2[s][:], ALU.mult, ALU.mult), reads=[xn, sx, G2[s]], writes=[tmp])
                            p.op('dve', lambda e: e.tensor_add(h2[:], tmp[:], B2[s][:]), reads=[tmp, B2[s]], writes=[h2])
                            p.op('act', lambda e: e.copy(h2b[:], h2[:]), reads=[h2], writes=[h2b])
                            p.dma('pool', H2[tok, :], h2b[:], reads=[h2b], writes=['H2'])
                            for rnd in range(2):
                                def tr(e):
                                    for k4 in range(4):
                                        kc = rnd * 4 + k4
                                        r = e.transpose(pt[:, k4, :], h2[:, kc * 128:(kc + 1) * 128], ident_f[:])
                                    return r
                                p.op('pe', tr, reads=[h2, ident_f], writes=[pt])
                                p.op('act', lambda e: e.copy(h2T[:, rnd * 4:(rnd + 1) * 4, :], pt[:]), reads=[pt], writes=[h2T])

                            def mmr(e):
                                for kc in range(8):
                                    r = e.matmul(pl[:, 0:36], h2T[:, kc, :], wr[:, kc, :], start=(kc == 0), stop=(kc == 7))
                                return r
                            p.op('pe', mmr, reads=[h2T, wr], writes=[pl])
                            p.op('dve', lambda e: e.tensor_add(lg[:], pl[:, 0:36], brb[:]), reads=[pl, brb], writes=[lg])
                            K_ = [sx, lg, r8, sel, sel2, oh]
                            p.op('dve', lambda e: e.tensor_reduce(sx[:, 2:3], lg[:, 0:4], AX.X, ALU.max), reads=K_, writes=[sx])
                            p.op('dve', lambda e: e.tensor_scalar(oh[:, 0, 0:4], lg[:, 0:4], sx[:, 2:3], None, ALU.is_equal), reads=K_, writes=[oh])
                            p.op('dve', lambda e: e.tensor_scalar(sx[:, 3:4], sx[:, 2:3], -1.0, None, ALU.mult), reads=K_, writes=[sx])
                            p.op('act', lambda e: e.activation(sel2[:, 0:4], lg[:, 0:4], AF.Exp, bias=sx[:, 3:4], accum_out=sx[:, 4:5]), reads=K_, writes=[sel2, sx])
                            p.op('dve', lambda e: e.reciprocal(sx[:, 5:6], sx[:, 4:5]), reads=K_, writes=[sx])
                            p.op('dve', lambda e: e.tensor_mul(r8[:], lg[:, 4:36].rearrange("p (g j) -> p g j", g=4), oh[:, 0, 0:4].unsqueeze(2).to_broadcast([128, 4, 8])), reads=K_, writes=[r8])
                            p.op('dve', lambda e: e.tensor_reduce(sel[:], r8[:].rearrange("p g j -> p j g"), AX.X, ALU.add), reads=K_, writes=[sel])
                            p.op('dve', lambda e: e.tensor_reduce(sx[:, 6:7], sel[:], AX.X, ALU.max), reads=K_, writes=[sx])
                            p.op('dve', lambda e: e.tensor_scalar(oh[:, 1, :], sel[:], sx[:, 6:7], None, ALU.is_equal), reads=K_, writes=[oh])
                            p.op('dve', lambda e: e.scalar_tensor_tensor(sel2[:], oh[:, 1, :], -1e30, sel[:], ALU.mult, ALU.add), reads=K_, writes=[sel2])
                            p.op('dve', lambda e: e.tensor_reduce(sx[:, 7:8], sel2[:], AX.X, ALU.max), reads=K_, writes=[sx])
                            p.op('dve', lambda e: e.tensor_scalar(oh[:, 2, :], sel2[:], sx[:, 7:8], None, ALU.is_equal), reads=K_, writes=[oh])
                            p.op('dve', lambda e: e.tensor_sub(sx[:, 8:9], sx[:, 6:7], sx[:, 7:8]), reads=K_, writes=[sx])
                            p.op('act', lambda e: e.activation(sx[:, 9:10], sx[:, 8:9], AF.Sigmoid), reads=K_, writes=[sx])
                            p.op('dve', lambda e: e.tensor_mul(W01[:, tt, 0:1], sx[:, 9:10], sx[:, 5:6]), reads=K_, writes=[W01])
                            p.op('dve', lambda e: e.tensor_sub(W01[:, tt, 1:2], sx[:, 5:6], W01[:, tt, 0:1]), reads=K_ + [W01], writes=[W01])
                            for k_, Mk in ((1, M0), (2, M1)):
                                p.op('dve', lambda e: e.tensor_mul(Mk[:, tt, :].rearrange("p (g j) -> p g j", g=4), oh[:, 0, 0:4].unsqueeze(2).to_broadcast([128, 4, 8]),
                                                                  oh[:, k_, :].unsqueeze(1).to_broadcast([128, 4, 8])), reads=K_, writes=[Mk])
                            p.op('dve', lambda e: e.tensor_add(Mh[:, tt, :], M0[:, tt, :], M1[:, tt, :]), reads=[M0, M1], writes=[Mh])
                    return run
                p.interleave([o_stream(0), o_stream(1)])
                lsb = p.sb(ph, [128, 128], BF16)
                lsf = p.sb(ph, [128, 128], F32)
                p.dma('sp', lsf[:], lstrict_d, writes=[lsf])
                p.op('dve', lambda e: e.tensor_copy(lsb[:], lsf[:]), reads=[lsf], writes=[lsb])
                carry = p.sb(ph, [128, 32], F32)
                RANK = p.sb(ph, [128, NT, 32], F32)
                p.op('dve', lambda e: e.memset(carry[:], 0.0), writes=[carry])
                p.op('dve', lambda e: e.memset(RANK[:], 0.0), writes=[RANK])
                for tt in range(T0, NT):
                    def mmk(e):
                        e.matmul(pl[:, 0:32], lsb[:], Mh[:, tt, :], start=True, stop=True)
                        return e.matmul(pl[:, 32:64], ones_b[:], Mh[:, tt, :], start=True, stop=True)
                    p.op('pe', mmk, reads=[lsb, ones_b, Mh], writes=[pl])
                    p.op('dve', lambda e: e.tensor_add(RANK[:, tt, :], pl[:, 0:32], carry[:]), reads=[pl, carry], writes=[RANK])
                    p.op('dve', lambda e: e.tensor_add(carry[:], carry[:], pl[:, 32:64]), reads=[pl, carry], writes=[carry])
                NK = (2 * T) // MOEB + 2
                thr = p.sb(ph, [128, NK], F32)
                p.op('pool', lambda e: e.iota(thr[:], [[MOEB, NK]], base=0, channel_multiplier=0, allow_small_or_imprecise_dtypes=True), writes=[thr])
                cmp_ = p.sb(ph, [128, 32, NK], F32)
                padded = p.sb(ph, [128, 32], F32)
                pend = [p.sb(ph, [128, 32], F32) for _ in range(2)]
                p.op('dve', lambda e: e.tensor_tensor(cmp_[:], carry[:].unsqueeze(2).to_broadcast([128, 32, NK]), thr[:].unsqueeze(1).to_broadcast([128, 32, NK]), ALU.is_gt),
                     reads=[carry, thr], writes=[cmp_])
                p.op('dve', lambda e: e.tensor_reduce(padded[:], cmp_[:], AX.X, ALU.add), reads=[cmp_], writes=[padded])
                p.op('dve', lambda e: e.tensor_scalar(padded[:], padded[:], float(MOEB), None, ALU.mult), reads=[padded], writes=[padded])
                p.op('dve', lambda e: e.tensor_copy(pend[0][:], padded[:]), reads=[padded], writes=[pend[0]])
                cur = 0
                for sft in (1, 2, 4, 8, 16):
                    a = pend[cur]; b2 = pend[1 - cur]
                    p.op('dve', lambda e: e.tensor_copy(b2[:, 0:sft], a[:, 0:sft]), reads=[a], writes=[b2])
                    p.op('dve', lambda e: e.tensor_add(b2[:, sft:32], a[:, sft:32], a[:, 0:32 - sft]), reads=[a, b2], writes=[b2])
                    cur = 1 - cur
                pe_ = pend[cur]
                pstart = pend[1 - cur]
                p.op('dve', lambda e: e.tensor_sub(pstart[:], pe_[:], padded[:]), reads=[pe_, padded], writes=[pstart])
                destf = p.sb(ph, [128, NT, 2], F32)
                big = p.sb(ph, [128, NT, 32], F32)
                p.op('dve', lambda e: e.tensor_add(RANK[:], RANK[:], pstart[:].unsqueeze(1).to_broadcast([128, NT, 32])), reads=[RANK, pstart], writes=[RANK])
                for k_, Mk in ((0, M0), (1, M1)):
                    p.op('dve', lambda e: e.tensor_mul(big[:], RANK[:], Mk[:]), reads=[RANK, Mk], writes=[big])
                    p.op('dve', lambda e: e.tensor_reduce(destf[:, :, k_], big[:], AX.X, ALU.add), reads=[big], writes=[destf])
                p.op('dve', lambda e: e.tensor_copy(DESTi[:], destf[:]), reads=[destf], writes=[DESTi])
                bvals = p.sb(ph, [128, NBLK], F32)
                p.op('pool', lambda e: e.iota(bvals[:], [[MOEB, NBLK]], base=0, channel_multiplier=0, allow_small_or_imprecise_dtypes=True), writes=[bvals])
                cmpb = p.sb(ph, [128, NBLK, 32], F32)
                bex = p.sb(ph, [128, NBLK], F32)
                p.op('dve', lambda e: e.tensor_tensor(cmpb[:], pe_[:].unsqueeze(1).to_broadcast([128, NBLK, 32]), bvals[:].unsqueeze(2).to_broadcast([128, NBLK, 32]), ALU.is_le),
                     reads=[pe_, bvals], writes=[cmpb])
                p.op('dve', lambda e: e.tensor_reduce(bex[:], cmpb[:], AX.X, ALU.add), reads=[cmpb], writes=[bex])
                p.op('dve', lambda e: e.tensor_scalar(bex[:], bex[:], float(NEXP - 1), None, ALU.min), reads=[bex], writes=[bex])
                pcol = p.sb(ph, [128, 1], F32)
                p.dma('sp', pcol[:], pcol_d, writes=[pcol])
                idxf = p.sb(ph, [128, NBLK, 8], F32)
                base = p.sb(ph, [128, NBLK], F32)
                p.op('dve', lambda e: e.tensor_scalar(base[:], bex[:], float(D), float(layer * NEXP * D), ALU.mult, ALU.add), reads=[bex], writes=[base])
                for kc in range(8):
                    p.op('dve', lambda e: e.tensor_scalar(idxf[:, :, kc], base[:], pcol[:, 0:1], float(kc * 128), ALU.add, ALU.add), reads=[base, pcol], writes=[idxf])
                p.op('dve', lambda e: e.tensor_copy(IDXGU[:], idxf[:]), reads=[idxf], writes=[IDXGU])
                p.op('dve', lambda e: e.tensor_scalar(base[:], bex[:], 256.0, float(layer * NEXP * 256), ALU.mult, ALU.add), reads=[bex], writes=[base])
                for fc in range(2):
                    p.op('dve', lambda e: e.tensor_scalar(idxf[:, :, fc], base[:], pcol[:, 0:1], float(fc * 128), ALU.add, ALU.add), reads=[base, pcol, IDXGU], writes=[idxf])
                p.op('dve', lambda e: e.tensor_copy(IDXDN[:], idxf[:, :, 0:2]), reads=[idxf], writes=[IDXDN])
                zt = p.sb(ph, [128, 8, D], BF16)
                p.op('dve', lambda e: e.memset(zt[:], 0.0), writes=[zt])
                for s0 in range(0, NSLOT, 1024):
                    n_ = min(1024, NSLOT - s0) // 128
                    p.dma('sp', XS[s0:s0 + n_ * 128, :].rearrange("(n p) d -> p n d", p=128), zt[:, 0:n_, :], reads=[zt], writes=['XS'])
                hd = [p.sb(ph, [128, D], BF16) for _ in range(2)]
                for tt in range(T0, NT):
                    hb_ = hd[tt % 2]
                    p.dma('sp', hb_[:], H2[tt * 128:(tt + 1) * 128, :], reads=['H2'], writes=[hb_])
                    for k_ in range(2):
                        p.idma(XS[:, :], bass.IndirectOffsetOnAxis(ap=DESTi[:, tt, k_:k_ + 1], axis=0), hb_[:], None,
                               reads=[hb_, DESTi, 'XS'], writes=[p.uk()])
                p.barrier()
            if dbg == 'O':
                break

            with ExitStack() as ph:
                sets = []
                for si in range(2):
                    B_ = dict(
                        wf=p.sb(ph, [128, 8, 512], F32), df=p.sb(ph, [128, 2, D], F32),
                        wgub=p.sb(ph, [128, 8, 512], BF16), wdnb=p.sb(ph, [128, 2, D], BF16),
                        xb=p.sb(ph, [128, 2, D], BF16), xsT=p.sb(ph, [128, 8, 256], BF16),
                        sg=p.sb(ph, [128, 2, 256], F32), hT=p.sb(ph, [128, 2, 256], BF16),
                        ysb=[p.sb(ph, [128, D], F32) for _ in range(2)],
                        pg=p.ps(ph, [128, 2, 256]), py=p.ps(ph, [128, D]), pst=p.ps(ph, [128, 8, 128], BF16))
                    sets.append(B_)

                def blk(b, B_):
                    wf, df, wgub, wdnb, xb, xsT, sg, hT, ysb, pg, py, pst = (B_[k] for k in ('wf', 'df', 'wgub', 'wdnb', 'xb', 'xsT', 'sg', 'hT', 'ysb', 'pg', 'py', 'pst'))
                    for kc in range(8):
                        p.idma(wf[:, kc, :], None, w_gu[:, :], bass.IndirectOffsetOnAxis(ap=IDXGU[:, b, kc:kc + 1], axis=0), reads=[IDXGU], writes=[(wf.name, kc)])
                    for fc in range(2):
                        p.idma(df[:, fc, :], None, w_dn[:, :], bass.IndirectOffsetOnAxis(ap=IDXDN[:, b, fc:fc + 1], axis=0), reads=[IDXDN], writes=[(df.name, fc)])
                    s0 = b * MOEB
                    p.dma('sp', xb[:], XS[s0:s0 + 256, :].rearrange("(s p) d -> p s d", p=128), reads=['XS'], writes=[xb])
                    p.op('act', lambda e: e.copy(wgub[:, 0:4, :], wf[:, 0:4, :]), reads=[(wf.name, kc) for kc in range(4)], writes=[(wgub.name, 0)])
                    p.op('dve', lambda e: e.tensor_copy(wgub[:, 4:8, :], wf[:, 4:8, :]), reads=[(wf.name, kc) for kc in range(4, 8)], writes=[(wgub.name, 1)])
                    p.op('act', lambda e: e.copy(wdnb[:, 0, :], df[:, 0, :]), reads=[(df.name, 0)], writes=[(wdnb.name, 0)])
                    p.op('dve', lambda e: e.tensor_copy(wdnb[:, 1, :], df[:, 1, :]), reads=[(df.name, 1)], writes=[(wdnb.name, 1)])
                    for s in range(2):
                        def tr(e):
                            for kc in range(8):
                                r = e.transpose(pst[:, kc, :], xb[:, s, kc * 128:(kc + 1) * 128], ident_b[:])
                            return r
                        p.op('pe', tr, reads=[xb, ident_b], writes=[pst])
                        p.op('act', lambda e: e.copy(xsT[:, :, s * 128:(s + 1) * 128], pst[:]), reads=[pst], writes=[xsT])
                    for half in range(2):
                        def mmg(e):
                            for n in range(2):
                                for kc in range(8):
                                    c0 = (half * 2 + n) * 128
                                    r = e.matmul(pg[:, n, :], wgub[:, kc, c0:c0 + 128], xsT[:, kc, :], start=(kc == 0), stop=(kc == 7))
                            return r
                        p.op('pe', mmg, reads=[(wgub.name, 0), (wgub.name, 1), xsT], writes=[pg])
                        if half == 0:
                            p.op('act', lambda e: e.activation(sg[:], pg[:], AF.Silu), reads=[pg], writes=[sg])
                        else:
                            p.op('dve', lambda e: e.tensor_mul(hT[:], sg[:], pg[:]), reads=[sg, pg], writes=[hT])
                    for s in range(2):
                        yb_ = ysb[s]

                        def mmy(e):
                            for nb in range(2):
                                for fc in range(2):
                                    r = e.matmul(py[:, nb * 512:(nb + 1) * 512], hT[:, fc, s * 128:(s + 1) * 128], wdnb[:, fc, nb * 512:(nb + 1) * 512], start=(fc == 0), stop=(fc == 1))
                            return r
                        p.op('pe', mmy, reads=[hT, (wdnb.name, 0), (wdnb.name, 1)], writes=[py])
                        if s == 0:
                            p.op('act', lambda e: e.copy(yb_[:], py[:]), reads=[py], writes=[yb_])
                        else:
                            p.op('dve', lambda e: e.tensor_copy(yb_[:], py[:]), reads=[py], writes=[yb_])
                        p.dma('sp', YS[s0 + s * 128:s0 + (s + 1) * 128, :], yb_[:], reads=[yb_], writes=[p.uk()])

                def run_set(si):
                    for b in range(si, NBLK, 2):
                        blk(b, sets[si])
                p.interleave([lambda: run_set(0), lambda: run_set(1)])
                p.barrier()

            with ExitStack() as ph:
                M5 = [p.sb(ph, [128, D], F32) for _ in range(2)]
                for s in range(2):
                    p.dma('sp', M5[s][:], MOD[layer, s:s + 1, 5 * D:6 * D].to_broadcast([128, D]), writes=[M5[s]])
                fg = p.sb(ph, [128, D], F32)
                p.dma('sp', fg[:], final_g[0:1, :].to_broadcast([128, D]), writes=[fg])
                g0 = [p.sb(ph, [128, D], F32) for _ in range(2)]
                g1_ = [p.sb(ph, [128, D], F32) for _ in range(2)]
                xt = [p.sb(ph, [128, D], F32) for _ in range(2)]
                ys_ = [p.sb(ph, [128, D], F32) for _ in range(2)]
                sxs = [p.sb(ph, [128, 4], F32) for _ in range(2)]

                def cb_stream(si):
                    def run():
                        for tt in range(T0 + si, NT, 2):
                            s = 1 if tt < LT else 0
                            tok = slice(tt * 128, (tt + 1) * 128)
                            a = g0[si]; b2 = g1_[si]; xb = xt[si]; y = ys_[si]; sx = sxs[si]
                            p.idma(a[:], None, YS[:, :], bass.IndirectOffsetOnAxis(ap=DESTi[:, tt, 0:1], axis=0), reads=['YS', DESTi], writes=[a])
                            p.idma(b2[:], None, YS[:, :], bass.IndirectOffsetOnAxis(ap=DESTi[:, tt, 1:2], axis=0), reads=['YS', DESTi], writes=[b2])
                            p.dma('sp', xb[:], X[tok, :], reads=[('X', tt)], writes=[xb])
                            p.op('dve', lambda e: e.tensor_scalar(y[:], a[:], W01[:, tt, 0:1], None, ALU.mult), reads=[a, W01], writes=[y])
                            p.op('dve', lambda e: e.scalar_tensor_tensor(y[:], b2[:], W01[:, tt, 1:2], y[:], ALU.mult, ALU.add), reads=[b2, W01, y], writes=[y])
                            p.op('dve', lambda e: e.tensor_mul(y[:], y[:], M5[s][:]), reads=[y, M5[s]], writes=[y])
                            p.op('dve', lambda e: e.tensor_add(xb[:], xb[:], y[:]), reads=[xb, y], writes=[xb])
                            if layer == 0:
                                p.dma('sp', X[tok, :], xb[:], reads=[xb], writes=[('X', tt)])
                            else:
                                p.op('act', lambda e: e.activation(y[:], xb[:], AF.Square, accum_out=sx[:, 0:1]), reads=[xb], writes=[y, sx])
                                p.op('dve', lambda e: e.tensor_scalar(sx[:, 1:2], sx[:, 0:1], 1.0 / D, EPS, ALU.mult, ALU.add), reads=[sx], writes=[sx])
                                rsqrt(sx[:, 1:2], [sx])
                                p.op('dve', lambda e: e.scalar_tensor_tensor(y[:], xb[:], sx[:, 1:2], fg[:], ALU.mult, ALU.mult), reads=[xb, sx, fg], writes=[y])
                                p.dma('sp', out[(tt - LT) * 128:(tt - LT + 1) * 128, :], y[:], reads=[y], writes=[p.uk()])
                    return run
                p.interleave([cb_stream(0), cb_stream(1)])
                p.barrier()
    return nc


_L, _N = 256, 4096
_NC_CACHE = {}


def _core_inputs(inp, b):
    L, N = _L, _N
    T = L + N
    f = lambda a: np.ascontiguousarray(np.asarray(a, dtype=np.float32))
    m = {}
    m['xin'] = f(np.concatenate([inp['ctx'][b], inp['x'][b]], axis=0))
    c2 = np.stack([np.asarray(inp['c'][b]), np.asarray(inp['c_ctx'])], axis=0)
    m['c2T'] = f(c2.reshape(2, 8, 128).transpose(2, 1, 0))
    return m


def _shared_inputs(inp):
    L, N = _L, _N
    T = L + N
    f = lambda a: np.ascontiguousarray(np.asarray(a, dtype=np.float32))
    m = {}
    for k in ['w_mod', 'b_mod', 'norm1_g', 'norm2_g', 'w_in', 'w_out', 'hgrn_norm_g', 'gla_norm_g', 'mla_w_uq', 'mla_w_ukv']:
        m[k] = f(inp[k])
    m['final_norm_g'] = f(np.asarray(inp['final_norm_g']).reshape(1, -1))
    m['hgrn_lb_logits'] = f(np.asarray(inp['hgrn_lb_logits']).reshape(2, 512))
    m['na_tab'] = f(na_bias_table(np.asarray(inp['na_rpb'], dtype=np.float32)).reshape(2, 4, 8, 64, 512))
    wg = np.zeros((2, 32, 256), np.float32)
    wg[:, 0:16, 0:128] = np.asarray(inp['gla_wg_f'])
    wg[:, 16:32, 128:256] = np.asarray(inp['gla_wg_b'])
    m['gla_wg_bd'] = wg
    m['gla_bg_cat'] = f(np.concatenate([np.asarray(inp['gla_bg_f']), np.asarray(inp['gla_bg_b'])], axis=1))
    m['mla_q_norm_g'] = f(np.asarray(inp['mla_q_norm_g']).reshape(2, 192, 1))
    m['mla_kv_norm_g'] = f(np.asarray(inp['mla_kv_norm_g']).reshape(2, 128, 1))
    m['moe_w_r'] = f(np.concatenate([np.asarray(inp['moe_w_rg']), np.asarray(inp['moe_w_re'])], axis=2))
    m['moe_b_r'] = f(np.concatenate([np.asarray(inp['moe_b_rg']), np.asarray(inp['moe_b_re'])], axis=1))
    m['moe_w_gu'] = f(np.asarray(inp['moe_w_gu']).reshape(2 * 32 * 1024, 512))
    m['moe_w_dn'] = f(np.asarray(inp['moe_w_dn']).reshape(2 * 32 * 256, 1024))
    hc = host_consts(L, N)
    hc['rope'] = hc['rope'].reshape(T, 32)
    m.update(hc)
    return m


def kernel(**inputs):
    inp = {k: np.asarray(v) for k, v in inputs.items()}
    B = inp['x'].shape[0]
    if 'nc' not in _NC_CACHE:
        _NC_CACHE['nc'] = build(_L, _N)
    nc = _NC_CACHE['nc']
    shared = _shared_inputs(inp)
    in_maps = []
    for core in range(8):
        m = dict(shared)
        m.update(_core_inputs(inp, core % B))
        in_maps.append(m)
    res = run_bass_kernel_spmd(nc, in_maps, core_ids=list(range(8)))
    outs = [np.asarray(res.results[b]['out'], dtype=np.float32) for b in range(B)]
    return np.stack(outs, axis=0)
```
